# Optimizing a Trainium2 kernel written in Bass

```python
import jax, jax.numpy as jnp
from jax import lax
import numpy as np

D_MODEL = 1024
BATCH = 8
SEQ = 4096
DEPTH = 2

GRID_W = 64
CTX_LEN = 256

N_BRANCH = 4
BRANCH_W = D_MODEL // N_BRANCH
NA_HEADS = 4
NA_HEAD_DIM = BRANCH_W // NA_HEADS
NA_WIN_H = 8
NA_WIN_W = 16
GLA_HEADS = 4
GLA_DV = BRANCH_W // GLA_HEADS
GLA_DK = GLA_DV // 2
GLA_GATE_RANK = 16
GLA_TAU = 16.0
GLA_CHUNK = 64
ROPE_BASE = 100.0
CONV_CH = BRANCH_W
CONV_WIDTH = 31
SGU_CH = BRANCH_W
SGU_GROUPS = 4
SGU_CHUNK = 128
PEER_HEADS = 8
PEER_NKEYS = 128
PEER_N_EXPERTS = PEER_NKEYS * PEER_NKEYS
PEER_DQ = 256
PEER_TOPK = 16
PEER_TOK_BLOCK = 128
ALPHA = (2 * DEPTH) ** 0.25
BETA = (8 * DEPTH) ** -0.25
NEG_INF = -1e30
LN_EPS = 1e-6

W_IN_SIZES = (3 * BRANCH_W, GLA_HEADS * GLA_DK, GLA_HEADS * GLA_DK, GLA_HEADS * GLA_DV, GLA_HEADS * GLA_DV,
              2 * GLA_GATE_RANK, 2 * CONV_CH, 2 * SGU_CH, N_BRANCH * D_MODEL)
W_IN_COLS = sum(W_IN_SIZES)
W_IN_SPLITS = tuple(int(s) for s in np.cumsum(W_IN_SIZES)[:-1])

kernel_name = "hybrid_dit_natten_gla_conformer_sgu_peer"


def layer_norm(x, g, b):
    xf = x.astype(jnp.float32)
    mu = xf.mean(-1, keepdims=True)
    var = jnp.square(xf - mu).mean(-1, keepdims=True)
    return ((xf - mu) * lax.rsqrt(var + LN_EPS)).astype(x.dtype) * g + b


def axial_rope(x, row, col):
    half = x.shape[-1] // 2
    nf = half // 2
    inv = 1.0 / (ROPE_BASE ** (jnp.arange(nf, dtype=jnp.float32) / nf))

    def rot(xa, pos):
        ang = pos.astype(jnp.float32)[:, None] * inv[None, :]
        cos = jnp.cos(ang)[None, :, None, :].astype(x.dtype)
        sin = jnp.sin(ang)[None, :, None, :].astype(x.dtype)
        x1, x2 = xa[..., :nf], xa[..., nf:]
        return jnp.concatenate([x1 * cos - x2 * sin, x1 * sin + x2 * cos], axis=-1)

    return jnp.concatenate([rot(x[..., :half], row), rot(x[..., half:], col)], axis=-1)


def neighbourhood_attention(qkv, qkv_c, rpb, need_ctx):
    B, T, _ = qkv.shape
    L = qkv_c.shape[1]
    rows = T // GRID_W
    kh = min(NA_WIN_H, rows)
    H, dh = NA_HEADS, NA_HEAD_DIM
    scale = dh ** -0.5
    q, k, v = [t.reshape(B, rows, GRID_W, H, dh) for t in jnp.split(qkv, 3, axis=-1)]
    qc, kc, vc = [t.reshape(B, L, H, dh) for t in jnp.split(qkv_c, 3, axis=-1)]
    r = jnp.arange(rows)
    r0 = jnp.clip(r - kh // 2, 0, rows - kh)
    key_rows = r0[:, None] + jnp.arange(kh)[None, :]
    kb = k[:, key_rows]
    vb = v[:, key_rows]
    colv = jnp.arange(GRID_W)
    c0 = jnp.clip(colv - NA_WIN_W // 2, 0, GRID_W - NA_WIN_W)
    in_win = (colv[None, :] >= c0[:, None]) & (colv[None, :] < c0[:, None] + NA_WIN_W)
    dr = key_rows - r[:, None] + NA_WIN_H - 1
    dc = jnp.clip(colv[None, :] - colv[:, None], -(NA_WIN_W - 1), NA_WIN_W - 1) + NA_WIN_W - 1
    bias = rpb[:, dr[:, None, :, None], dc[None, :, None, :]]
    s_win = jnp.einsum('brqhd,brkwhd->bhrqkw', q, kb).astype(jnp.float32) * scale + bias[None].astype(jnp.float32)
    s_win = jnp.where(in_win[:, None, :], s_win, NEG_INF)
    s_ctx = jnp.einsum('brqhd,blhd->bhrql', q, kc).astype(jnp.float32) * scale
    nwin = kh * GRID_W
    p = jax.nn.softmax(jnp.concatenate([s_win.reshape(B, H, rows, GRID_W, nwin), s_ctx], axis=-1), axis=-1).astype(v.dtype)
    p_win = p[..., :nwin].reshape(B, H, rows, GRID_W, kh, GRID_W)
    p_ctx = p[..., nwin:]
    o = jnp.einsum('bhrqkw,brkwhd->brqhd', p_win, vb) + jnp.einsum('bhrql,blhd->brqhd', p_ctx, vc)
    y = o.reshape(B, T, H * dh)
    if need_ctx:
        s_cc = jnp.einsum('blhd,bmhd->bhlm', qc, kc).astype(jnp.float32) * scale
        oc = jnp.einsum('bhlm,bmhd->blhd', jax.nn.softmax(s_cc, axis=-1).astype(vc.dtype), vc)
        yc = oc.reshape(B, L, H * dh)
    else:
        yc = None
    return y, yc


def gla_chunked(q, k, v, log_a, s0):
    B, T, H, dk = q.shape
    C = GLA_CHUNK
    N = T // C

    def chunks(t):
        return t.astype(jnp.float32).reshape(B, N, C, H, t.shape[-1]).transpose(0, 3, 1, 2, 4)

    qf, kf, vf, la = chunks(q), chunks(k), chunks(v), chunks(log_a)
    b = jnp.cumsum(la, axis=3)
    b_last = b[:, :, :, -1:]
    q_s = qf * jnp.exp(b)
    k_s = kf * jnp.exp(-b)
    k_end = kf * jnp.exp(b_last - b)
    lower = jnp.tril(jnp.ones((C, C), dtype=bool))
    a_intra = jnp.where(lower, jnp.einsum('bhncd,bhnsd->bhncs', q_s, k_s), 0.0)
    o_intra = jnp.einsum('bhncs,bhnse->bhnce', a_intra, vf)
    u = jnp.einsum('bhncd,bhnce->bhnde', k_end, vf)
    decay = jnp.exp(b_last[:, :, :, 0])

    def step(s, inp):
        d, un = inp
        return d[..., None] * s + un, s

    s_final, s_prev = lax.scan(step, s0, (decay.transpose(2, 0, 1, 3), u.transpose(2, 0, 1, 3, 4)))
    s_prev = s_prev.transpose(1, 2, 0, 3, 4)
    o = o_intra + jnp.einsum('bhncd,bhnde->bhnce', q_s, s_prev)
    return o.transpose(0, 2, 3, 1, 4).reshape(B, T, H, v.shape[-1]), s_final


def _gla_prep(q, k, v, lo, gate_up, gate_b):
    B, T, _ = q.shape
    q = q.reshape(B, T, GLA_HEADS, GLA_DK) * GLA_DK ** -0.5
    k = k.reshape(B, T, GLA_HEADS, GLA_DK)
    v = v.reshape(B, T, GLA_HEADS, GLA_DV)
    lo_f, lo_b = jnp.split(lo, 2, axis=-1)

    def log_gate(l, d):
        logits = (l @ gate_up[d] + gate_b[d]).astype(jnp.float32)
        return (jax.nn.log_sigmoid(logits) / GLA_TAU).reshape(B, T, GLA_HEADS, GLA_DK)

    return q, k, v, log_gate(lo_f, 0), log_gate(lo_b, 1)


def _gla_out(o, r, norm_g):
    B, T = o.shape[:2]
    o = o * lax.rsqrt(jnp.mean(jnp.square(o), axis=-1, keepdims=True) + LN_EPS) * norm_g
    return o.reshape(B, T, GLA_HEADS * GLA_DV).astype(r.dtype) * jax.nn.silu(r)


def gla_branch(zl, zc, gate_up, gate_b, norm_g, row, col, need_ctx):
    ql, kl, vl, lfl, lbl = _gla_prep(zl[0], zl[1], zl[2], zl[4], gate_up, gate_b)
    ql, kl = axial_rope(ql, row, col), axial_rope(kl, row, col)
    qc, kc, vc, lfc, lbc = _gla_prep(zc[0], zc[1], zc[2], zc[4], gate_up, gate_b)
    s0 = jnp.zeros((ql.shape[0], GLA_HEADS, GLA_DK, GLA_DV), jnp.float32)

    def flip(t):
        return jnp.flip(t, axis=1)

    oc_f, sc_f = gla_chunked(qc, kc, vc, lfc, s0)
    oc_b, sc_b = gla_chunked(flip(qc), flip(kc), flip(vc), flip(lbc), s0)
    ol_f, _ = gla_chunked(ql, kl, vl, lfl, sc_f)
    ol_b, _ = gla_chunked(flip(ql), flip(kl), flip(vl), flip(lbl), sc_b)
    y = _gla_out(ol_f + flip(ol_b), zl[3], norm_g)
    yc = _gla_out(oc_f + flip(oc_b), zc[3], norm_g) if need_ctx else None
    return y, yc


def conformer_conv(z, dw, b, ln_g, ln_b):
    a, g = jnp.split(z, 2, axis=-1)
    y = a * jax.nn.sigmoid(g)
    y = lax.conv_general_dilated(y, dw[:, None, :], window_strides=(1,),
                                 padding=[(CONV_WIDTH // 2, CONV_WIDTH // 2)],
                                 dimension_numbers=('NWC', 'WIO', 'NWC'),
                                 feature_group_count=CONV_CH) + b
    return jax.nn.silu(layer_norm(y, ln_g, ln_b))


def spatial_gating(z, ln_g, ln_b, ws, bs):
    z = jax.nn.gelu(z)
    u, v = jnp.split(z, 2, axis=-1)
    v = layer_norm(v, ln_g, ln_b)
    B, T, _ = v.shape
    vb = v.reshape(B, T // SGU_CHUNK, SGU_CHUNK, SGU_GROUPS, SGU_CH // SGU_GROUPS)
    s = jnp.einsum('gpq,bnqgc->bnpgc', ws, vb) + bs.T[None, None, :, :, None]
    return u * s.reshape(B, T, SGU_CH)


def merge_branches(ys, gates, w_branch, w_out):
    merged = None
    for i in range(N_BRANCH):
        term = jax.nn.sigmoid(gates[..., i * D_MODEL:(i + 1) * D_MODEL]) * (ys[i] @ w_branch[i])
        merged = term if merged is None else merged + term
    return merged @ w_out


def token_mixer(h, hc, row, col, need_ctx, w_in, na_rpb, gla_gate_up, gla_gate_b, gla_norm_g,
                conv_dw, conv_b, conv_ln_g, conv_ln_b, sgu_ln_g, sgu_ln_b, sgu_ws, sgu_bs, w_branch, w_out):
    z = jnp.split(h @ w_in, W_IN_SPLITS, axis=-1)
    zc = jnp.split(hc @ w_in, W_IN_SPLITS, axis=-1)
    y_na, yc_na = neighbourhood_attention(z[0], zc[0], na_rpb, need_ctx)
    y_gla, yc_gla = gla_branch(z[1:6], zc[1:6], gla_gate_up, gla_gate_b, gla_norm_g, row, col, need_ctx)
    y_cv = conformer_conv(z[6], conv_dw, conv_b, conv_ln_g, conv_ln_b)
    y_sg = spatial_gating(z[7], sgu_ln_g, sgu_ln_b, sgu_ws, sgu_bs)
    y = merge_branches((y_na, y_gla, y_cv, y_sg), z[8], w_branch, w_out)
    if need_ctx:
        yc_cv = conformer_conv(zc[6], conv_dw, conv_b, conv_ln_g, conv_ln_b)
        yc_sg = spatial_gating(zc[7], sgu_ln_g, sgu_ln_b, sgu_ws, sgu_bs)
        yc = merge_branches((yc_na, yc_gla, yc_cv, yc_sg), zc[8], w_branch, w_out)
    else:
        yc = None
    return y, yc


def peer_ffn(h, wq, keys, u_tab, v_tab):
    B, T, D = h.shape
    tok = h.reshape(-1, PEER_TOK_BLOCK, D)
    K = PEER_TOPK

    def block(xb):
        P = xb.shape[0]
        q = (xb @ wq).reshape(P, PEER_HEADS, 2, PEER_DQ // 2)
        s = jnp.einsum('phsd,hskd->phsk', q, keys).astype(jnp.float32)
        top_s, top_i = lax.top_k(s, K)
        cand_s = (top_s[:, :, 0, :, None] + top_s[:, :, 1, None, :]).reshape(P, PEER_HEADS, K * K)
        cand_i = (top_i[:, :, 0, :, None] * PEER_NKEYS + top_i[:, :, 1, None, :]).reshape(P, PEER_HEADS, K * K)
        best_s, best_j = lax.top_k(cand_s, K)
        idx = jnp.take_along_axis(cand_i, best_j, axis=-1)
        g = jax.nn.softmax(best_s, axis=-1).astype(xb.dtype)
        a = jax.nn.gelu(jnp.einsum('pd,phkd->phk', xb, u_tab[idx]))
        return jnp.einsum('phk,phkd->pd', g * a, v_tab[idx])

    return lax.map(block, tok).reshape(B, T, D)


def setup_inputs(seed: int = 0) -> dict:
    key = jax.random.key(seed)
    ks = iter(jax.random.split(key, 40))
    D, L = D_MODEL, DEPTH

    def nrm(shape, scale):
        return jax.random.normal(next(ks), shape, jnp.float32) * scale

    return {
        "x": nrm((BATCH, SEQ, D), 1.0),
        "c": nrm((BATCH, D), 1.0),
        "ctx": nrm((BATCH, CTX_LEN, D), 1.0),
        "c_ctx": nrm((D,), 1.0),
        "ada_w": nrm((L, D, 6 * D), 0.5 * D ** -0.5),
        "ada_b": nrm((L, 6 * D), 0.02),
        "w_in": nrm((L, D, W_IN_COLS), D ** -0.5),
        "na_rpb": nrm((L, NA_HEADS, 2 * NA_WIN_H - 1, 2 * NA_WIN_W - 1), 0.1),
        "gla_gate_up": nrm((L, 2, GLA_GATE_RANK, GLA_HEADS * GLA_DK), GLA_GATE_RANK ** -0.5),
        "gla_gate_b": nrm((L, 2, GLA_HEADS * GLA_DK), 0.1),
        "gla_norm_g": 1.0 + nrm((L, GLA_HEADS, GLA_DV), 0.02),
        "conv_dw": nrm((L, CONV_WIDTH, CONV_CH), CONV_WIDTH ** -0.5),
        "conv_b": nrm((L, CONV_CH), 0.02),
        "conv_ln_g": 1.0 + nrm((L, CONV_CH), 0.02),
        "conv_ln_b": nrm((L, CONV_CH), 0.02),
        "sgu_ln_g": 1.0 + nrm((L, SGU_CH), 0.02),
        "sgu_ln_b": nrm((L, SGU_CH), 0.02),
        "sgu_ws": nrm((L, SGU_GROUPS, SGU_CHUNK, SGU_CHUNK), 0.5 * SGU_CHUNK ** -0.5),
        "sgu_bs": 1.0 + nrm((L, SGU_GROUPS, SGU_CHUNK), 0.02),
        "w_branch": nrm((L, N_BRANCH, BRANCH_W, D), BRANCH_W ** -0.5),
        "w_out": nrm((L, D, D), BETA * D ** -0.5),
        "ln1_g": 1.0 + nrm((L, D), 0.02),
        "ln1_b": nrm((L, D), 0.02),
        "peer_wq": nrm((L, D, PEER_HEADS * PEER_DQ), D ** -0.5),
        "peer_keys": nrm((L, PEER_HEADS, 2, PEER_NKEYS, PEER_DQ // 2), (PEER_DQ // 2) ** -0.5),
        "peer_u": nrm((L, PEER_N_EXPERTS, D), D ** -0.5),
        "peer_v": nrm((L, PEER_N_EXPERTS, D), BETA),
        "ln2_g": 1.0 + nrm((L, D), 0.02),
        "ln2_b": nrm((L, D), 0.02),
    }


def reference(x, c, ctx, c_ctx, ada_w, ada_b, w_in, na_rpb, gla_gate_up, gla_gate_b, gla_norm_g,
              conv_dw, conv_b, conv_ln_g, conv_ln_b, sgu_ln_g, sgu_ln_b, sgu_ws, sgu_bs, w_branch, w_out,
              ln1_g, ln1_b, peer_wq, peer_keys, peer_u, peer_v, ln2_g, ln2_b):
    T = x.shape[1]
    t = jnp.arange(T)
    row, col = t // GRID_W, t % GRID_W
    c_silu = jax.nn.silu(c)
    cc_silu = jax.nn.silu(c_ctx)
    xc = ctx
    for l in range(DEPTH):
        need_ctx = l < DEPTH - 1
        mod = c_silu @ ada_w[l] + ada_b[l]
        mod_c = cc_silu @ ada_w[l] + ada_b[l]
        sh1, sc1, g1, sh2, sc2, g2 = [m[:, None, :] for m in jnp.split(mod, 6, axis=-1)]
        sh1c, sc1c, g1c, sh2c, sc2c, g2c = jnp.split(mod_c, 6, axis=-1)
        h = x * (1 + sc1) + sh1
        hc = xc * (1 + sc1c) + sh1c
        y, yc = token_mixer(h, hc, row, col, need_ctx, w_in[l], na_rpb[l], gla_gate_up[l], gla_gate_b[l],
                            gla_norm_g[l], conv_dw[l], conv_b[l], conv_ln_g[l], conv_ln_b[l], sgu_ln_g[l],
                            sgu_ln_b[l], sgu_ws[l], sgu_bs[l], w_branch[l], w_out[l])
        x = layer_norm(ALPHA * x + g1 * y, ln1_g[l], ln1_b[l])
        h2 = x * (1 + sc2) + sh2
        x = layer_norm(ALPHA * x + g2 * peer_ffn(h2, peer_wq[l], peer_keys[l], peer_u[l], peer_v[l]), ln2_g[l], ln2_b[l])
        if need_ctx:
            xc = layer_norm(ALPHA * xc + g1c * yc, ln1_g[l], ln1_b[l])
            h2c = xc * (1 + sc2c) + sh2c
            xc = layer_norm(ALPHA * xc + g2c * peer_ffn(h2c, peer_wq[l], peer_keys[l], peer_u[l], peer_v[l]), ln2_g[l], ln2_b[l])
    return x
```

```python
import numpy as np
from contextlib import ExitStack, contextmanager
import concourse.bass as bass
import concourse.mybir as mybir
from concourse.bass_utils import run_bass_kernel_spmd

F32 = mybir.dt.float32
BF16 = mybir.dt.bfloat16
I32 = mybir.dt.int32
U32 = mybir.dt.uint32
AF = mybir.ActivationFunctionType
ALU = mybir.AluOpType
AX = mybir.AxisListType

D_MODEL = 1024
SEQ = 4096
CTX = 256
NTOK = SEQ + CTX
NTT = NTOK // 128
DEPTH = 2
W_IN_COLS = 6688
ALPHA = (2 * DEPTH) ** 0.25
LN_EPS = 1e-6
C_QKV, C_GQ, C_GK, C_GV, C_GR, C_GLO, C_CONV, C_SGU, C_GATE = 0, 768, 896, 1024, 1280, 1536, 1568, 2080, 2592


class Buf:
    def __init__(self, h):
        self.h = h
        self.st = {}

    def __getitem__(self, idx):
        return self.h[idx]


class Sync:
    NDMA = 8

    def __init__(self, nc, stack):
        self.nc = nc
        self.stack = stack
        self.E = {}
        self.semobj = {}
        for name, eng in (("pe", nc.tensor), ("act", nc.scalar), ("dve", nc.vector),
                          ("pool", nc.gpsimd), ("sp", nc.sync)):
            sem = stack.enter_context(nc.semaphore("s_" + name))
            self.E[name] = dict(eng=eng, sem=sem, cnt=0, seen={}, dq=[], dn=0)
            self.semobj[id(sem)] = sem
        for q in ("sp", "pool", "act"):
            e = self.E[q]
            e["dq"] = [stack.enter_context(nc.semaphore("d_%s%d" % (q, i))) for i in range(self.NDMA)]
            for s in e["dq"]:
                self.semobj[id(s)] = s
        self.bg = [stack.enter_context(nc.semaphore("bg%d" % i)) for i in range(4)]
        for b in self.bg:
            self.semobj[id(b)] = b
        self.pending_dma = {}
        self.cur = stack
        self.uid = 0

    def tile(self, shape, dt, name=None):
        self.uid += 1
        return Buf(self.cur.enter_context(self.nc.sbuf_tensor("%s_%d" % (name or "t", self.uid), list(shape), dt)))

    def psum(self, shape, dt=F32, name=None):
        self.uid += 1
        return Buf(self.cur.enter_context(self.nc.psum_tensor("%s_%d" % (name or "p", self.uid), list(shape), dt)))

    @contextmanager
    def phase(self):
        prev = self.cur
        with ExitStack() as st:
            self.cur = st
            yield
            self.barrier()
        self.cur = prev

    @staticmethod
    def _merge(out, d):
        for k, v in d.items():
            if out.get(k, 0) < v:
                out[k] = v

    def _deps(self, reads, writes):
        out = {}
        for b, key in reads:
            keys = list(b.st.keys()) if key is None else [key, None]
            for k in keys:
                st = b.st.get(k)
                if st:
                    self._merge(out, st[0])
        for b, key in writes:
            keys = list(b.st.keys()) if key is None else [key, None]
            for k in keys:
                st = b.st.get(k)
                if st:
                    self._merge(out, st[0])
                    self._merge(out, st[1])
        return out

    def _wait(self, ename, deps):
        e = self.E[ename]
        own = id(e["sem"])
        for sid, val in deps.items():
            if ename == "pe" and sid == own:
                continue
            if e["seen"].get(sid, 0) >= val:
                continue
            e["eng"].wait_ge(self.semobj[sid], val)
            e["seen"][sid] = val

    def _mark(self, reads, writes, sid, val):
        for b, key in reads:
            st = b.st.setdefault(key, [{}, {}])
            if st[1].get(sid, 0) < val:
                st[1][sid] = val
        for b, key in writes:
            if key is None:
                b.st = {None: [{sid: val}, {}]}
            else:
                b.st[key] = [{sid: val}, {}]

    @staticmethod
    def _norm(lst):
        return [(x, None) if isinstance(x, Buf) else x for x in lst]

    def op(self, ename, fn, reads=(), writes=()):
        reads = self._norm(reads)
        writes = self._norm(writes)
        e = self.E[ename]
        self._wait(ename, self._deps(reads, writes))
        ins = fn(e["eng"])
        e["cnt"] += 1
        ins.then_inc(e["sem"], 1)
        self._mark(reads, writes, id(e["sem"]), e["cnt"])
        return ins

    def dma(self, qname, fn, reads=(), writes=()):
        reads = self._norm(reads)
        writes = self._norm(writes)
        e = self.E[qname]
        slot = e["dn"] % self.NDMA
        val = (e["dn"] // self.NDMA + 1) * 16
        sem = e["dq"][slot]
        deps = self._deps(reads, writes)
        if val > 16:
            self._merge(deps, {id(sem): val - 16})
        self._wait(qname, deps)
        ins = fn(e["eng"])
        ins.then_inc(sem, 16)
        e["dn"] += 1
        self._mark(reads, writes, id(sem), val)
        self._merge(self.pending_dma, {id(sem): val})
        return ins

    def bg_dma(self, qname, fn, sem, n_prev):
        e = self.E[qname]
        ins = fn(e["eng"])
        ins.then_inc(sem, 16)
        return (n_prev + 1) * 16

    def wait_sem(self, enames, sem, val):
        for n in enames:
            self._wait(n, {id(sem): val})

    def barrier(self):
        allv = dict(self.pending_dma)
        for n, e in self.E.items():
            if e["cnt"]:
                allv[id(e["sem"])] = e["cnt"]
        for n in self.E:
            self._wait(n, allv)

    def load(self, t, src, q="sp", key=None):
        return self.dma(q, lambda e: e.dma_start(out=t, in_=src), writes=[(self._b, key)] if False else [])


class Rot:
    def __init__(self, S, n, shape, dt, psum=False, name=None):
        self.bufs = [(S.psum(shape, dt, name) if psum else S.tile(shape, dt, name)) for _ in range(n)]
        self.i = 0

    def next(self):
        b = self.bufs[self.i % len(self.bufs)]
        self.i += 1
        return b


def phase_mod(S, D, l):
    with S.phase():
        cT = S.tile([128, 8, 2], F32, "cT")
        cs = S.tile([128, 8, 2], F32, "cs")
        ones = S.tile([1, 2], F32, "ones")
        ab = S.tile([1, 6144], F32, "ab")
        wa = Rot(S, 4, [128, 8, 512], F32, name="wa")
        pm = Rot(S, 4, [2, 512], F32, psum=True, name="pm")
        mr = Rot(S, 4, [2, 512], F32, name="mr")
        S.dma("sp", lambda e: e.dma_start(out=cT[:], in_=D["cvecT"]), writes=[cT])
        S.dma("sp", lambda e: e.dma_start(out=ab[:], in_=D["ada_b"][l:l + 1, :]), writes=[ab])
        S.op("dve", lambda e: e.memset(ones[:], 1.0), writes=[ones])
        S.op("act", lambda e: e.activation(out=cs[:], in_=cT[:], func=AF.Silu), reads=[cT], writes=[cs])
        aw = D["ada_w"][l].rearrange("(k p) n -> p k n", p=128)
        wl = {}

        def ld(n):
            w_ = wa.next()
            S.dma("sp", lambda e: e.dma_start(out=w_[:], in_=aw[:, :, n * 512:(n + 1) * 512]), writes=[w_])
            wl[n] = w_

        for n in range(3):
            ld(n)
        for n in range(12):
            if n + 3 < 12:
                ld(n + 3)
            w = wl.pop(n)
            p = pm.next()
            for k in range(8):
                S.op("pe", lambda e: e.matmul(p[:], lhsT=cs[:, k, :], rhs=w[:, k, :], start=(k == 0), stop=False),
                     reads=[cs, w], writes=[p])
            S.op("pe", lambda e: e.matmul(p[:], lhsT=ones[:], rhs=ab[:, n * 512:(n + 1) * 512], start=False, stop=True),
                 reads=[ones, ab], writes=[p])
            m = mr.next()
            plus1 = 1.0 if n in (2, 3, 8, 9) else 0.0
            S.op("dve", lambda e: e.tensor_scalar(out=m[:], in0=p[:], scalar1=plus1, scalar2=None, op0=ALU.add),
                 reads=[p], writes=[m])
            S.dma("sp", lambda e: e.dma_start(out=D["modv"][l, :, n * 512:(n + 1) * 512], in_=m[:]), reads=[m])


def bcast_load(S, D, l, j, c0, n, name):
    t = S.tile([128, n], F32, name)
    S.dma("sp", lambda e: e.dma_start(out=t[:], in_=D["modv"][l, j, c0:c0 + n].partition_broadcast(128)), writes=[t])
    return t


def vec_bcast(S, src, n, name):
    t = S.tile([128, n], F32, name)
    S.dma("sp", lambda e: e.dma_start(out=t[:], in_=src.partition_broadcast(128)), writes=[t])
    return t


def phase_win(S, D, l, tiles=range(NTT), xin="xs"):
    with S.phase():
        wb = S.tile([128, 8, W_IN_COLS], BF16, "wb")
        wv = D["w_in"][l].rearrange("(k p) n -> p k n", p=128)
        for k in range(8):
            S.dma("pool", lambda e: e.dma_start(out=wb[:, k, :], in_=wv[:, k, :]), writes=[(wb, k)])
        scb = [bcast_load(S, D, l, j, 1024, 1024, "scb") for j in range(2)]
        shb = [bcast_load(S, D, l, j, 0, 1024, "shb") for j in range(2)]
        xt = Rot(S, 3, [128, 1024], F32, name="xt")
        hf = Rot(S, 2, [128, 1024], F32, name="hf")
        hb = Rot(S, 2, [128, 1024], BF16, name="hb")
        hT = Rot(S, 3, [128, 8, 128], BF16, name="hT")
        pT = Rot(S, 2, [128, 8, 128], BF16, psum=True, name="pT")
        pz = Rot(S, 4, [128, 512], F32, psum=True, name="pz")
        zs = Rot(S, 3, [128, 2048], F32, name="zs")
        ident = D["ident_bf"]
        cnt = 0
        def st_l(tt):
            j = 0 if tt < 32 else 1
            x = xt.next()
            S.dma("sp", lambda e: e.dma_start(out=x[:], in_=D[xin][tt * 128:(tt + 1) * 128, :]), writes=[x])
            return dict(tt=tt, j=j, x=x)

        def st_a(c_):
            tt, j, x = c_["tt"], c_["j"], c_["x"]
            h1 = hf.next()
            S.op("dve", lambda e: e.tensor_tensor(out=h1[:], in0=x[:], in1=scb[j][:], op=ALU.mult), reads=[x, scb[j]], writes=[h1])
            h2 = hb.next()
            S.op("pool", lambda e: e.tensor_tensor(out=h2[:], in0=h1[:], in1=shb[j][:], op=ALU.add), reads=[h1, shb[j]], writes=[h2])
            p = pT.next()
            for k in range(8):
                S.op("pe", lambda e: e.transpose(out=p[:, k, :], in_=h2[:, k * 128:(k + 1) * 128], identity=ident[:]),
                     reads=[h2, ident], writes=[(p, k)])
            t = hT.next()
            S.op("act", lambda e: e.copy(out=t[:], in_=p[:]), reads=[p], writes=[t])
            c_.update(t=t)

        def st_b(c_):
            nonlocal cnt
            tt, t = c_["tt"], c_["t"]
            for g0 in range(0, W_IN_COLS, 2048):
                gw = min(2048, W_IN_COLS - g0)
                zt = zs.next()
                for c0 in range(g0, g0 + gw, 512):
                    cw = min(512, W_IN_COLS - c0)
                    pp = pz.next()
                    for k in range(8):
                        S.op("pe", lambda e: e.matmul(pp[:, :cw], lhsT=t[:, k, :], rhs=wb[:, k, c0:c0 + cw],
                                                      start=(k == 0), stop=(k == 7)), reads=[t, wb], writes=[pp])
                    eng = "act" if cnt % 2 == 0 else "dve"
                    cnt += 1
                    if eng == "act":
                        S.op("act", lambda e: e.copy(out=zt[:, c0 - g0:c0 - g0 + cw], in_=pp[:, :cw]), reads=[pp], writes=[(zt, c0)])
                    else:
                        S.op("dve", lambda e: e.tensor_copy(out=zt[:, c0 - g0:c0 - g0 + cw], in_=pp[:, :cw]), reads=[pp], writes=[(zt, c0)])
                S.dma("sp", lambda e: e.dma_start(out=D["z"][tt * 128:(tt + 1) * 128, g0:g0 + gw], in_=zt[:, :gw]), reads=[zt])

        tiles = list(tiles)
        st_ = {}
        nj = len(tiles)
        for i in range(nj + 2):
            if i < nj:
                st_[i] = st_l(tiles[i])
            if 0 <= i - 1 < nj:
                st_a(st_[i - 1])
            if 0 <= i - 2 < nj:
                st_b(st_.pop(i - 2))


def layer_norm(S, xin, xdeps, n, gb, bb, out, tmp, small):
    nch = (n + 511) // 512
    for i in range(nch):
        w = min(512, n - i * 512)
        S.op("dve", lambda e: e.bn_stats(out=small[:, 8 + 6 * i:8 + 6 * i + 6], in_=xin[:, i * 512:i * 512 + w]),
             reads=xdeps, writes=[(small, "st%d" % i)])
    S.op("dve", lambda e: e.bn_aggr(out=small[:, 0:2], in_=small[:, 8:8 + 6 * nch]), reads=[small], writes=[(small, "mv")])
    mh = D_CONST["mhalf"]
    S.op("pool", lambda e: e.tensor_scalar(out=small[:, 2:3], in0=small[:, 1:2], scalar1=LN_EPS, scalar2=None, op0=ALU.add),
         reads=[(small, "mv")], writes=[(small, "ve")])
    S.op("pool", lambda e: e.tensor_tensor(out=small[:, 4:5], in0=small[:, 2:3], in1=mh[:, 0:1], op=ALU.pow),
         reads=[(small, "ve"), mh], writes=[(small, "rs")])
    S.op("dve", lambda e: e.tensor_scalar(out=tmp[:, :n], in0=xin, scalar1=small[:, 0:1], scalar2=small[:, 4:5],
                                          op0=ALU.subtract, op1=ALU.mult), reads=list(xdeps) + [small], writes=[tmp])
    S.op("pool", lambda e: e.tensor_tensor(out=tmp[:, :n], in0=tmp[:, :n], in1=gb[:, :n], op=ALU.mult), reads=[tmp, gb], writes=[tmp])
    S.op("pool", lambda e: e.tensor_tensor(out=out[:, :n], in0=tmp[:, :n], in1=bb[:, :n], op=ALU.add), reads=[tmp, bb], writes=[out])


def phase_conv(S, D, l, do_ctx=True):
    PAD = 15
    with S.phase():
        dw = S.tile([128, 2, 31], F32, "dw")
        cb = S.tile([128, 2], F32, "cb")
        S.dma("sp", lambda e: e.dma_start(out=dw[:], in_=D["conv_dwT"][l].rearrange("(c p) j -> p c j", p=128)), writes=[dw])
        S.dma("sp", lambda e: e.dma_start(out=cb[:], in_=D["conv_bT"][l]), writes=[cb])
        lng = vec_bcast(S, D["conv_ln_g"][l], 256, "lng")
        lnb = vec_bcast(S, D["conv_ln_b"][l], 256, "lnb")
        identf = D["ident_f"]
        yT = S.tile([128, 2, PAD + SEQ + PAD], F32, "yT")
        cv = S.tile([128, 2, SEQ], F32, "cv")
        zt = Rot(S, 4, [128, 512], F32, name="zt")
        sg = Rot(S, 4, [128, 256], F32, name="sg")
        yy = Rot(S, 4, [128, 256], F32, name="yy")
        pcs = Rot(S, 4, [128, 256], F32, name="pcs")
        pt = Rot(S, 2, [128, 2, 128], F32, psum=True, name="pt")
        pb = Rot(S, 2, [128, 256], F32, psum=True, name="pb")
        tmp = Rot(S, 2, [128, 256], F32, name="tmp")
        sm = Rot(S, 2, [128, 32], F32, name="sm")
        ln = Rot(S, 2, [128, 256], F32, name="ln")
        yo = Rot(S, 2, [128, 256], F32, name="yo")
        for (base, T) in ((0, SEQ), (SEQ, CTX)) if do_ctx else ((0, SEQ),):
            S.op("pool", lambda e: e.memset(yT[:, :, 0:PAD], 0.0), writes=[(yT, "padl")])
            S.op("pool", lambda e: e.memset(yT[:, :, PAD + T:PAD + T + PAD], 0.0), writes=[(yT, "padr")])
            ys_ = {}

            def a_front(i):
                r0 = base + i * 128
                z = zt.next()
                S.dma("sp", lambda e: e.dma_start(out=z[:], in_=D["z"][r0:r0 + 128, C_CONV:C_CONV + 512]), writes=[z])
                s = sg.next()
                S.op("act", lambda e: e.activation(out=s[:], in_=z[:, 256:512], func=AF.Sigmoid), reads=[z], writes=[s])
                y = yy.next()
                S.op("dve", lambda e: e.tensor_tensor(out=y[:], in0=z[:, 0:256], in1=s[:], op=ALU.mult), reads=[z, s], writes=[y])
                ys_[i] = y

            NT_ = T // 128
            for i in range(min(2, NT_)):
                a_front(i)
            for i in range(NT_):
                if i + 2 < NT_:
                    a_front(i + 2)
                y = ys_.pop(i)
                p = pt.next()
                for c in range(2):
                    S.op("pe", lambda e: e.transpose(out=p[:, c, :], in_=y[:, c * 128:(c + 1) * 128], identity=identf[:]),
                         reads=[y, identf], writes=[(p, c)])
                S.op("act", lambda e: e.copy(out=yT[:, :, PAD + i * 128:PAD + (i + 1) * 128], in_=p[:]), reads=[p], writes=[(yT, i)])
            CH = 1024 if T >= 1024 else T
            for t0 in range(0, T, CH):
                for c in range(2):
                    key = ("cv", t0, c)
                    S.op("dve", lambda e: e.tensor_scalar(out=cv[:, c, t0:t0 + CH], in0=yT[:, c, t0:t0 + CH], scalar1=dw[:, c, 0:1],
                                                          scalar2=cb[:, c:c + 1], op0=ALU.mult, op1=ALU.add),
                         reads=[yT, dw, cb], writes=[(cv, key)])
                    for j in range(1, 31):
                        S.op("dve", lambda e: e.scalar_tensor_tensor(out=cv[:, c, t0:t0 + CH], in0=yT[:, c, t0 + j:t0 + j + CH],
                                                                     scalar=dw[:, c, j:j + 1], in1=cv[:, c, t0:t0 + CH],
                                                                     op0=ALU.mult, op1=ALU.add),
                             reads=[yT, dw, (cv, key)], writes=[(cv, key)])
            ps_ = {}

            def c_front(i):
                p_ = pb.next()
                for c in range(2):
                    S.op("pe", lambda e: e.transpose(out=p_[:, c * 128:(c + 1) * 128], in_=cv[:, c, i * 128:(i + 1) * 128], identity=identf[:]),
                         reads=[cv, identf], writes=[(p_, c)])
                pc_ = pcs.next()
                S.op("act", lambda e: e.copy(out=pc_[:], in_=p_[:]), reads=[p_], writes=[pc_])
                ps_[i] = pc_

            for i in range(min(2, NT_)):
                c_front(i)
            for i in range(NT_):
                if i + 2 < NT_:
                    c_front(i + 2)
                r0 = base + i * 128
                p = ps_.pop(i)
                t = tmp.next(); s = sm.next(); o = ln.next()
                layer_norm(S, p[:, :], [p], 256, lng, lnb, o, t, s)
                y = yo.next()
                S.op("act", lambda e: e.activation(out=y[:], in_=o[:], func=AF.Silu), reads=[o], writes=[y])
                S.dma("sp", lambda e: e.dma_start(out=D["ycat"][r0:r0 + 128, 512:768], in_=y[:]), reads=[y])
            S.barrier()


def phase_sgu(S, D, l, tiles=range(NTT)):
    with S.phase():
        ws = S.tile([128, 4, 128], F32, "ws")
        bs = S.tile([128, 4], F32, "bs")
        S.dma("sp", lambda e: e.dma_start(out=ws[:], in_=D["sgu_wsT"][l].rearrange("g q p -> q g p")), writes=[ws])
        S.dma("sp", lambda e: e.dma_start(out=bs[:], in_=D["sgu_bsT"][l]), writes=[bs])
        lng = vec_bcast(S, D["sgu_ln_g"][l], 256, "lng")
        lnb = vec_bcast(S, D["sgu_ln_b"][l], 256, "lnb")
        zt = Rot(S, 4, [128, 512], F32, name="zt")
        ge = Rot(S, 4, [128, 512], F32, name="ge")
        tmp = Rot(S, 2, [128, 256], F32, name="tmp")
        sm = Rot(S, 2, [128, 32], F32, name="sm")
        vn = Rot(S, 2, [128, 256], F32, name="vn")
        ps = Rot(S, 2, [128, 256], F32, psum=True, name="ps")
        sb = Rot(S, 2, [128, 256], F32, name="sb")
        yo = Rot(S, 2, [128, 256], F32, name="yo")
        def st_a(tt):
            r0 = tt * 128
            z = zt.next()
            S.dma("sp", lambda e: e.dma_start(out=z[:], in_=D["z"][r0:r0 + 128, C_SGU:C_SGU + 512]), writes=[z])
            g = ge.next()
            S.op("act", lambda e: e.activation(out=g[:], in_=z[:], func=AF.Gelu_apprx_tanh), reads=[z], writes=[g])
            return (r0, g)

        tiles = list(tiles)
        pend = {}
        for i in range(len(tiles) + 2):
            if i < len(tiles):
                pend[i] = st_a(tiles[i])
            if i - 2 < 0:
                continue
            r0, g = pend.pop(i - 2)
            t = tmp.next(); s = sm.next(); v = vn.next()
            layer_norm(S, g[:, 256:512], [g], 256, lng, lnb, v, t, s)
            p = ps.next()
            for gi in range(4):
                S.op("pe", lambda e: e.matmul(p[:, gi * 64:(gi + 1) * 64], lhsT=ws[:, gi, :], rhs=v[:, gi * 64:(gi + 1) * 64],
                                              start=True, stop=True), reads=[ws, v], writes=[(p, gi)])
            sbt = sb.next()
            S.op("dve", lambda e: e.tensor_tensor(out=sbt[:].rearrange("p (g c) -> p g c", g=4), in0=p[:].rearrange("p (g c) -> p g c", g=4),
                                                  in1=bs[:].unsqueeze(2).to_broadcast([128, 4, 64]), op=ALU.add),
                 reads=[p, bs], writes=[sbt])
            y = yo.next()
            S.op("dve", lambda e: e.tensor_tensor(out=y[:], in0=sbt[:], in1=g[:, 0:256], op=ALU.mult), reads=[sbt, g], writes=[y])
            S.dma("sp", lambda e: e.dma_start(out=D["ycat"][r0:r0 + 128, 768:1024], in_=y[:]), reads=[y])


def phase_merge(S, D, l, tiles=range(NTT), xin="xs", xout="xs"):
    with S.phase():
        wbr = S.tile([128, 8, 1024], BF16, "wbr")
        wo = S.tile([128, 8, 1024], BF16, "wo")
        S.dma("pool", lambda e: e.dma_start(out=wbr[:], in_=D["w_branch"][l].rearrange("(k p) n -> p k n", p=128)), writes=[wbr])
        S.dma("pool", lambda e: e.dma_start(out=wo[:], in_=D["w_out"][l].rearrange("(k p) n -> p k n", p=128)), writes=[wo])
        g1b = [bcast_load(S, D, l, j, 2048, 1024, "g1b") for j in range(2)]
        lng = vec_bcast(S, D["ln1_g"][l], 1024, "lng")
        lnb = vec_bcast(S, D["ln1_b"][l], 1024, "lnb")
        ident = D["ident_bf"]
        yt = Rot(S, 3, [128, 1024], F32, name="yt")
        yb = Rot(S, 2, [128, 1024], BF16, name="yb")
        yT = Rot(S, 2, [128, 8, 128], BF16, name="yT")
        gt = Rot(S, 3, [128, 4096], F32, name="gt")
        sg = Rot(S, 2, [128, 1024], F32, name="sg")
        tm = Rot(S, 2, [128, 1024], F32, name="tm")
        mg = Rot(S, 1, [128, 1024], F32, name="mg")
        mb = Rot(S, 3, [128, 1024], BF16, name="mb")
        mT = Rot(S, 2, [128, 8, 128], BF16, name="mT")
        xt = Rot(S, 4, [128, 1024], F32, name="xt")
        rr = Rot(S, 2, [128, 1024], F32, name="rr")
        sm = Rot(S, 2, [128, 32], F32, name="sm")
        xo = Rot(S, 2, [128, 1024], F32, name="xo")
        pT = Rot(S, 2, [128, 8, 128], BF16, psum=True, name="pT")
        pP = Rot(S, 2, [128, 1024], F32, psum=True, name="pP")
        pO = Rot(S, 1, [128, 1024], F32, psum=True, name="pO")
        def st_a(tt):
            j = 0 if tt < 32 else 1
            r0 = tt * 128
            y = yt.next()
            S.dma("sp", lambda e: e.dma_start(out=y[:], in_=D["ycat"][r0:r0 + 128, :]), writes=[y])
            g = gt.next()
            S.dma("sp", lambda e: e.dma_start(out=g[:], in_=D["z"][r0:r0 + 128, C_GATE:C_GATE + 4096]), writes=[g])
            x = xt.next()
            S.dma("sp", lambda e: e.dma_start(out=x[:], in_=D[xin][r0:r0 + 128, :]), writes=[x])
            return dict(j=j, r0=r0, y=y, g=g, x=x)

        def st_b(c_):
            j, r0, y, g, x = (c_[k] for k in ("j", "r0", "y", "g", "x"))
            b = yb.next()
            S.op("dve", lambda e: e.tensor_copy(out=b[:], in_=y[:]), reads=[y], writes=[b])
            p = pT.next()
            for k in range(8):
                S.op("pe", lambda e: e.transpose(out=p[:, k, :], in_=b[:, k * 128:(k + 1) * 128], identity=ident[:]),
                     reads=[b, ident], writes=[(p, k)])
            t = yT.next()
            S.op("act", lambda e: e.copy(out=t[:], in_=p[:]), reads=[p], writes=[t])
            m = mg.next()
            mbt = mb.next()
            for i in range(4):
                pp = pP.next()
                for half in range(2):
                    for kk in range(2):
                        S.op("pe", lambda e: e.matmul(pp[:, half * 512:(half + 1) * 512], lhsT=t[:, i * 2 + kk, :],
                                                      rhs=wbr[:, i * 2 + kk, half * 512:(half + 1) * 512], start=(kk == 0), stop=(kk == 1)),
                             reads=[t, wbr], writes=[(pp, half)])
                s = sg.next()
                S.op("act", lambda e: e.activation(out=s[:], in_=g[:, i * 1024:(i + 1) * 1024], func=AF.Sigmoid), reads=[g], writes=[s])
                if i == 0:
                    S.op("dve", lambda e: e.tensor_tensor(out=m[:], in0=pp[:], in1=s[:], op=ALU.mult), reads=[pp, s], writes=[m])
                else:
                    tmp = tm.next()
                    S.op("dve", lambda e: e.tensor_tensor(out=tmp[:], in0=pp[:], in1=s[:], op=ALU.mult), reads=[pp, s], writes=[tmp])
                    dst = mbt if i == 3 else m
                    S.op("pool", lambda e: e.tensor_tensor(out=dst[:], in0=m[:], in1=tmp[:], op=ALU.add), reads=[m, tmp], writes=[dst])
            c_.update(mbt=mbt)

        def st_c(c_):
            j, r0, x, mbt = (c_[k] for k in ("j", "r0", "x", "mbt"))
            p = pT.next()
            for k in range(8):
                S.op("pe", lambda e: e.transpose(out=p[:, k, :], in_=mbt[:, k * 128:(k + 1) * 128], identity=ident[:]),
                     reads=[mbt, ident], writes=[(p, k)])
            t2 = mT.next()
            S.op("act", lambda e: e.copy(out=t2[:], in_=p[:]), reads=[p], writes=[t2])
            po = pO.next()
            for half in range(2):
                for k in range(8):
                    S.op("pe", lambda e: e.matmul(po[:, half * 512:(half + 1) * 512], lhsT=t2[:, k, :],
                                                  rhs=wo[:, k, half * 512:(half + 1) * 512], start=(k == 0), stop=(k == 7)),
                         reads=[t2, wo], writes=[(po, half)])
            r = rr.next()
            S.op("dve", lambda e: e.tensor_tensor(out=r[:], in0=po[:], in1=g1b[j][:], op=ALU.mult), reads=[po, g1b[j]], writes=[r])
            S.op("dve", lambda e: e.scalar_tensor_tensor(out=r[:], in0=x[:], scalar=ALPHA, in1=r[:], op0=ALU.mult, op1=ALU.add),
                 reads=[x, r], writes=[r])
            tmp = tm.next(); s = sm.next(); o = xo.next()
            layer_norm(S, r[:, :], [r], 1024, lng, lnb, o, tmp, s)
            S.dma("sp", lambda e: e.dma_start(out=D[xout][r0:r0 + 128, :], in_=o[:]), reads=[o])

        tiles = list(tiles)
        st_ = {}
        nj = len(tiles)
        for i in range(nj + 2):
            if i < nj:
                st_[i] = st_a(tiles[i])
            if 0 <= i - 1 < nj:
                st_b(st_[i - 1])
            if 0 <= i - 2 < nj:
                st_c(st_.pop(i - 2))


def phase_na(S, D, l, need_ctx=True, rows=range(64), heads=range(4)):
    scale = 64 ** -0.5
    with S.phase():
        ident = D["ident_bf"]
        qkT = S.tile([128, 4, NTOK], BF16, "qkT")
        va = S.tile([128, 32, 256], BF16, "va")
        vb = S.tile([128, 31, 256], BF16, "vb")
        vc = S.tile([128, 2, 256], BF16, "vc")
        zv = D["z"]
        S.dma("pool", lambda e: e.dma_start(out=va[:], in_=zv[0:4096, 512:768].rearrange("(t p) c -> p t c", p=128)), writes=[va])
        S.dma("pool", lambda e: e.dma_start(out=vb[:], in_=zv[64:64 + 31 * 128, 512:768].rearrange("(t p) c -> p t c", p=128)), writes=[vb])
        S.dma("pool", lambda e: e.dma_start(out=vc[:], in_=zv[4096:4352, 512:768].rearrange("(t p) c -> p t c", p=128)), writes=[vc])
        with S.phase():
            qk = Rot(S, 3, [128, 512], BF16, name="qk")
            pq = Rot(S, 2, [128, 4, 128], BF16, psum=True, name="pq")
            for tt in range(NTT):
                t = qk.next()
                S.dma("pool", lambda e: e.dma_start(out=t[:], in_=zv[tt * 128:(tt + 1) * 128, 0:512]), writes=[t])
                p = pq.next()
                for c in range(4):
                    S.op("pe", lambda e: e.transpose(out=p[:, c, :], in_=t[:, c * 128:(c + 1) * 128], identity=ident[:]),
                         reads=[t, ident], writes=[(p, c)])
                S.op("act", lambda e: e.copy(out=qkT[:, :, tt * 128:(tt + 1) * 128], in_=p[:]), reads=[p], writes=[(qkT, tt)])
        bias = Rot(S, 4, [64, 8, 512], F32, name="bias")
        ps1 = Rot(S, 2, [128, 512], F32, psum=True, name="ps1")
        ps2 = Rot(S, 2, [128, 256], F32, psum=True, name="ps2")
        ppT = Rot(S, 2, [128, 6, 128], BF16, psum=True, name="ppT")
        po = Rot(S, 2, [128, 64], F32, psum=True, name="po")
        sc = Rot(S, 3, [128, 768], F32, name="sc")
        pe_ = Rot(S, 3, [128, 768], BF16, name="pexp")
        pT = Rot(S, 3, [128, 6, 128], BF16, name="pT")
        sm = Rot(S, 6, [128, 4], F32, name="sm")
        yo = Rot(S, 4, [128, 64], F32, name="yo")

        def att_s1(h, M, qcols, kwin, cls, vchunks, bt, out_ap):
            hp, pb = h // 2, (h % 2) * 64
            qTs = qkT[pb:pb + 64, hp, qcols[0]:qcols[1]]
            s = sc.next(); nk = 0
            if kwin is not None:
                p1 = ps1.next()
                S.op("pe", lambda e: e.matmul(p1[:M, :], lhsT=qTs, rhs=qkT[pb:pb + 64, 2 + hp, kwin:kwin + 512], start=True, stop=True),
                     reads=[qkT], writes=[p1])
                S.op("dve", lambda e: e.scalar_tensor_tensor(out=s[:M, 0:512], in0=p1[:M, :], scalar=scale, in1=bt[:M, cls, :],
                                                             op0=ALU.mult, op1=ALU.add), reads=[p1, bt], writes=[(s, "w")])
                nk = 512
            p2 = ps2.next()
            S.op("pe", lambda e: e.matmul(p2[:M, :], lhsT=qTs, rhs=qkT[pb:pb + 64, 2 + hp, 4096:4352], start=True, stop=True),
                 reads=[qkT], writes=[p2])
            S.op("act", lambda e: e.mul(out=s[:M, nk:nk + 256], in_=p2[:M, :], mul=scale), reads=[p2], writes=[(s, "c")])
            nk += 256
            m = sm.next()
            S.op("dve", lambda e: e.tensor_reduce(out=m[:M, 0:1], in_=s[:M, :nk], axis=AX.X, op=ALU.max, negate=True),
                 reads=[s], writes=[(m, 0)])
            return dict(h=h, M=M, vchunks=vchunks, out_ap=out_ap, s=s, m=m, nk=nk)

        def att_s2(c_):
            M, s, m, nk = c_["M"], c_["s"], c_["m"], c_["nk"]
            pe = pe_.next()
            S.op("act", lambda e: e.activation(out=pe[:M, :nk], in_=s[:M, :nk], func=AF.Exp, bias=m[:M, 0:1], scale=1.0,
                                               accum_out=m[:M, 1:2]), reads=[s, (m, 0)], writes=[pe, (m, 1)])
            nch = nk // 128
            pp = ppT.next()
            for kc in range(nch):
                S.op("pe", lambda e: e.transpose(out=pp[:, kc, :M], in_=pe[:M, kc * 128:(kc + 1) * 128], identity=ident[:M, :M]),
                     reads=[pe, ident], writes=[(pp, kc)])
            pt = pT.next()
            S.op("dve", lambda e: e.tensor_copy(out=pt[:, :nch, :M], in_=pp[:, :nch, :M]), reads=[pp], writes=[pt])
            c_.update(pt=pt, nch=nch)

        def att_s3(c_):
            h, M, vchunks, out_ap, m, pt, nch = (c_[k] for k in ("h", "M", "vchunks", "out_ap", "m", "pt", "nch"))
            o = po.next()
            for kc in range(nch):
                vbuf, vi = vchunks[kc]
                S.op("pe", lambda e: e.matmul(o[:M, :], lhsT=pt[:, kc, :M], rhs=vbuf[:, vi, h * 64:(h + 1) * 64],
                                              start=(kc == 0), stop=(kc == nch - 1)), reads=[pt, vbuf], writes=[o])
            S.op("dve", lambda e: e.reciprocal(out=m[:M, 2:3], in_=m[:M, 1:2]), reads=[(m, 1)], writes=[(m, 2)])
            y = yo.next()
            S.op("dve", lambda e: e.tensor_scalar(out=y[:M, :], in0=o[:M, :], scalar1=m[:M, 2:3], scalar2=None, op0=ALU.mult),
                 reads=[o, (m, 2)], writes=[y])
            S.dma("sp", lambda e: e.dma_start(out=out_ap, in_=y[:M, :]), reads=[y])

        jobs = []
        for h in heads:
            bt = bias.next()
            S.dma("sp", lambda e: e.dma_start(out=bt[:], in_=D["na_bias"][l, h].rearrange("c q k -> q c k")), writes=[bt])
            for r in rows:
                r0 = min(max(r - 4, 0), 56)
                cls = r if r < 4 else (4 if r <= 60 else r - 56)
                if r0 % 2 == 0:
                    vch = [(va, r0 // 2 + jj) for jj in range(4)]
                else:
                    vch = [(vb, (r0 - 1) // 2 + jj) for jj in range(4)]
                vch += [(vc, 0), (vc, 1)]
                jobs.append((h, 64, (r * 64, (r + 1) * 64), r0 * 64, cls, vch, bt,
                             D["ycat"][r * 64:(r + 1) * 64, h * 64:(h + 1) * 64]))
            if need_ctx:
                for ct in range(2):
                    jobs.append((h, 128, (4096 + ct * 128, 4096 + (ct + 1) * 128), None, None, [(vc, 0), (vc, 1)], None,
                                 D["ycat"][4096 + ct * 128:4096 + (ct + 1) * 128, h * 64:(h + 1) * 64]))
        st_ = {}
        nj = len(jobs)
        for i in range(nj + 2):
            if i < nj:
                st_[i] = att_s1(*jobs[i])
            if 0 <= i - 1 < nj:
                att_s2(st_[i - 1])
            if 0 <= i - 2 < nj:
                att_s3(st_.pop(i - 2))


def phase_gla(S, D, l, need_ctx=True, lat_chunks=64):
    qscale = 32 ** -0.5
    with S.phase():
        identf = D["ident_f"]
        identb = D["ident_bf"]
        ropeC = S.tile([64, 64, 2, 8], F32, "ropeC")
        ropeS = S.tile([64, 64, 2, 8], F32, "ropeS")
        mt = S.tile([64, 2, 64], F32, "mt")
        tri = S.tile([64, 2, 64], F32, "tri")
        blk = S.tile([128, 4, 64], F32, "blk")
        gw = S.tile([33, 2, 128], F32, "gw")
        ngb = vec_bcast(S, D["gla_norm_g"][l], 256, "ngb")
        for t, src in ((ropeC, D["ropeC"]), (ropeS, D["ropeS"]), (mt, D["gla_mt"]), (tri, D["gla_tri"]), (blk, D["gla_blk"]),
                       (gw, D["gla_gw"][l].rearrange("d k n -> k d n"))):
            S.dma("sp", lambda e: e.dma_start(out=t[:], in_=src), writes=[t])
        Sblk = S.tile([128, 256], F32, "Sblk")
        Sbf = S.tile([128, 256], BF16, "Sbf")
        zc_r = Rot(S, 10, [64, 800], F32, name="zc")
        qkr = Rot(S, 2, [64, 256], F32, name="qkr")
        rt = Rot(S, 4, [64, 8, 2, 8], F32, name="rt")
        loT = Rot(S, 3, [33, 64], F32, name="loT")
        for b in loT.bufs:
            S.op("pool", lambda e: e.memset(b[32:33, :], 1.0), writes=[(b, "one")])
        e1 = Rot(S, 2, [64, 128], F32, name="e1")
        sp = Rot(S, 2, [64, 128], F32, name="sp")
        eb = Rot(S, 5, [128, 64], F32, name="eb")
        enb = Rot(S, 3, [128, 64], F32, name="enb")
        qs = Rot(S, 3, [128, 64], BF16, name="qs")
        ks = Rot(S, 2, [128, 64], BF16, name="ks")
        ke = Rot(S, 2, [128, 64], BF16, name="ke")
        qb = Rot(S, 2, [128, 4, 64], BF16, name="qb")
        kend = Rot(S, 3, [64, 128], BF16, name="kend")
        am = Rot(S, 3, [64, 4, 64], BF16, name="am")
        vbf = Rot(S, 3, [64, 256], BF16, name="vbf")
        tu = Rot(S, 4, [128, 256], F32, name="tu")
        of_ = Rot(S, 10, [64, 256], F32, name="of")
        osum = Rot(S, 2, [64, 256], F32, name="osum")
        sq = Rot(S, 2, [64, 256], F32, name="sq")
        ms = Rot(S, 2, [64, 16], F32, name="ms")
        sr = Rot(S, 2, [64, 256], F32, name="sr")
        yo = Rot(S, 2, [64, 256], F32, name="yo")
        bankA = Rot(S, 3, [128, 512], F32, psum=True, name="bankA")
        pkT = Rot(S, 1, [64, 128], BF16, psum=True, name="pkT")
        pA = Rot(S, 1, [64, 256], F32, psum=True, name="pA")
        pO = Rot(S, 2, [64, 256], F32, psum=True, name="pO")
        pU = Rot(S, 1, [128, 256], F32, psum=True, name="pU")

        def chunk_l(tok0, latent, n, d, want_out):
            z = zc_r.next()
            S.dma("sp", lambda e: e.dma_start(out=z[:], in_=D["z"][tok0:tok0 + 64, C_GQ:C_GQ + 800]), writes=[z])
            o_pre = None
            if want_out and d == 1:
                o_pre = of_.next()
                S.dma("sp", lambda en: en.dma_start(out=o_pre[:], in_=D["ofwd"][tok0:tok0 + 64, :]), writes=[o_pre])
            return dict(tok0=tok0, latent=latent, n=n, d=d, want_out=want_out, z=z, o_pre=o_pre)

        def chunk_a1(c_):
            tok0, latent, n, d, want_out, z = (c_[k] for k in ("tok0", "latent", "n", "d", "want_out", "z"))
            if latent:
                q = qkr.next()
                zv = z[:, 0:256].rearrange("p (h a b f) -> p h a b f", h=8, a=2, b=2)
                qv = q[:].rearrange("p (h a b f) -> p h a b f", h=8, a=2, b=2)
                xa, xb_ = zv[:, :, :, 0, :], zv[:, :, :, 1, :]
                cosb = ropeC[:, n, :, :].unsqueeze(1).to_broadcast([64, 8, 2, 8])
                sinb = ropeS[:, n, :, :].unsqueeze(1).to_broadcast([64, 8, 2, 8])
                t1, t2, t3, t4 = rt.next(), rt.next(), rt.next(), rt.next()
                S.op("dve", lambda e: e.tensor_tensor(out=t1[:], in0=xa, in1=cosb, op=ALU.mult), reads=[z, ropeC], writes=[t1])
                S.op("pool", lambda e: e.tensor_tensor(out=t2[:], in0=xb_, in1=sinb, op=ALU.mult), reads=[z, ropeS], writes=[t2])
                S.op("pool", lambda e: e.tensor_tensor(out=t3[:], in0=xa, in1=sinb, op=ALU.mult), reads=[z, ropeS], writes=[t3])
                S.op("dve", lambda e: e.tensor_tensor(out=t4[:], in0=xb_, in1=cosb, op=ALU.mult), reads=[z, ropeC], writes=[t4])
                S.op("dve", lambda e: e.tensor_tensor(out=qv[:, :, :, 0, :], in0=t1[:], in1=t2[:], op=ALU.subtract), reads=[t1, t2], writes=[(q, "a")])
                S.op("pool", lambda e: e.tensor_tensor(out=qv[:, :, :, 1, :], in0=t3[:], in1=t4[:], op=ALU.add), reads=[t3, t4], writes=[(q, "b")])
                qsrc, qdep = q, q
            else:
                qsrc, qdep = z, z
            A = bankA.next()
            for c in range(2):
                S.op("pe", lambda e: e.transpose(out=A[:, c * 64:(c + 1) * 64], in_=qsrc[:, c * 128:(c + 1) * 128], identity=identf[:64, :64]),
                     reads=[qdep, identf], writes=[(A, "qk%d" % c)])
            S.op("pe", lambda e: e.transpose(out=A[0:32, 320:384], in_=z[:, 768:800], identity=identf[:64, :64]),
                 reads=[z, identf], writes=[(A, "lo")])
            lt = loT.next()
            S.op("act", lambda e: e.copy(out=lt[0:32, :], in_=A[0:32, 320:384]), reads=[(A, "lo")], writes=[(lt, "d")])
            c_.update(A=A, lt=lt)

        def chunk_a2(c_):
            d, A, lt = c_["d"], c_["A"], c_["lt"]
            S.op("pe", lambda e: e.matmul(A[0:64, 192:320], lhsT=lt[:, :], rhs=gw[:, d, :], start=True, stop=True),
                 reads=[lt, gw], writes=[(A, "g")])
            e = e1.next()
            S.op("act", lambda en: en.activation(out=e[:], in_=A[0:64, 192:320], func=AF.Exp, scale=-1.0), reads=[(A, "g")], writes=[e])
            s_ = sp.next()
            S.op("act", lambda en: en.activation(out=s_[:], in_=e[:], func=AF.Ln, bias=1.0, scale=1.0), reads=[e], writes=[s_])
            S.op("pe", lambda en: en.matmul(A[:, 128:192], lhsT=s_[:, :], rhs=mt[:, d, :], start=True, stop=True),
                 reads=[s_, mt], writes=[(A, "b")])
            ebt = eb.next(); enbt = enb.next()
            S.op("act", lambda en: en.activation(out=ebt[:], in_=A[:, 128:192], func=AF.Exp), reads=[(A, "b")], writes=[ebt])
            S.op("act", lambda en: en.activation(out=enbt[:], in_=A[:, 128:192], func=AF.Exp, scale=-1.0), reads=[(A, "b")], writes=[enbt])
            dec = ebt[:, 63:64] if d == 0 else ebt[:, 0:1]
            c_.update(ebt=ebt, enbt=enbt, dec=dec)

        def chunk_a3(c_):
            d, A, z, ebt, enbt, dec = (c_[k] for k in ("d", "A", "z", "ebt", "enbt", "dec"))
            qst = qs.next(); kst = ks.next(); ket = ke.next(); qbt = qb.next()
            S.op("dve", lambda en: en.scalar_tensor_tensor(out=qst[:], in0=A[:, 0:64], scalar=qscale, in1=ebt[:], op0=ALU.mult, op1=ALU.mult),
                 reads=[(A, "qk0"), ebt], writes=[qst])
            S.op("dve", lambda en: en.tensor_tensor(out=kst[:], in0=A[:, 64:128], in1=enbt[:], op=ALU.mult), reads=[(A, "qk1"), enbt], writes=[kst])
            S.op("dve", lambda en: en.scalar_tensor_tensor(out=ket[:], in0=A[:, 64:128], scalar=dec, in1=enbt[:], op0=ALU.mult, op1=ALU.mult),
                 reads=[(A, "qk1"), enbt, ebt], writes=[ket])
            S.op("pool", lambda en: en.tensor_tensor(out=qbt[:], in0=qst[:].unsqueeze(1).to_broadcast([128, 4, 64]), in1=blk[:], op=ALU.mult),
                 reads=[qst, blk], writes=[qbt])
            pk = pkT.next()
            S.op("pe", lambda en: en.transpose(out=pk[:], in_=ket[:], identity=identb[:]), reads=[ket, identb], writes=[pk])
            kt = kend.next()
            S.op("act", lambda en: en.copy(out=kt[:], in_=pk[:]), reads=[pk], writes=[kt])
            v = vbf.next()
            S.op("pool", lambda en: en.tensor_copy(out=v[:], in_=z[:, 256:512]), reads=[z], writes=[v])
            pa = pA.next()
            S.op("pe", lambda en: en.matmul(pa[:], lhsT=kst[:], rhs=qbt[:].rearrange("p h c -> p (h c)"), start=True, stop=True),
                 reads=[kst, qbt], writes=[pa])
            amt = am.next()
            S.op("dve", lambda en: en.tensor_tensor(out=amt[:], in0=pa[:].rearrange("p (h c) -> p h c", h=4),
                                                   in1=tri[:, d, :].unsqueeze(1).to_broadcast([64, 4, 64]), op=ALU.mult),
                 reads=[pa, tri], writes=[amt])
            pu = pU.next()
            S.op("pe", lambda en: en.matmul(pu[:], lhsT=kt[:], rhs=v[:], start=True, stop=True), reads=[kt, v], writes=[pu])
            tut = tu.next()
            S.op("dve", lambda en: en.tensor_tensor(out=tut[:], in0=pu[:], in1=blk[:].rearrange("p h c -> p (h c)"), op=ALU.mult),
                 reads=[pu, blk], writes=[tut])
            c_.update(qst=qst, kt=kt, amt=amt, v=v, tut=tut)

        def chunk_b(c_):
            tok0, d, want_out, z, ebt, dec, qst, kt, amt, v = (c_[k] for k in ("tok0", "d", "want_out", "z", "ebt", "dec", "qst", "kt", "amt", "v"))
            tut = c_["tut"]
            po = pO.next()
            S.op("pe", lambda en: en.matmul(po[:], lhsT=qst[:], rhs=Sbf[:], start=True, stop=False, skip_group_check=True),
                 reads=[qst, Sbf], writes=[po])
            for h in range(4):
                S.op("pe", lambda en: en.matmul(po[:, h * 64:(h + 1) * 64], lhsT=amt[:, h, :], rhs=v[:, h * 64:(h + 1) * 64],
                                               start=False, stop=True, skip_group_check=True), reads=[amt, v], writes=[po])
            S.op("dve", lambda en: en.scalar_tensor_tensor(out=Sblk[:], in0=Sblk[:], scalar=dec, in1=tut[:], op0=ALU.mult, op1=ALU.add),
                 reads=[Sblk, ebt, tut], writes=[Sblk])
            S.op("act", lambda en: en.copy(out=Sbf[:], in_=Sblk[:]), reads=[Sblk], writes=[Sbf])
            if not want_out:
                return
            if d == 0:
                o = of_.next()
                S.op("act", lambda en: en.copy(out=o[:], in_=po[:]), reads=[po], writes=[o])
                S.dma("sp", lambda en: en.dma_start(out=D["ofwd"][tok0:tok0 + 64, :], in_=o[:]), reads=[o])
                return
            o = c_["o_pre"]
            os_ = osum.next()
            S.op("dve", lambda en: en.tensor_tensor(out=os_[:], in0=po[:], in1=o[:], op=ALU.add), reads=[po, o], writes=[os_])
            q2 = sq.next()
            S.op("pool", lambda en: en.tensor_tensor(out=q2[:], in0=os_[:], in1=os_[:], op=ALU.mult), reads=[os_], writes=[q2])
            m = ms.next()
            S.op("dve", lambda en: en.tensor_reduce(out=m[:, 0:4], in_=q2[:].rearrange("p (h c) -> p h c", h=4), axis=AX.X, op=ALU.add),
                 reads=[q2], writes=[(m, 0)])
            S.op("dve", lambda en: en.tensor_scalar(out=m[:, 4:8], in0=m[:, 0:4], scalar1=1.0 / 64, scalar2=LN_EPS, op0=ALU.mult, op1=ALU.add),
                 reads=[(m, 0)], writes=[(m, 1)])
            S.op("act", lambda en: en.activation(out=m[:, 8:12], in_=m[:, 4:8], func=AF.Ln), reads=[(m, 1)], writes=[(m, 2)])
            S.op("act", lambda en: en.activation(out=m[:, 12:16], in_=m[:, 8:12], func=AF.Exp, scale=-0.5), reads=[(m, 2)], writes=[(m, 3)])
            S.op("dve", lambda en: en.tensor_tensor(out=os_[:].rearrange("p (h c) -> p h c", h=4), in0=os_[:].rearrange("p (h c) -> p h c", h=4),
                                                   in1=m[:, 12:16].unsqueeze(2).to_broadcast([64, 4, 64]), op=ALU.mult),
                 reads=[os_, (m, 3)], writes=[os_])
            S.op("pool", lambda en: en.tensor_tensor(out=os_[:], in0=os_[:], in1=ngb[0:64, :], op=ALU.mult), reads=[os_, ngb], writes=[os_])
            srt = sr.next()
            S.op("act", lambda en: en.activation(out=srt[:], in_=z[:, 512:768], func=AF.Exp, scale=-1.0), reads=[z], writes=[srt])
            S.op("pool", lambda en: en.tensor_scalar(out=srt[:], in0=srt[:], scalar1=1.0, scalar2=None, op0=ALU.add), reads=[srt], writes=[srt])
            S.op("dve", lambda en: en.reciprocal(out=srt[:], in_=srt[:]), reads=[srt], writes=[srt])
            S.op("pool", lambda en: en.tensor_tensor(out=srt[:], in0=srt[:], in1=z[:, 512:768], op=ALU.mult), reads=[srt, z], writes=[srt])
            y = yo.next()
            S.op("dve", lambda en: en.tensor_tensor(out=y[:], in0=os_[:], in1=srt[:], op=ALU.mult), reads=[os_, srt], writes=[y])
            S.dma("sp", lambda en: en.dma_start(out=D["ycat"][tok0:tok0 + 64, 256:512], in_=y[:]), reads=[y])

        for d in range(2):
            S.op("pool", lambda en: en.memset(Sblk[:], 0.0), writes=[Sblk])
            S.op("pool", lambda en: en.memset(Sbf[:], 0.0), writes=[Sbf])
            order = list(range(4)) if d == 0 else list(range(3, -1, -1))
            jobs = [(SEQ + 64 * i, False, 0, d, need_ctx) for i in order]
            order = list(range(lat_chunks)) if d == 0 else list(range(lat_chunks - 1, -1, -1))
            jobs += [(64 * i, True, i, d, True) for i in order]
            st_ = {}
            nj = len(jobs)
            PF = 4
            for i in range(nj + 3 + PF):
                if i < nj:
                    st_[i] = chunk_l(*jobs[i])
                if 0 <= i - PF < nj:
                    chunk_a1(st_[i - PF])
                if 0 <= i - PF - 1 < nj:
                    chunk_a2(st_[i - PF - 1])
                if 0 <= i - PF - 2 < nj:
                    chunk_a3(st_[i - PF - 2])
                if 0 <= i - PF - 3 < nj:
                    chunk_b(st_.pop(i - PF - 3))
            S.barrier()


def phase_peer_topk(S, D, l, tiles=range(NTT), xin="xs"):
    with S.phase():
        ident = D["ident_bf"]
        wq = S.tile([128, 8, 2048], BF16, "wq")
        S.dma("pool", lambda e: e.dma_start(out=wq[:], in_=D["peer_wq"][l].rearrange("(k p) n -> p k n", p=128)), writes=[wq])
        kT = S.tile([128, 16, 128], F32, "kT")
        S.dma("sp", lambda e: e.dma_start(out=kT[:], in_=D["peer_keysT"][l].rearrange("b d k -> d b k")), writes=[kT])
        iota = S.tile([128, 16, 16], F32, "iota")
        S.dma("sp", lambda e: e.dma_start(out=iota[:], in_=D["iota_kk"]), writes=[iota])
        scb = [bcast_load(S, D, l, j, 4096, 1024, "scb") for j in range(2)]
        shb = [bcast_load(S, D, l, j, 3072, 1024, "shb") for j in range(2)]
        xt = Rot(S, 2, [128, 1024], F32, name="xt")
        hf = Rot(S, 1, [128, 1024], F32, name="hf")
        hb = Rot(S, 2, [128, 1024], BF16, name="hb")
        hT = Rot(S, 2, [128, 8, 128], BF16, name="hT")
        qT = Rot(S, 2, [128, 16, 128], F32, name="qT")
        sc = Rot(S, 3, [128, 16, 128], F32, name="sc")
        wk16 = Rot(S, 1, [128, 16, 128], F32, name="wk16")
        wk8 = Rot(S, 1, [128, 8, 256], F32, name="wk8")
        t1 = Rot(S, 3, [128, 16, 16], F32, name="t1")
        ti = Rot(S, 3, [128, 16, 16], U32, name="ti")
        tif = Rot(S, 3, [128, 16, 16], F32, name="tif")
        cs = Rot(S, 2, [128, 8, 256], F32, name="cs")
        bs = Rot(S, 3, [128, 8, 16], F32, name="bs")
        bj = Rot(S, 3, [128, 8, 16], U32, name="bj")
        hi = Rot(S, 2, [128, 8, 16], U32, name="hi")
        lo = Rot(S, 2, [128, 8, 16], U32, name="lo")
        hif = Rot(S, 2, [128, 8, 16], F32, name="hif")
        lof = Rot(S, 2, [128, 8, 16], F32, name="lof")
        oh = Rot(S, 2, [128, 8, 16, 16], F32, name="oh")
        ee = Rot(S, 4, [128, 8, 16], F32, name="ee")
        ei = Rot(S, 2, [128, 128], I32, name="ei")
        gg = Rot(S, 2, [128, 8, 16], F32, name="gg")
        sm = Rot(S, 2, [128, 32], F32, name="sm")
        pT = Rot(S, 2, [128, 8, 128], BF16, psum=True, name="pT")
        pq = Rot(S, 3, [128, 4, 128], F32, psum=True, name="pq")
        def stage_a(tt):
            j = 0 if tt < 32 else 1
            r0 = tt * 128
            x = xt.next()
            S.dma("sp", lambda e: e.dma_start(out=x[:], in_=D[xin][r0:r0 + 128, :]), writes=[x])
            h1 = hf.next()
            S.op("pool", lambda e: e.tensor_tensor(out=h1[:], in0=x[:], in1=scb[j][:], op=ALU.mult), reads=[x, scb[j]], writes=[h1])
            h2 = hb.next()
            S.op("pool", lambda e: e.tensor_tensor(out=h2[:], in0=h1[:], in1=shb[j][:], op=ALU.add), reads=[h1, shb[j]], writes=[h2])
            p = pT.next()
            for k in range(8):
                S.op("pe", lambda e: e.transpose(out=p[:, k, :], in_=h2[:, k * 128:(k + 1) * 128], identity=ident[:]),
                     reads=[h2, ident], writes=[(p, k)])
            t = hT.next()
            S.op("act", lambda e: e.copy(out=t[:], in_=p[:]), reads=[p], writes=[t])
            q = qT.next()
            for g in range(4):
                pp = pq.next()
                for b in range(4):
                    blk = g * 4 + b
                    for k in range(8):
                        S.op("pe", lambda e: e.matmul(pp[:, b, :], lhsT=wq[:, k, blk * 128:(blk + 1) * 128], rhs=t[:, k, :],
                                                      start=(k == 0), stop=(k == 7)), reads=[wq, t], writes=[(pp, b)])
                S.op("act", lambda e: e.copy(out=q[:, g * 4:(g + 1) * 4, :], in_=pp[:]), reads=[pp], writes=[(q, g)])
            s = sc.next()
            for g in range(4):
                pp = pq.next()
                for b in range(4):
                    blk = g * 4 + b
                    S.op("pe", lambda e: e.matmul(pp[:, b, :], lhsT=q[:, blk, :], rhs=kT[:, blk, :], start=True, stop=True),
                         reads=[q, kT], writes=[(pp, b)])
                S.op("act", lambda e: e.copy(out=s[:, g * 4:(g + 1) * 4, :], in_=pp[:]), reads=[pp], writes=[(s, g)])
            return dict(r0=r0, s=s)

        def stage_b1(st_):
            r0, s = st_["r0"], st_["s"]
            tv = t1.next(); tix = ti.next()
            w16 = wk16.next()
            for blk in range(16):
                S.op("dve", lambda e: e.max(out=tv[:, blk, 0:8], in_=s[:, blk, :]), reads=[s], writes=[(tv, (blk, 0))])
            for blk in range(16):
                S.op("dve", lambda e: e.match_replace(out=w16[:, blk, :], in_to_replace=tv[:, blk, 0:8], in_values=s[:, blk, :], imm_value=-1e30),
                     reads=[s, (tv, (blk, 0))], writes=[(w16, blk)])
            for blk in range(16):
                S.op("dve", lambda e: e.max(out=tv[:, blk, 8:16], in_=w16[:, blk, :]), reads=[(w16, blk)], writes=[(tv, (blk, 1))])
            for blk in range(16):
                S.op("dve", lambda e: e.max_index(out=tix[:, blk, 0:8], in_max=tv[:, blk, 0:8], in_values=s[:, blk, :]),
                     reads=[s, (tv, (blk, 0))], writes=[(tix, (blk, 0))])
            for blk in range(16):
                S.op("dve", lambda e: e.max_index(out=tix[:, blk, 8:16], in_max=tv[:, blk, 8:16], in_values=w16[:, blk, :]),
                     reads=[(w16, blk), (tv, (blk, 1))], writes=[(tix, (blk, 1))])
            st_.update(tv=tv, tix=tix)

        def stage_b2(st_):
            tv, tix = st_["tv"], st_["tix"]
            tf = tif.next()
            S.op("pool", lambda e: e.tensor_copy(out=tf[:], in_=tix[:]), reads=[tix], writes=[tf])
            c = cs.next()
            tv4 = tv[:].rearrange("p (h s) k -> p h s k", s=2)
            S.op("dve", lambda e: e.tensor_tensor(out=c[:].rearrange("p h (i j) -> p h i j", i=16),
                                                  in0=tv4[:, :, 0, :].unsqueeze(3).to_broadcast([128, 8, 16, 16]),
                                                  in1=tv4[:, :, 1, :].unsqueeze(2).to_broadcast([128, 8, 16, 16]), op=ALU.add),
                 reads=[tv], writes=[c])
            b_ = bs.next(); bjx = bj.next()
            w8 = wk8.next()
            for h in range(8):
                S.op("dve", lambda e: e.max(out=b_[:, h, 0:8], in_=c[:, h, :]), reads=[c], writes=[(b_, (h, 0))])
            for h in range(8):
                S.op("dve", lambda e: e.match_replace(out=w8[:, h, :], in_to_replace=b_[:, h, 0:8], in_values=c[:, h, :], imm_value=-1e30),
                     reads=[c, (b_, (h, 0))], writes=[(w8, h)])
            for h in range(8):
                S.op("dve", lambda e: e.max(out=b_[:, h, 8:16], in_=w8[:, h, :]), reads=[(w8, h)], writes=[(b_, (h, 1))])
            for h in range(8):
                S.op("dve", lambda e: e.max_index(out=bjx[:, h, 0:8], in_max=b_[:, h, 0:8], in_values=c[:, h, :]),
                     reads=[c, (b_, (h, 0))], writes=[(bjx, (h, 0))])
            for h in range(8):
                S.op("dve", lambda e: e.max_index(out=bjx[:, h, 8:16], in_max=b_[:, h, 8:16], in_values=w8[:, h, :]),
                     reads=[(w8, h), (b_, (h, 1))], writes=[(bjx, (h, 1))])
            st_.update(tf=tf, b_=b_, bjx=bjx)

        def stage_b3(st_):
            r0, tf, b_, bjx = (st_[k] for k in ("r0", "tf", "b_", "bjx"))
            hx = hi.next(); lx = lo.next(); hfx = hif.next(); lfx = lof.next()
            S.op("dve", lambda e: e.tensor_single_scalar(out=hx[:], in_=bjx[:], scalar=4, op=ALU.logical_shift_right), reads=[bjx], writes=[hx])
            S.op("dve", lambda e: e.tensor_single_scalar(out=lx[:], in_=bjx[:], scalar=15, op=ALU.bitwise_and), reads=[bjx], writes=[lx])
            S.op("pool", lambda e: e.tensor_copy(out=hfx[:], in_=hx[:]), reads=[hx], writes=[hfx])
            S.op("pool", lambda e: e.tensor_copy(out=lfx[:], in_=lx[:]), reads=[lx], writes=[lfx])
            tf4 = tf[:].rearrange("p (h s) k -> p h s k", s=2)
            es = []
            for (sel, half) in ((hfx, 0), (lfx, 1)):
                o = oh.next()
                S.op("dve", lambda e: e.tensor_tensor(out=o[:], in0=sel[:].unsqueeze(3).to_broadcast([128, 8, 16, 16]),
                                                      in1=iota[:].unsqueeze(1).to_broadcast([128, 8, 16, 16]), op=ALU.is_equal),
                     reads=[sel, iota], writes=[o])
                S.op("pool", lambda e: e.tensor_tensor(out=o[:], in0=o[:], in1=tf4[:, :, half, :].unsqueeze(2).to_broadcast([128, 8, 16, 16]),
                                                       op=ALU.mult), reads=[o, tf], writes=[o])
                ex = ee.next()
                S.op("dve", lambda e: e.tensor_reduce(out=ex[:].rearrange("p h k -> p (h k)"), in_=o[:].rearrange("p h k i -> p (h k) i"),
                                                      axis=AX.X, op=ALU.add), reads=[o], writes=[ex])
                es.append(ex)
            ef = ee.next()
            S.op("dve", lambda e: e.scalar_tensor_tensor(out=ef[:], in0=es[0][:], scalar=128.0, in1=es[1][:], op0=ALU.mult, op1=ALU.add),
                 reads=[es[0], es[1]], writes=[ef])
            eix = ei.next()
            S.op("dve", lambda e: e.tensor_copy(out=eix[:], in_=ef[:].rearrange("p h k -> p (h k)")), reads=[ef], writes=[eix])
            identf = D["ident_f"]
            ptr = pq.next()
            S.op("pe", lambda e: e.transpose(out=ptr[:, 0, :], in_=ef[:].rearrange("p h k -> p (h k)"), identity=identf[:]),
                 reads=[ef, identf], writes=[(ptr, 0)])
            S.op("dve", lambda e: e.tensor_copy(out=eix[:], in_=ptr[:, 0, :]), reads=[(ptr, 0)], writes=[eix])
            S.dma("sp", lambda e: e.dma_start(out=D["pidx"][r0:r0 + 128, :], in_=eix[:]), reads=[eix])
            g_ = gg.next(); m = sm.next()
            S.op("dve", lambda e: e.tensor_tensor(out=g_[:], in0=b_[:], in1=b_[:, :, 0:1].to_broadcast([128, 8, 16]), op=ALU.subtract),
                 reads=[b_], writes=[g_])
            S.op("act", lambda e: e.activation(out=g_[:], in_=g_[:], func=AF.Exp), reads=[g_], writes=[g_])
            S.op("dve", lambda e: e.tensor_reduce(out=m[:, 0:8], in_=g_[:], axis=AX.X, op=ALU.add), reads=[g_], writes=[(m, 0)])
            S.op("dve", lambda e: e.reciprocal(out=m[:, 8:16], in_=m[:, 0:8]), reads=[(m, 0)], writes=[(m, 1)])
            S.op("dve", lambda e: e.tensor_tensor(out=g_[:], in0=g_[:], in1=m[:, 8:16].unsqueeze(2).to_broadcast([128, 8, 16]), op=ALU.mult),
                 reads=[g_, (m, 1)], writes=[g_])
            S.op("pe", lambda e: e.transpose(out=ptr[:, 1, :], in_=g_[:].rearrange("p h k -> p (h k)"), identity=identf[:]),
                 reads=[g_, identf], writes=[(ptr, 1)])
            gT_ = qT.next()
            S.op("act", lambda e: e.copy(out=gT_[:, 0, :], in_=ptr[:, 1, :]), reads=[(ptr, 1)], writes=[gT_])
            S.dma("sp", lambda e: e.dma_start(out=D["pgt"][r0:r0 + 128, :], in_=gT_[:, 0, :]), reads=[gT_])


        tiles = list(tiles)
        stt = {}
        nj = len(tiles)
        for i in range(nj + 3):
            if i < nj:
                stt[i] = stage_a(tiles[i])
            if 0 <= i - 1 < nj:
                stage_b1(stt[i - 1])
            if 0 <= i - 2 < nj:
                stage_b2(stt[i - 2])
            if 0 <= i - 3 < nj:
                stage_b3(stt.pop(i - 3))


TAB_CHUNK = 512


def start_table_convert(S, D, l):
    vals = []
    for ti, nm in enumerate(("peer_u", "peer_v")):
        sem = S.bg[2 * l + ti]
        n = 0
        for r0 in range(0, 16384, TAB_CHUNK):
            v = S.bg_dma("pool", lambda e: e.dma_start(out=D["peer_uvb%d" % l][r0:r0 + TAB_CHUNK, ti * 1024:(ti + 1) * 1024],
                                                       in_=D["%s%d" % (nm, l)][r0:r0 + TAB_CHUNK, :]), sem, n)
            n += 1
        vals.append(v)
    return vals


def phase_peer_ffn(S, D, l, tiles=range(NTT), xin="xs", xout="xs", conv_vals=None):
    tiles = list(tiles)
    if conv_vals is not None:
        for ti in range(2):
            S.wait_sem(("pool", "sp"), S.bg[2 * l + ti], conv_vals[ti])
    with S.phase():
        ident = D["ident_bf"]
        uvb = D["peer_uvb%d" % l]
        scb = [bcast_load(S, D, l, j, 4096, 1024, "scb") for j in range(2)]
        shb = [bcast_load(S, D, l, j, 3072, 1024, "shb") for j in range(2)]
        g2b = [bcast_load(S, D, l, j, 5120, 1024, "g2b") for j in range(2)]
        lng = vec_bcast(S, D["ln2_g"][l], 1024, "lng")
        lnb = vec_bcast(S, D["ln2_b"][l], 1024, "lnb")
        xt = Rot(S, 3, [128, 1024], F32, name="xt")
        hf = Rot(S, 2, [128, 1024], F32, name="hf")
        hb = Rot(S, 3, [128, 1024], BF16, name="hb")
        idx = Rot(S, 3, [128, 128], I32, name="idx")
        gt = Rot(S, 3, [128, 128], F32, name="gt")
        gb = Rot(S, 10, [128, 2048], BF16, name="gb")
        junk = Rot(S, 2, [128, 1024], BF16, name="junk")
        aT = Rot(S, 2, [128, 128], F32, name="aT")
        cf = Rot(S, 2, [128, 128], F32, name="cf")
        zb = Rot(S, 6, [128, 255], BF16, name="zb")
        for b in zb.bufs:
            S.op("pool", lambda e: e.memset(b[:], 0.0), writes=[b])
        tm = Rot(S, 2, [128, 1024], F32, name="tm")
        sm = Rot(S, 2, [128, 32], F32, name="sm")
        xo = Rot(S, 2, [128, 1024], F32, name="xo")
        pb = Rot(S, 3, [128, 1024], F32, psum=True, name="pb")
        po_r = Rot(S, 1, [128, 1024], F32, psum=True, name="po")
        def load_stage(tt):
            j = 0 if tt < 32 else 1
            r0 = tt * 128
            x = xt.next(); ix = idx.next(); g = gt.next()
            S.dma("sp", lambda e: e.dma_start(out=ix[:], in_=D["pidx"][r0:r0 + 128, :]), writes=[ix])
            S.dma("sp", lambda e: e.dma_start(out=x[:], in_=D[xin][r0:r0 + 128, :]), writes=[x])
            S.dma("sp", lambda e: e.dma_start(out=g[:], in_=D["pgt"][r0:r0 + 128, :]), writes=[g])
            h1 = hf.next()
            S.op("dve", lambda e: e.tensor_tensor(out=h1[:], in0=x[:], in1=scb[j][:], op=ALU.mult), reads=[x, scb[j]], writes=[h1])
            h = hb.next()
            S.op("dve", lambda e: e.tensor_tensor(out=h[:], in0=h1[:], in1=shb[j][:], op=ALU.add), reads=[h1, shb[j]], writes=[h])
            return (j, r0, x, ix, g, h)

        nxt = load_stage(tiles[0]) if tiles else None
        for ti_, tt in enumerate(tiles):
            j, r0, x, ix, g, h = nxt
            nxt = load_stage(tiles[ti_ + 1]) if ti_ + 1 < len(tiles) else None
            at = aT.next(); c = cf.next(); po = po_r.next()
            LAG = 3
            uvs = {}

            pbs = {}

            def stage_a0(t):
                uv = gb.next()
                uvs[t] = uv
                S.dma("pool", lambda e: e.indirect_dma_start(out=uv[:], out_offset=None, in_=uvb,
                                                             in_offset=bass.IndirectOffsetOnAxis(ap=ix[:, t:t + 1], axis=0)),
                      reads=[ix], writes=[uv])
                p = pb.next()
                pbs[t] = p
                for half in range(2):
                    S.op("pe", lambda e: e.matmul(p[:, half * 512:(half + 1) * 512], lhsT=ident[:, t:t + 1].to_broadcast([128, 128]),
                                                  rhs=h[:, half * 512:(half + 1) * 512], start=True, stop=True),
                         reads=[ident, h], writes=[(p, half)])

            def stage_a1(t):
                uv = uvs[t]
                p = pbs.pop(t)
                jk = junk.next()
                S.op("dve", lambda e: e.scalar_tensor_tensor(out=jk[:], in0=uv[:, 0:1024], scalar=1.0, in1=p[:], op0=ALU.mult, op1=ALU.mult,
                                                             accum_out=at[:, t:t + 1]), reads=[(uv, "u"), p], writes=[jk, (at, t)])
                S.op("act", lambda e: e.activation(out=c[:, t:t + 1], in_=at[:, t:t + 1], func=AF.Gelu_apprx_tanh),
                     reads=[(at, t)], writes=[(c, t)])

            def stage_b(t):
                uv = uvs.pop(t)
                z = zb.next()
                S.op("dve", lambda e: e.tensor_tensor(out=z[:, 127:128], in0=c[:, t:t + 1], in1=g[:, t:t + 1], op=ALU.mult),
                     reads=[(c, t), g], writes=[z])
                for half in range(2):
                    S.op("pe", lambda e: e.matmul(po[:, half * 512:(half + 1) * 512], lhsT=z[:, 127 - t:255 - t],
                                                  rhs=uv[:, 1024 + half * 512:1024 + (half + 1) * 512], start=(t == 0), stop=(t == 127)),
                         reads=[z, (uv, "v")], writes=[(po, half)])

            for s_i in range(128 + LAG + 1):
                if s_i < 128:
                    stage_a0(s_i)
                if 0 <= s_i - 1 < 128:
                    stage_a1(s_i - 1)
                if 0 <= s_i - 1 - LAG < 128:
                    stage_b(s_i - 1 - LAG)
            t_ = tm.next()
            S.op("dve", lambda e: e.tensor_tensor(out=t_[:], in0=po[:], in1=g2b[j][:], op=ALU.mult), reads=[po, g2b[j]], writes=[t_])
            S.op("dve", lambda e: e.scalar_tensor_tensor(out=t_[:], in0=x[:], scalar=ALPHA, in1=t_[:], op0=ALU.mult, op1=ALU.add),
                 reads=[x, t_], writes=[t_])
            t2 = tm.next(); s_ = sm.next(); o = xo.next()
            layer_norm(S, t_[:, :], [t_], 1024, lng, lnb, o, t2, s_)
            S.dma("sp", lambda e: e.dma_start(out=D[xout][r0:r0 + 128, :], in_=o[:]), reads=[o])


D_CONST = {}


def make_consts(S, D):
    mh = S.tile([128, 1], F32, "mhalf")
    S.op("pool", lambda e: e.memset(mh[:], -0.5), writes=[mh])
    D_CONST["mhalf"] = mh
    ib = S.tile([128, 128], BF16, "ident_bf")
    S.op("pool", lambda e: e.memset(ib[:], 0.0), writes=[ib])
    S.op("pool", lambda e: e.affine_select(out=ib[:], in_=ib[:], pattern=[[-1, 128]], compare_op=ALU.not_equal,
                                           fill=1.0, base=0, channel_multiplier=1), reads=[ib], writes=[ib])
    D["ident_bf"] = ib
    i32 = S.tile([128, 128], F32, "ident_f")
    S.op("pool", lambda e: e.memset(i32[:], 0.0), writes=[i32])
    S.op("pool", lambda e: e.affine_select(out=i32[:], in_=i32[:], pattern=[[-1, 128]], compare_op=ALU.not_equal,
                                           fill=1.0, base=0, channel_multiplier=1), reads=[i32], writes=[i32])
    D["ident_f"] = i32


INPUT_SPECS = {
    "xs": ([NTOK, 1024], F32),
    "cvecT": ([128, 8, 2], F32),
    "ada_w": ([2, 1024, 6144], F32),
    "ada_b": ([2, 6144], F32),
    "w_in": ([2, 1024, W_IN_COLS], F32),
    "w_branch": ([2, 1024, 1024], F32),
    "w_out": ([2, 1024, 1024], F32),
    "ln1_g": ([2, 1024], F32),
    "ln1_b": ([2, 1024], F32),
    "na_bias": ([2, 4, 8, 64, 512], F32),
    "ropeC": ([64, 64, 2, 8], F32),
    "ropeS": ([64, 64, 2, 8], F32),
    "gla_mt": ([64, 2, 64], F32),
    "gla_tri": ([64, 2, 64], F32),
    "gla_blk": ([128, 4, 64], F32),
    "gla_gw": ([2, 2, 33, 128], F32),
    "gla_norm_g": ([2, 256], F32),
    "peer_wq": ([2, 1024, 2048], F32),
    "peer_keysT": ([2, 16, 128, 128], F32),
    "peer_u0": ([16384, 1024], F32),
    "peer_u1": ([16384, 1024], F32),
    "peer_v0": ([16384, 1024], F32),
    "peer_v1": ([16384, 1024], F32),
    "ln2_g": ([2, 1024], F32),
    "ln2_b": ([2, 1024], F32),
    "iota_kk": ([128, 16, 16], F32),
    "conv_dwT": ([2, 256, 31], F32),
    "conv_bT": ([2, 128, 2], F32),
    "conv_ln_g": ([2, 256], F32),
    "conv_ln_b": ([2, 256], F32),
    "sgu_ln_g": ([2, 256], F32),
    "sgu_ln_b": ([2, 256], F32),
    "sgu_wsT": ([2, 4, 128, 128], F32),
    "sgu_bsT": ([2, 128, 4], F32),
}
SCRATCH_SPECS = {
    "modv": ([2, 2, 6144], F32),
    "z": ([NTOK, W_IN_COLS], F32),
    "ycat": ([NTOK, 1024], F32),
    "ofwd": ([NTOK, 256], F32),
    "pidx": ([NTOK, 128], I32),
    "pgt": ([NTOK, 128], F32),
    "xa": ([NTOK, 1024], F32),
    "peer_uvb0": ([16384, 2048], BF16),
    "peer_uvb1": ([16384, 2048], BF16),
    "out": ([SEQ, 1024], F32),
}


def build_program(plan, ext_in=(), ext_out=(), inputs=None):
    nc = bass.Bass("TRN2", target_bir_lowering=False)
    D = {}
    for name, (shape, dt) in INPUT_SPECS.items():
        if inputs is not None and name not in inputs:
            continue
        D[name] = nc.dram_tensor(name, shape, dt, kind="ExternalInput").ap()
    for name, (shape, dt) in SCRATCH_SPECS.items():
        kind = "ExternalInput" if name in ext_in else ("ExternalOutput" if name in ext_out else "Internal")
        D[name] = nc.dram_tensor(name, shape, dt, kind=kind).ap()
    with ExitStack() as st:
        S = Sync(nc, st)
        make_consts(S, D)
        plan(S, D)
        S.barrier()
    return nc


def na_bias_table(rpb):
    L = rpb.shape[0]
    W = 64
    col = np.arange(W)
    c0 = np.clip(col - 8, 0, W - 16)
    in_win = (col[None, :] >= c0[:, None]) & (col[None, :] < c0[:, None] + 16)
    dc = np.clip(col[None, :] - col[:, None], -15, 15) + 15
    out = np.empty((L, 4, 8, 64, 512), np.float32)
    reps = [0, 1, 2, 3, 30, 61, 62, 63]
    for ci, r in enumerate(reps):
        r0 = min(max(r - 4, 0), 56)
        for k in range(8):
            dr = r0 + k - r + 7
            b = rpb[:, :, dr][:, :, dc]
            out[:, :, ci, :, k * 64:(k + 1) * 64] = np.where(in_win[None, None], b, np.float32(-1e30))
    return out


def gla_consts():
    inv = (1.0 / (np.float32(100.0) ** (np.arange(8, dtype=np.float32) / np.float32(8)))).astype(np.float32)
    pos = np.arange(64, dtype=np.float32)
    ang = (pos[:, None] * inv[None, :]).astype(np.float32)
    C = np.cos(ang).astype(np.float32)
    Sn = np.sin(ang).astype(np.float32)
    ropeC = np.empty((64, 64, 2, 8), np.float32)
    ropeS = np.empty((64, 64, 2, 8), np.float32)
    ropeC[:, :, 0, :] = C[None, :, :]
    ropeS[:, :, 0, :] = Sn[None, :, :]
    ropeC[:, :, 1, :] = C[:, None, :]
    ropeS[:, :, 1, :] = Sn[:, None, :]
    s = np.arange(64)[:, None]
    c = np.arange(64)[None, :]
    tri = np.stack([(s <= c), (s >= c)], 1).astype(np.float32)
    mt = (tri * np.float32(-1.0 / 16)).astype(np.float32)
    blk = np.zeros((128, 4, 64), np.float32)
    for h in range(4):
        blk[h * 32:(h + 1) * 32, h, :] = 1
    return dict(ropeC=ropeC, ropeS=ropeS, gla_mt=mt, gla_tri=tri, gla_blk=blk)


def gla_gw_layout(gate_up, gate_b):
    L = gate_up.shape[0]
    out = np.zeros((L, 2, 33, 128), np.float32)
    for d in range(2):
        out[:, d, d * 16:(d + 1) * 16, :] = gate_up[:, d]
        out[:, d, 32, :] = gate_b[:, d]
    return out


def host_weights(inp):
    f = lambda a: np.ascontiguousarray(np.asarray(a, dtype=np.float32))
    W = {}
    for l in range(2):
        W["peer_u%d" % l] = f(np.asarray(inp["peer_u"])[l])
        W["peer_v%d" % l] = f(np.asarray(inp["peer_v"])[l])
    for k in ("ada_w", "ada_b", "w_in", "w_out", "ln1_g", "ln1_b", "peer_wq", "ln2_g", "ln2_b",
              "conv_ln_g", "conv_ln_b", "sgu_ln_g", "sgu_ln_b"):
        W[k] = f(inp[k])
    W["w_branch"] = f(np.asarray(inp["w_branch"]).reshape(2, 1024, 1024))
    W["na_bias"] = na_bias_table(f(inp["na_rpb"]))
    W.update(gla_consts())
    W["gla_gw"] = gla_gw_layout(f(inp["gla_gate_up"]), f(inp["gla_gate_b"]))
    W["gla_norm_g"] = f(np.asarray(inp["gla_norm_g"]).reshape(2, 256))
    W["conv_dwT"] = f(np.asarray(inp["conv_dw"]).transpose(0, 2, 1))
    W["conv_bT"] = f(np.asarray(inp["conv_b"]).reshape(2, 2, 128).transpose(0, 2, 1))
    W["sgu_wsT"] = f(np.asarray(inp["sgu_ws"]).transpose(0, 1, 3, 2))
    W["sgu_bsT"] = f(np.asarray(inp["sgu_bs"]).transpose(0, 2, 1))
    W["peer_keysT"] = f(np.asarray(inp["peer_keys"]).reshape(2, 16, 128, 128).transpose(0, 1, 3, 2))
    W["iota_kk"] = f(np.broadcast_to(np.arange(16, dtype=np.float32)[None, None, :], (128, 16, 16)))
    return W


def full_plan(S, D):
    cv = [start_table_convert(S, D, l) for l in range(DEPTH)]
    for l in range(DEPTH):
        need_ctx = l < DEPTH - 1
        tl = range(NTT) if need_ctx else range(32)
        xin = "xs" if l == 0 else "xa"
        phase_mod(S, D, l)
        phase_win(S, D, l, xin=xin)
        phase_na(S, D, l, need_ctx=need_ctx)
        phase_gla(S, D, l, need_ctx=need_ctx)
        phase_conv(S, D, l, do_ctx=need_ctx)
        phase_sgu(S, D, l, tiles=tl)
        phase_merge(S, D, l, tiles=tl, xin=xin, xout="xa")
        phase_peer_topk(S, D, l, tiles=tl, xin="xa")
        phase_peer_ffn(S, D, l, tiles=tl, xin="xa", xout=("xa" if need_ctx else "out"), conv_vals=cv[l])


_CACHE = {}


def kernel(**inputs):
    x = np.asarray(inputs["x"], dtype=np.float32)
    c = np.asarray(inputs["c"], dtype=np.float32)
    ctx = np.asarray(inputs["ctx"], dtype=np.float32)
    c_ctx = np.asarray(inputs["c_ctx"], dtype=np.float32)
    B = x.shape[0]
    W = host_weights(inputs)
    if "nc" not in _CACHE:
        _CACHE["nc"] = build_program(full_plan, ext_out=("out",))
    nc = _CACHE["nc"]
    in_maps = []
    for b in range(B):
        m = dict(W)
        m["xs"] = np.ascontiguousarray(np.concatenate([x[b], ctx[b]], 0))
        cvec = np.stack([c[b], c_ctx], 0)
        m["cvecT"] = np.ascontiguousarray(cvec.reshape(2, 8, 128).transpose(2, 1, 0))
        in_maps.append(m)
    res = run_bass_kernel_spmd(nc, in_maps, core_ids=list(range(B)))
    return np.stack([np.asarray(r["out"], dtype=np.float32) for r in res.results], 0)
```

```python
import numpy as np
from contextlib import ExitStack, contextmanager
import concourse.bass as bass
import concourse.mybir as mybir
from concourse.bass_utils import run_bass_kernel_spmd

F32 = mybir.dt.float32
BF16 = mybir.dt.bfloat16
I32 = mybir.dt.int32
U32 = mybir.dt.uint32
AF = mybir.ActivationFunctionType
ALU = mybir.AluOpType
AX = mybir.AxisListType

D_MODEL = 1024
SEQ = 4096
CTX = 256
NTOK = SEQ + CTX
NTT = NTOK // 128
DEPTH = 2
W_IN_COLS = 6688
ALPHA = (2 * DEPTH) ** 0.25
LN_EPS = 1e-6
C_QKV, C_GQ, C_GK, C_GV, C_GR, C_GLO, C_CONV, C_SGU, C_GATE = 0, 768, 896, 1024, 1280, 1536, 1568, 2080, 2592


class Buf:
    def __init__(self, h):
        self.h = h
        self.st = {}

    def __getitem__(self, idx):
        return self.h[idx]


class Sync:
    NDMA = 8

    def __init__(self, nc, stack):
        self.nc = nc
        self.stack = stack
        self.E = {}
        self.semobj = {}
        for name, eng in (("pe", nc.tensor), ("act", nc.scalar), ("dve", nc.vector),
                          ("pool", nc.gpsimd), ("sp", nc.sync)):
            sem = stack.enter_context(nc.semaphore("s_" + name))
            self.E[name] = dict(eng=eng, sem=sem, cnt=0, seen={}, dq=[], dn=0)
            self.semobj[id(sem)] = sem
        for q in ("sp", "pool", "act"):
            e = self.E[q]
            e["dq"] = [stack.enter_context(nc.semaphore("d_%s%d" % (q, i))) for i in range(self.NDMA)]
            for s in e["dq"]:
                self.semobj[id(s)] = s
        self.bg = [stack.enter_context(nc.semaphore("bg%d" % i)) for i in range(4)]
        for b in self.bg:
            self.semobj[id(b)] = b
        self.pending_dma = {}
        self.cur = stack
        self.uid = 0

    def tile(self, shape, dt, name=None):
        self.uid += 1
        return Buf(self.cur.enter_context(self.nc.sbuf_tensor("%s_%d" % (name or "t", self.uid), list(shape), dt)))

    def psum(self, shape, dt=F32, name=None):
        self.uid += 1
        return Buf(self.cur.enter_context(self.nc.psum_tensor("%s_%d" % (name or "p", self.uid), list(shape), dt)))

    @contextmanager
    def phase(self):
        prev = self.cur
        with ExitStack() as st:
            self.cur = st
            yield
            self.barrier()
        self.cur = prev

    @staticmethod
    def _merge(out, d):
        for k, v in d.items():
            if out.get(k, 0) < v:
                out[k] = v

    def _deps(self, reads, writes):
        out = {}
        for b, key in reads:
            keys = list(b.st.keys()) if key is None else [key, None]
            for k in keys:
                st = b.st.get(k)
                if st:
                    self._merge(out, st[0])
        for b, key in writes:
            keys = list(b.st.keys()) if key is None else [key, None]
            for k in keys:
                st = b.st.get(k)
                if st:
                    self._merge(out, st[0])
                    self._merge(out, st[1])
        return out

    def _wait(self, ename, deps):
        e = self.E[ename]
        own = id(e["sem"])
        for sid, val in deps.items():
            if ename == "pe" and sid == own:
                continue
            if e["seen"].get(sid, 0) >= val:
                continue
            e["eng"].wait_ge(self.semobj[sid], val)
            e["seen"][sid] = val

    def _mark(self, reads, writes, sid, val):
        for b, key in reads:
            st = b.st.setdefault(key, [{}, {}])
            if st[1].get(sid, 0) < val:
                st[1][sid] = val
        for b, key in writes:
            if key is None:
                b.st = {None: [{sid: val}, {}]}
            else:
                b.st[key] = [{sid: val}, {}]

    @staticmethod
    def _norm(lst):
        return [(x, None) if isinstance(x, Buf) else x for x in lst]

    def op(self, ename, fn, reads=(), writes=()):
        reads = self._norm(reads)
        writes = self._norm(writes)
        e = self.E[ename]
        self._wait(ename, self._deps(reads, writes))
        ins = fn(e["eng"])
        e["cnt"] += 1
        ins.then_inc(e["sem"], 1)
        self._mark(reads, writes, id(e["sem"]), e["cnt"])
        return ins

    def dma(self, qname, fn, reads=(), writes=()):
        reads = self._norm(reads)
        writes = self._norm(writes)
        e = self.E[qname]
        slot = e["dn"] % self.NDMA
        val = (e["dn"] // self.NDMA + 1) * 16
        sem = e["dq"][slot]
        deps = self._deps(reads, writes)
        if val > 16:
            self._merge(deps, {id(sem): val - 16})
        self._wait(qname, deps)
        ins = fn(e["eng"])
        ins.then_inc(sem, 16)
        e["dn"] += 1
        self._mark(reads, writes, id(sem), val)
        self._merge(self.pending_dma, {id(sem): val})
        return ins

    def bg_dma(self, qname, fn, sem, n_prev):
        e = self.E[qname]
        ins = fn(e["eng"])
        ins.then_inc(sem, 16)
        return (n_prev + 1) * 16

    def wait_sem(self, enames, sem, val):
        for n in enames:
            self._wait(n, {id(sem): val})

    def barrier(self):
        allv = dict(self.pending_dma)
        for n, e in self.E.items():
            if e["cnt"]:
                allv[id(e["sem"])] = e["cnt"]
        for n in self.E:
            self._wait(n, allv)

    def load(self, t, src, q="sp", key=None):
        return self.dma(q, lambda e: e.dma_start(out=t, in_=src), writes=[(self._b, key)] if False else [])


class Rot:
    def __init__(self, S, n, shape, dt, psum=False, name=None):
        self.bufs = [(S.psum(shape, dt, name) if psum else S.tile(shape, dt, name)) for _ in range(n)]
        self.i = 0

    def next(self):
        b = self.bufs[self.i % len(self.bufs)]
        self.i += 1
        return b


def phase_mod(S, D, l):
    with S.phase():
        cT = S.tile([128, 8, 2], F32, "cT")
        cs = S.tile([128, 8, 2], F32, "cs")
        ones = S.tile([1, 2], F32, "ones")
        ab = S.tile([1, 6144], F32, "ab")
        wa = Rot(S, 4, [128, 8, 512], F32, name="wa")
        pm = Rot(S, 4, [2, 512], F32, psum=True, name="pm")
        mr = Rot(S, 4, [2, 512], F32, name="mr")
        S.dma("sp", lambda e: e.dma_start(out=cT[:], in_=D["cvecT"]), writes=[cT])
        S.dma("sp", lambda e: e.dma_start(out=ab[:], in_=D["ada_b"][l:l + 1, :]), writes=[ab])
        S.op("dve", lambda e: e.memset(ones[:], 1.0), writes=[ones])
        S.op("act", lambda e: e.activation(out=cs[:], in_=cT[:], func=AF.Silu), reads=[cT], writes=[cs])
        aw = D["ada_w"][l].rearrange("(k p) n -> p k n", p=128)
        wl = {}

        def ld(n):
            w_ = wa.next()
            S.dma("sp", lambda e: e.dma_start(out=w_[:], in_=aw[:, :, n * 512:(n + 1) * 512]), writes=[w_])
            wl[n] = w_

        for n in range(3):
            ld(n)
        for n in range(12):
            if n + 3 < 12:
                ld(n + 3)
            w = wl.pop(n)
            p = pm.next()
            for k in range(8):
                S.op("pe", lambda e: e.matmul(p[:], lhsT=cs[:, k, :], rhs=w[:, k, :], start=(k == 0), stop=False),
                     reads=[cs, w], writes=[p])
            S.op("pe", lambda e: e.matmul(p[:], lhsT=ones[:], rhs=ab[:, n * 512:(n + 1) * 512], start=False, stop=True),
                 reads=[ones, ab], writes=[p])
            m = mr.next()
            plus1 = 1.0 if n in (2, 3, 8, 9) else 0.0
            S.op("dve", lambda e: e.tensor_scalar(out=m[:], in0=p[:], scalar1=plus1, scalar2=None, op0=ALU.add),
                 reads=[p], writes=[m])
            S.dma("sp", lambda e: e.dma_start(out=D["modv"][l, :, n * 512:(n + 1) * 512], in_=m[:]), reads=[m])


def bcast_load(S, D, l, j, c0, n, name):
    t = S.tile([128, n], F32, name)
    S.dma("sp", lambda e: e.dma_start(out=t[:], in_=D["modv"][l, j, c0:c0 + n].partition_broadcast(128)), writes=[t])
    return t


def vec_bcast(S, src, n, name):
    t = S.tile([128, n], F32, name)
    S.dma("sp", lambda e: e.dma_start(out=t[:], in_=src.partition_broadcast(128)), writes=[t])
    return t


def phase_win(S, D, l, tiles=range(NTT), xin="xs", post_weights=None):
    with S.phase():
        wb = S.tile([128, 8, W_IN_COLS], BF16, "wb")
        wv = D["w_in"][l].rearrange("(k p) n -> p k n", p=128)
        for k in range(8):
            S.dma("pool", lambda e: e.dma_start(out=wb[:, k, :], in_=wv[:, k, :]), writes=[(wb, k)])
        if post_weights is not None:
            post_weights()
        scb = [bcast_load(S, D, l, j, 1024, 1024, "scb") for j in range(2)]
        shb = [bcast_load(S, D, l, j, 0, 1024, "shb") for j in range(2)]
        xt = Rot(S, 3, [128, 1024], F32, name="xt")
        hf = Rot(S, 2, [128, 1024], F32, name="hf")
        hb = Rot(S, 2, [128, 1024], BF16, name="hb")
        hT = Rot(S, 3, [128, 8, 128], BF16, name="hT")
        pT = Rot(S, 2, [128, 8, 128], BF16, psum=True, name="pT")
        pz = Rot(S, 4, [128, 512], F32, psum=True, name="pz")
        zs = Rot(S, 3, [128, 2048], F32, name="zs")
        ident = D["ident_bf"]
        cnt = 0
        def st_l(tt):
            j = 0 if tt < 32 else 1
            x = xt.next()
            S.dma("sp", lambda e: e.dma_start(out=x[:], in_=D[xin][tt * 128:(tt + 1) * 128, :]), writes=[x])
            return dict(tt=tt, j=j, x=x)

        def st_a(c_):
            tt, j, x = c_["tt"], c_["j"], c_["x"]
            h1 = hf.next()
            S.op("dve", lambda e: e.tensor_tensor(out=h1[:], in0=x[:], in1=scb[j][:], op=ALU.mult), reads=[x, scb[j]], writes=[h1])
            h2 = hb.next()
            S.op("dve", lambda e: e.tensor_tensor(out=h2[:], in0=h1[:], in1=shb[j][:], op=ALU.add), reads=[h1, shb[j]], writes=[h2])
            p = pT.next()
            for k in range(8):
                S.op("pe", lambda e: e.transpose(out=p[:, k, :], in_=h2[:, k * 128:(k + 1) * 128], identity=ident[:]),
                     reads=[h2, ident], writes=[(p, k)])
            t = hT.next()
            S.op("act", lambda e: e.copy(out=t[:], in_=p[:]), reads=[p], writes=[t])
            c_.update(t=t)

        def st_b(c_):
            nonlocal cnt
            tt, t = c_["tt"], c_["t"]
            for g0 in range(0, W_IN_COLS, 2048):
                gw = min(2048, W_IN_COLS - g0)
                zt = zs.next()
                for c0 in range(g0, g0 + gw, 512):
                    cw = min(512, W_IN_COLS - c0)
                    pp = pz.next()
                    for k in range(8):
                        S.op("pe", lambda e: e.matmul(pp[:, :cw], lhsT=t[:, k, :], rhs=wb[:, k, c0:c0 + cw],
                                                      start=(k == 0), stop=(k == 7)), reads=[t, wb], writes=[pp])
                    eng = "act" if cnt % 2 == 0 else "dve"
                    cnt += 1
                    if eng == "act":
                        S.op("act", lambda e: e.copy(out=zt[:, c0 - g0:c0 - g0 + cw], in_=pp[:, :cw]), reads=[pp], writes=[(zt, c0)])
                    else:
                        S.op("dve", lambda e: e.tensor_copy(out=zt[:, c0 - g0:c0 - g0 + cw], in_=pp[:, :cw]), reads=[pp], writes=[(zt, c0)])
                S.dma("sp", lambda e: e.dma_start(out=D["z"][tt * 128:(tt + 1) * 128, g0:g0 + gw], in_=zt[:, :gw]), reads=[zt])

        tiles = list(tiles)
        st_ = {}
        nj = len(tiles)
        for i in range(nj + 2):
            if i < nj:
                st_[i] = st_l(tiles[i])
            if 0 <= i - 1 < nj:
                st_a(st_[i - 1])
            if 0 <= i - 2 < nj:
                st_b(st_.pop(i - 2))


def layer_norm(S, xin, xdeps, n, gb, bb, out, tmp, small):
    nch = (n + 511) // 512
    for i in range(nch):
        w = min(512, n - i * 512)
        S.op("dve", lambda e: e.bn_stats(out=small[:, 8 + 6 * i:8 + 6 * i + 6], in_=xin[:, i * 512:i * 512 + w]),
             reads=xdeps, writes=[(small, "st%d" % i)])
    S.op("dve", lambda e: e.bn_aggr(out=small[:, 0:2], in_=small[:, 8:8 + 6 * nch]), reads=[small], writes=[(small, "mv")])
    mh = D_CONST["mhalf"]
    S.op("pool", lambda e: e.tensor_scalar(out=small[:, 2:3], in0=small[:, 1:2], scalar1=LN_EPS, scalar2=None, op0=ALU.add),
         reads=[(small, "mv")], writes=[(small, "ve")])
    S.op("pool", lambda e: e.tensor_tensor(out=small[:, 4:5], in0=small[:, 2:3], in1=mh[:, 0:1], op=ALU.pow),
         reads=[(small, "ve"), mh], writes=[(small, "rs")])
    S.op("dve", lambda e: e.tensor_scalar(out=tmp[:, :n], in0=xin, scalar1=small[:, 0:1], scalar2=small[:, 4:5],
                                          op0=ALU.subtract, op1=ALU.mult), reads=list(xdeps) + [small], writes=[tmp])
    S.op("pool", lambda e: e.tensor_tensor(out=tmp[:, :n], in0=tmp[:, :n], in1=gb[:, :n], op=ALU.mult), reads=[tmp, gb], writes=[tmp])
    S.op("pool", lambda e: e.tensor_tensor(out=out[:, :n], in0=tmp[:, :n], in1=bb[:, :n], op=ALU.add), reads=[tmp, bb], writes=[out])


def phase_conv(S, D, l, do_ctx=True):
    PAD = 15
    with S.phase():
        dw = S.tile([128, 2, 31], F32, "dw")
        cb = S.tile([128, 2], F32, "cb")
        S.dma("sp", lambda e: e.dma_start(out=dw[:], in_=D["conv_dwT"][l].rearrange("(c p) j -> p c j", p=128)), writes=[dw])
        S.dma("sp", lambda e: e.dma_start(out=cb[:], in_=D["conv_bT"][l]), writes=[cb])
        lng = vec_bcast(S, D["conv_ln_g"][l], 256, "lng")
        lnb = vec_bcast(S, D["conv_ln_b"][l], 256, "lnb")
        identf = D["ident_f"]
        yT = S.tile([128, 2, PAD + SEQ + PAD], F32, "yT")
        cv = S.tile([128, 2, SEQ], F32, "cv")
        zt = Rot(S, 4, [128, 512], F32, name="zt")
        sg = Rot(S, 4, [128, 256], F32, name="sg")
        yy = Rot(S, 4, [128, 256], F32, name="yy")
        pcs = Rot(S, 4, [128, 256], F32, name="pcs")
        pt = Rot(S, 2, [128, 2, 128], F32, psum=True, name="pt")
        pb = Rot(S, 2, [128, 256], F32, psum=True, name="pb")
        tmp = Rot(S, 2, [128, 256], F32, name="tmp")
        sm = Rot(S, 2, [128, 32], F32, name="sm")
        ln = Rot(S, 2, [128, 256], F32, name="ln")
        yo = Rot(S, 2, [128, 256], F32, name="yo")
        for (base, T) in ((0, SEQ), (SEQ, CTX)) if do_ctx else ((0, SEQ),):
            S.op("pool", lambda e: e.memset(yT[:, :, 0:PAD], 0.0), writes=[(yT, "padl")])
            S.op("pool", lambda e: e.memset(yT[:, :, PAD + T:PAD + T + PAD], 0.0), writes=[(yT, "padr")])
            ys_ = {}

            def a_front(i):
                r0 = base + i * 128
                z = zt.next()
                S.dma("sp", lambda e: e.dma_start(out=z[:], in_=D["z"][r0:r0 + 128, C_CONV:C_CONV + 512]), writes=[z])
                s = sg.next()
                S.op("act", lambda e: e.activation(out=s[:], in_=z[:, 256:512], func=AF.Sigmoid), reads=[z], writes=[s])
                y = yy.next()
                S.op("dve", lambda e: e.tensor_tensor(out=y[:], in0=z[:, 0:256], in1=s[:], op=ALU.mult), reads=[z, s], writes=[y])
                ys_[i] = y

            NT_ = T // 128
            for i in range(min(2, NT_)):
                a_front(i)
            for i in range(NT_):
                if i + 2 < NT_:
                    a_front(i + 2)
                y = ys_.pop(i)
                p = pt.next()
                for c in range(2):
                    S.op("pe", lambda e: e.transpose(out=p[:, c, :], in_=y[:, c * 128:(c + 1) * 128], identity=identf[:]),
                         reads=[y, identf], writes=[(p, c)])
                S.op("act", lambda e: e.copy(out=yT[:, :, PAD + i * 128:PAD + (i + 1) * 128], in_=p[:]), reads=[p], writes=[(yT, i)])
            CH = 1024 if T >= 1024 else T
            for t0 in range(0, T, CH):
                for c in range(2):
                    key = ("cv", t0, c)
                    S.op("dve", lambda e: e.tensor_scalar(out=cv[:, c, t0:t0 + CH], in0=yT[:, c, t0:t0 + CH], scalar1=dw[:, c, 0:1],
                                                          scalar2=cb[:, c:c + 1], op0=ALU.mult, op1=ALU.add),
                         reads=[yT, dw, cb], writes=[(cv, key)])
                    for j in range(1, 31):
                        S.op("dve", lambda e: e.scalar_tensor_tensor(out=cv[:, c, t0:t0 + CH], in0=yT[:, c, t0 + j:t0 + j + CH],
                                                                     scalar=dw[:, c, j:j + 1], in1=cv[:, c, t0:t0 + CH],
                                                                     op0=ALU.mult, op1=ALU.add),
                             reads=[yT, dw, (cv, key)], writes=[(cv, key)])
            ps_ = {}

            def c_front(i):
                p_ = pb.next()
                for c in range(2):
                    S.op("pe", lambda e: e.transpose(out=p_[:, c * 128:(c + 1) * 128], in_=cv[:, c, i * 128:(i + 1) * 128], identity=identf[:]),
                         reads=[cv, identf], writes=[(p_, c)])
                pc_ = pcs.next()
                S.op("act", lambda e: e.copy(out=pc_[:], in_=p_[:]), reads=[p_], writes=[pc_])
                ps_[i] = pc_

            for i in range(min(2, NT_)):
                c_front(i)
            for i in range(NT_):
                if i + 2 < NT_:
                    c_front(i + 2)
                r0 = base + i * 128
                p = ps_.pop(i)
                t = tmp.next(); s = sm.next(); o = ln.next()
                layer_norm(S, p[:, :], [p], 256, lng, lnb, o, t, s)
                y = yo.next()
                S.op("act", lambda e: e.activation(out=y[:], in_=o[:], func=AF.Silu), reads=[o], writes=[y])
                S.dma("sp", lambda e: e.dma_start(out=D["ycat"][r0:r0 + 128, 512:768], in_=y[:]), reads=[y])
            S.barrier()


def phase_sgu(S, D, l, tiles=range(NTT)):
    with S.phase():
        ws = S.tile([128, 4, 128], F32, "ws")
        bs = S.tile([128, 4], F32, "bs")
        S.dma("sp", lambda e: e.dma_start(out=ws[:], in_=D["sgu_wsT"][l].rearrange("g q p -> q g p")), writes=[ws])
        S.dma("sp", lambda e: e.dma_start(out=bs[:], in_=D["sgu_bsT"][l]), writes=[bs])
        lng = vec_bcast(S, D["sgu_ln_g"][l], 256, "lng")
        lnb = vec_bcast(S, D["sgu_ln_b"][l], 256, "lnb")
        zt = Rot(S, 4, [128, 512], F32, name="zt")
        ge = Rot(S, 4, [128, 512], F32, name="ge")
        tmp = Rot(S, 2, [128, 256], F32, name="tmp")
        sm = Rot(S, 2, [128, 32], F32, name="sm")
        vn = Rot(S, 2, [128, 256], F32, name="vn")
        ps = Rot(S, 2, [128, 256], F32, psum=True, name="ps")
        sb = Rot(S, 2, [128, 256], F32, name="sb")
        yo = Rot(S, 2, [128, 256], F32, name="yo")
        def st_a(tt):
            r0 = tt * 128
            z = zt.next()
            S.dma("sp", lambda e: e.dma_start(out=z[:], in_=D["z"][r0:r0 + 128, C_SGU:C_SGU + 512]), writes=[z])
            g = ge.next()
            S.op("act", lambda e: e.activation(out=g[:], in_=z[:], func=AF.Gelu_apprx_tanh), reads=[z], writes=[g])
            return (r0, g)

        tiles = list(tiles)
        pend = {}
        for i in range(len(tiles) + 2):
            if i < len(tiles):
                pend[i] = st_a(tiles[i])
            if i - 2 < 0:
                continue
            r0, g = pend.pop(i - 2)
            t = tmp.next(); s = sm.next(); v = vn.next()
            layer_norm(S, g[:, 256:512], [g], 256, lng, lnb, v, t, s)
            p = ps.next()
            for gi in range(4):
                S.op("pe", lambda e: e.matmul(p[:, gi * 64:(gi + 1) * 64], lhsT=ws[:, gi, :], rhs=v[:, gi * 64:(gi + 1) * 64],
                                              start=True, stop=True), reads=[ws, v], writes=[(p, gi)])
            sbt = sb.next()
            S.op("dve", lambda e: e.tensor_tensor(out=sbt[:].rearrange("p (g c) -> p g c", g=4), in0=p[:].rearrange("p (g c) -> p g c", g=4),
                                                  in1=bs[:].unsqueeze(2).to_broadcast([128, 4, 64]), op=ALU.add),
                 reads=[p, bs], writes=[sbt])
            y = yo.next()
            S.op("dve", lambda e: e.tensor_tensor(out=y[:], in0=sbt[:], in1=g[:, 0:256], op=ALU.mult), reads=[sbt, g], writes=[y])
            S.dma("sp", lambda e: e.dma_start(out=D["ycat"][r0:r0 + 128, 768:1024], in_=y[:]), reads=[y])


def phase_merge(S, D, l, tiles=range(NTT), xin="xs", xout="xs"):
    with S.phase():
        wbr = S.tile([128, 8, 1024], BF16, "wbr")
        wo = S.tile([128, 8, 1024], BF16, "wo")
        S.dma("pool", lambda e: e.dma_start(out=wbr[:], in_=D["w_branch"][l].rearrange("(k p) n -> p k n", p=128)), writes=[wbr])
        S.dma("pool", lambda e: e.dma_start(out=wo[:], in_=D["w_out"][l].rearrange("(k p) n -> p k n", p=128)), writes=[wo])
        g1b = [bcast_load(S, D, l, j, 2048, 1024, "g1b") for j in range(2)]
        lng = vec_bcast(S, D["ln1_g"][l], 1024, "lng")
        lnb = vec_bcast(S, D["ln1_b"][l], 1024, "lnb")
        ident = D["ident_bf"]
        yt = Rot(S, 3, [128, 1024], F32, name="yt")
        yb = Rot(S, 2, [128, 1024], BF16, name="yb")
        yT = Rot(S, 2, [128, 8, 128], BF16, name="yT")
        gt = Rot(S, 3, [128, 4096], F32, name="gt")
        sg = Rot(S, 2, [128, 1024], F32, name="sg")
        tm = Rot(S, 2, [128, 1024], F32, name="tm")
        mg = Rot(S, 1, [128, 1024], F32, name="mg")
        mb = Rot(S, 3, [128, 1024], BF16, name="mb")
        mT = Rot(S, 2, [128, 8, 128], BF16, name="mT")
        xt = Rot(S, 4, [128, 1024], F32, name="xt")
        rr = Rot(S, 2, [128, 1024], F32, name="rr")
        sm = Rot(S, 2, [128, 32], F32, name="sm")
        xo = Rot(S, 2, [128, 1024], F32, name="xo")
        pT = Rot(S, 2, [128, 8, 128], BF16, psum=True, name="pT")
        pP = Rot(S, 2, [128, 1024], F32, psum=True, name="pP")
        pO = Rot(S, 1, [128, 1024], F32, psum=True, name="pO")
        def st_a(tt):
            j = 0 if tt < 32 else 1
            r0 = tt * 128
            y = yt.next()
            S.dma("sp", lambda e: e.dma_start(out=y[:], in_=D["ycat"][r0:r0 + 128, :]), writes=[y])
            g = gt.next()
            S.dma("sp", lambda e: e.dma_start(out=g[:], in_=D["z"][r0:r0 + 128, C_GATE:C_GATE + 4096]), writes=[g])
            x = xt.next()
            S.dma("sp", lambda e: e.dma_start(out=x[:], in_=D[xin][r0:r0 + 128, :]), writes=[x])
            return dict(j=j, r0=r0, y=y, g=g, x=x)

        def st_b(c_):
            j, r0, y, g, x = (c_[k] for k in ("j", "r0", "y", "g", "x"))
            b = yb.next()
            S.op("dve", lambda e: e.tensor_copy(out=b[:], in_=y[:]), reads=[y], writes=[b])
            p = pT.next()
            for k in range(8):
                S.op("pe", lambda e: e.transpose(out=p[:, k, :], in_=b[:, k * 128:(k + 1) * 128], identity=ident[:]),
                     reads=[b, ident], writes=[(p, k)])
            t = yT.next()
            S.op("act", lambda e: e.copy(out=t[:], in_=p[:]), reads=[p], writes=[t])
            m = mg.next()
            mbt = mb.next()
            for i in range(4):
                pp = pP.next()
                for half in range(2):
                    for kk in range(2):
                        S.op("pe", lambda e: e.matmul(pp[:, half * 512:(half + 1) * 512], lhsT=t[:, i * 2 + kk, :],
                                                      rhs=wbr[:, i * 2 + kk, half * 512:(half + 1) * 512], start=(kk == 0), stop=(kk == 1)),
                             reads=[t, wbr], writes=[(pp, half)])
                s = sg.next()
                S.op("act", lambda e: e.activation(out=s[:], in_=g[:, i * 1024:(i + 1) * 1024], func=AF.Sigmoid), reads=[g], writes=[s])
                if i == 0:
                    S.op("dve", lambda e: e.tensor_tensor(out=m[:], in0=pp[:], in1=s[:], op=ALU.mult), reads=[pp, s], writes=[m])
                else:
                    tmp = tm.next()
                    S.op("dve", lambda e: e.tensor_tensor(out=tmp[:], in0=pp[:], in1=s[:], op=ALU.mult), reads=[pp, s], writes=[tmp])
                    dst = mbt if i == 3 else m
                    S.op("pool", lambda e: e.tensor_tensor(out=dst[:], in0=m[:], in1=tmp[:], op=ALU.add), reads=[m, tmp], writes=[dst])
            c_.update(mbt=mbt)

        def st_c(c_):
            j, r0, x, mbt = (c_[k] for k in ("j", "r0", "x", "mbt"))
            p = pT.next()
            for k in range(8):
                S.op("pe", lambda e: e.transpose(out=p[:, k, :], in_=mbt[:, k * 128:(k + 1) * 128], identity=ident[:]),
                     reads=[mbt, ident], writes=[(p, k)])
            t2 = mT.next()
            S.op("act", lambda e: e.copy(out=t2[:], in_=p[:]), reads=[p], writes=[t2])
            po = pO.next()
            for half in range(2):
                for k in range(8):
                    S.op("pe", lambda e: e.matmul(po[:, half * 512:(half + 1) * 512], lhsT=t2[:, k, :],
                                                  rhs=wo[:, k, half * 512:(half + 1) * 512], start=(k == 0), stop=(k == 7)),
                         reads=[t2, wo], writes=[(po, half)])
            r = rr.next()
            S.op("dve", lambda e: e.tensor_tensor(out=r[:], in0=po[:], in1=g1b[j][:], op=ALU.mult), reads=[po, g1b[j]], writes=[r])
            S.op("dve", lambda e: e.scalar_tensor_tensor(out=r[:], in0=x[:], scalar=ALPHA, in1=r[:], op0=ALU.mult, op1=ALU.add),
                 reads=[x, r], writes=[r])
            tmp = tm.next(); s = sm.next(); o = xo.next()
            layer_norm(S, r[:, :], [r], 1024, lng, lnb, o, tmp, s)
            S.dma("sp", lambda e: e.dma_start(out=D[xout][r0:r0 + 128, :], in_=o[:]), reads=[o])

        tiles = list(tiles)
        st_ = {}
        nj = len(tiles)
        for i in range(nj + 2):
            if i < nj:
                st_[i] = st_a(tiles[i])
            if 0 <= i - 1 < nj:
                st_b(st_[i - 1])
            if 0 <= i - 2 < nj:
                st_c(st_.pop(i - 2))


def phase_na(S, D, l, need_ctx=True, rows=range(64), heads=range(4)):
    scale = 64 ** -0.5
    with S.phase():
        ident = D["ident_bf"]
        qkT = S.tile([128, 4, NTOK], BF16, "qkT")
        va = S.tile([128, 32, 256], BF16, "va")
        vb = S.tile([128, 31, 256], BF16, "vb")
        vc = S.tile([128, 2, 256], BF16, "vc")
        zv = D["z"]
        S.dma("pool", lambda e: e.dma_start(out=va[:], in_=zv[0:4096, 512:768].rearrange("(t p) c -> p t c", p=128)), writes=[va])
        S.dma("pool", lambda e: e.dma_start(out=vb[:], in_=zv[64:64 + 31 * 128, 512:768].rearrange("(t p) c -> p t c", p=128)), writes=[vb])
        S.dma("pool", lambda e: e.dma_start(out=vc[:], in_=zv[4096:4352, 512:768].rearrange("(t p) c -> p t c", p=128)), writes=[vc])
        with S.phase():
            qk = Rot(S, 3, [128, 512], BF16, name="qk")
            pq = Rot(S, 2, [128, 4, 128], BF16, psum=True, name="pq")
            for tt in range(NTT):
                t = qk.next()
                S.dma("pool", lambda e: e.dma_start(out=t[:], in_=zv[tt * 128:(tt + 1) * 128, 0:512]), writes=[t])
                p = pq.next()
                for c in range(4):
                    S.op("pe", lambda e: e.transpose(out=p[:, c, :], in_=t[:, c * 128:(c + 1) * 128], identity=ident[:]),
                         reads=[t, ident], writes=[(p, c)])
                S.op("act", lambda e: e.copy(out=qkT[:, :, tt * 128:(tt + 1) * 128], in_=p[:]), reads=[p], writes=[(qkT, tt)])
        bias = Rot(S, 4, [64, 8, 512], F32, name="bias")
        ps1 = Rot(S, 2, [128, 512], F32, psum=True, name="ps1")
        ps2 = Rot(S, 2, [128, 256], F32, psum=True, name="ps2")
        ppT = Rot(S, 2, [128, 6, 128], BF16, psum=True, name="ppT")
        po = Rot(S, 2, [128, 64], F32, psum=True, name="po")
        sc = Rot(S, 3, [128, 768], F32, name="sc")
        pe_ = Rot(S, 3, [128, 768], BF16, name="pexp")
        pT = Rot(S, 3, [128, 6, 128], BF16, name="pT")
        sm = Rot(S, 6, [128, 4], F32, name="sm")
        yo = Rot(S, 4, [128, 64], F32, name="yo")

        def att_s1(h, M, qcols, kwin, cls, vchunks, bt, out_ap):
            hp, pb = h // 2, (h % 2) * 64
            qTs = qkT[pb:pb + 64, hp, qcols[0]:qcols[1]]
            s = sc.next(); nk = 0
            if kwin is not None:
                p1 = ps1.next()
                S.op("pe", lambda e: e.matmul(p1[:M, :], lhsT=qTs, rhs=qkT[pb:pb + 64, 2 + hp, kwin:kwin + 512], start=True, stop=True),
                     reads=[qkT], writes=[p1])
                S.op("dve", lambda e: e.scalar_tensor_tensor(out=s[:M, 0:512], in0=p1[:M, :], scalar=scale, in1=bt[:M, cls, :],
                                                             op0=ALU.mult, op1=ALU.add), reads=[p1, bt], writes=[(s, "w")])
                nk = 512
            p2 = ps2.next()
            S.op("pe", lambda e: e.matmul(p2[:M, :], lhsT=qTs, rhs=qkT[pb:pb + 64, 2 + hp, 4096:4352], start=True, stop=True),
                 reads=[qkT], writes=[p2])
            S.op("act", lambda e: e.mul(out=s[:M, nk:nk + 256], in_=p2[:M, :], mul=scale), reads=[p2], writes=[(s, "c")])
            nk += 256
            m = sm.next()
            S.op("dve", lambda e: e.tensor_reduce(out=m[:M, 0:1], in_=s[:M, :nk], axis=AX.X, op=ALU.max, negate=True),
                 reads=[s], writes=[(m, 0)])
            return dict(h=h, M=M, vchunks=vchunks, out_ap=out_ap, s=s, m=m, nk=nk)

        def att_s2(c_):
            M, s, m, nk = c_["M"], c_["s"], c_["m"], c_["nk"]
            pe = pe_.next()
            S.op("act", lambda e: e.activation(out=pe[:M, :nk], in_=s[:M, :nk], func=AF.Exp, bias=m[:M, 0:1], scale=1.0,
                                               accum_out=m[:M, 1:2]), reads=[s, (m, 0)], writes=[pe, (m, 1)])
            nch = nk // 128
            pp = ppT.next()
            for kc in range(nch):
                S.op("pe", lambda e: e.transpose(out=pp[:, kc, :M], in_=pe[:M, kc * 128:(kc + 1) * 128], identity=ident[:M, :M]),
                     reads=[pe, ident], writes=[(pp, kc)])
            pt = pT.next()
            S.op("dve", lambda e: e.tensor_copy(out=pt[:, :nch, :M], in_=pp[:, :nch, :M]), reads=[pp], writes=[pt])
            c_.update(pt=pt, nch=nch)

        def att_s3(c_):
            h, M, vchunks, out_ap, m, pt, nch = (c_[k] for k in ("h", "M", "vchunks", "out_ap", "m", "pt", "nch"))
            o = po.next()
            for kc in range(nch):
                vbuf, vi = vchunks[kc]
                S.op("pe", lambda e: e.matmul(o[:M, :], lhsT=pt[:, kc, :M], rhs=vbuf[:, vi, h * 64:(h + 1) * 64],
                                              start=(kc == 0), stop=(kc == nch - 1)), reads=[pt, vbuf], writes=[o])
            S.op("dve", lambda e: e.reciprocal(out=m[:M, 2:3], in_=m[:M, 1:2]), reads=[(m, 1)], writes=[(m, 2)])
            y = yo.next()
            S.op("dve", lambda e: e.tensor_scalar(out=y[:M, :], in0=o[:M, :], scalar1=m[:M, 2:3], scalar2=None, op0=ALU.mult),
                 reads=[o, (m, 2)], writes=[y])
            S.dma("sp", lambda e: e.dma_start(out=out_ap, in_=y[:M, :]), reads=[y])

        jobs = []
        for h in heads:
            bt = bias.next()
            S.dma("sp", lambda e: e.dma_start(out=bt[:], in_=D["na_bias"][l, h].rearrange("c q k -> q c k")), writes=[bt])
            for r in rows:
                r0 = min(max(r - 4, 0), 56)
                cls = r if r < 4 else (4 if r <= 60 else r - 56)
                if r0 % 2 == 0:
                    vch = [(va, r0 // 2 + jj) for jj in range(4)]
                else:
                    vch = [(vb, (r0 - 1) // 2 + jj) for jj in range(4)]
                vch += [(vc, 0), (vc, 1)]
                jobs.append((h, 64, (r * 64, (r + 1) * 64), r0 * 64, cls, vch, bt,
                             D["ycat"][r * 64:(r + 1) * 64, h * 64:(h + 1) * 64]))
            if need_ctx:
                for ct in range(2):
                    jobs.append((h, 128, (4096 + ct * 128, 4096 + (ct + 1) * 128), None, None, [(vc, 0), (vc, 1)], None,
                                 D["ycat"][4096 + ct * 128:4096 + (ct + 1) * 128, h * 64:(h + 1) * 64]))
        st_ = {}
        nj = len(jobs)
        for i in range(nj + 2):
            if i < nj:
                st_[i] = att_s1(*jobs[i])
            if 0 <= i - 1 < nj:
                att_s2(st_[i - 1])
            if 0 <= i - 2 < nj:
                att_s3(st_.pop(i - 2))


def phase_gla(S, D, l, need_ctx=True, lat_chunks=64):
    qscale = 32 ** -0.5
    with S.phase():
        identf = D["ident_f"]
        identb = D["ident_bf"]
        ropeC = S.tile([64, 64, 2, 8], F32, "ropeC")
        ropeS = S.tile([64, 64, 2, 8], F32, "ropeS")
        mt = S.tile([64, 2, 64], F32, "mt")
        tri = S.tile([64, 2, 64], F32, "tri")
        blk = S.tile([128, 4, 64], F32, "blk")
        gw = S.tile([33, 2, 128], F32, "gw")
        ngb = vec_bcast(S, D["gla_norm_g"][l], 256, "ngb")
        for t, src in ((ropeC, D["ropeC"]), (ropeS, D["ropeS"]), (mt, D["gla_mt"]), (tri, D["gla_tri"]), (blk, D["gla_blk"]),
                       (gw, D["gla_gw"][l].rearrange("d k n -> k d n"))):
            S.dma("sp", lambda e: e.dma_start(out=t[:], in_=src), writes=[t])
        Sblk = S.tile([128, 256], F32, "Sblk")
        Sbf = S.tile([128, 256], BF16, "Sbf")
        zc_r = Rot(S, 10, [64, 800], F32, name="zc")
        qkr = Rot(S, 2, [64, 256], F32, name="qkr")
        rt = Rot(S, 4, [64, 8, 2, 8], F32, name="rt")
        loT = Rot(S, 3, [33, 64], F32, name="loT")
        for b in loT.bufs:
            S.op("pool", lambda e: e.memset(b[32:33, :], 1.0), writes=[(b, "one")])
        e1 = Rot(S, 2, [64, 128], F32, name="e1")
        sp = Rot(S, 2, [64, 128], F32, name="sp")
        eb = Rot(S, 5, [128, 64], F32, name="eb")
        enb = Rot(S, 3, [128, 64], F32, name="enb")
        qs = Rot(S, 3, [128, 64], BF16, name="qs")
        ks = Rot(S, 2, [128, 64], BF16, name="ks")
        ke = Rot(S, 2, [128, 64], BF16, name="ke")
        qb = Rot(S, 2, [128, 4, 64], BF16, name="qb")
        kend = Rot(S, 3, [64, 128], BF16, name="kend")
        am = Rot(S, 3, [64, 4, 64], BF16, name="am")
        vbf = Rot(S, 3, [64, 256], BF16, name="vbf")
        tu = Rot(S, 4, [128, 256], F32, name="tu")
        of_ = Rot(S, 10, [64, 256], F32, name="of")
        osum = Rot(S, 2, [64, 256], F32, name="osum")
        sq = Rot(S, 2, [64, 256], F32, name="sq")
        ms = Rot(S, 2, [64, 16], F32, name="ms")
        sr = Rot(S, 2, [64, 256], F32, name="sr")
        yo = Rot(S, 2, [64, 256], F32, name="yo")
        bankA = Rot(S, 3, [128, 512], F32, psum=True, name="bankA")
        pkT = Rot(S, 1, [64, 128], BF16, psum=True, name="pkT")
        pA = Rot(S, 1, [64, 256], F32, psum=True, name="pA")
        pO = Rot(S, 2, [64, 256], F32, psum=True, name="pO")
        pU = Rot(S, 1, [128, 256], F32, psum=True, name="pU")

        def chunk_l(tok0, latent, n, d, want_out):
            z = zc_r.next()
            S.dma("sp", lambda e: e.dma_start(out=z[:], in_=D["z"][tok0:tok0 + 64, C_GQ:C_GQ + 800]), writes=[z])
            o_pre = None
            if want_out and d == 1:
                o_pre = of_.next()
                S.dma("sp", lambda en: en.dma_start(out=o_pre[:], in_=D["ofwd"][tok0:tok0 + 64, :]), writes=[o_pre])
            return dict(tok0=tok0, latent=latent, n=n, d=d, want_out=want_out, z=z, o_pre=o_pre)

        def chunk_a1(c_):
            tok0, latent, n, d, want_out, z = (c_[k] for k in ("tok0", "latent", "n", "d", "want_out", "z"))
            if latent:
                q = qkr.next()
                zv = z[:, 0:256].rearrange("p (h a b f) -> p h a b f", h=8, a=2, b=2)
                qv = q[:].rearrange("p (h a b f) -> p h a b f", h=8, a=2, b=2)
                xa, xb_ = zv[:, :, :, 0, :], zv[:, :, :, 1, :]
                cosb = ropeC[:, n, :, :].unsqueeze(1).to_broadcast([64, 8, 2, 8])
                sinb = ropeS[:, n, :, :].unsqueeze(1).to_broadcast([64, 8, 2, 8])
                t1, t2, t3, t4 = rt.next(), rt.next(), rt.next(), rt.next()
                S.op("dve", lambda e: e.tensor_tensor(out=t1[:], in0=xa, in1=cosb, op=ALU.mult), reads=[z, ropeC], writes=[t1])
                S.op("pool", lambda e: e.tensor_tensor(out=t2[:], in0=xb_, in1=sinb, op=ALU.mult), reads=[z, ropeS], writes=[t2])
                S.op("pool", lambda e: e.tensor_tensor(out=t3[:], in0=xa, in1=sinb, op=ALU.mult), reads=[z, ropeS], writes=[t3])
                S.op("dve", lambda e: e.tensor_tensor(out=t4[:], in0=xb_, in1=cosb, op=ALU.mult), reads=[z, ropeC], writes=[t4])
                S.op("dve", lambda e: e.tensor_tensor(out=qv[:, :, :, 0, :], in0=t1[:], in1=t2[:], op=ALU.subtract), reads=[t1, t2], writes=[(q, "a")])
                S.op("pool", lambda e: e.tensor_tensor(out=qv[:, :, :, 1, :], in0=t3[:], in1=t4[:], op=ALU.add), reads=[t3, t4], writes=[(q, "b")])
                qsrc, qdep = q, q
            else:
                qsrc, qdep = z, z
            A = bankA.next()
            for c in range(2):
                S.op("pe", lambda e: e.transpose(out=A[:, c * 64:(c + 1) * 64], in_=qsrc[:, c * 128:(c + 1) * 128], identity=identf[:64, :64]),
                     reads=[qdep, identf], writes=[(A, "qk%d" % c)])
            S.op("pe", lambda e: e.transpose(out=A[0:32, 320:384], in_=z[:, 768:800], identity=identf[:64, :64]),
                 reads=[z, identf], writes=[(A, "lo")])
            lt = loT.next()
            S.op("act", lambda e: e.copy(out=lt[0:32, :], in_=A[0:32, 320:384]), reads=[(A, "lo")], writes=[(lt, "d")])
            c_.update(A=A, lt=lt)

        def chunk_a2(c_):
            d, A, lt = c_["d"], c_["A"], c_["lt"]
            S.op("pe", lambda e: e.matmul(A[0:64, 192:320], lhsT=lt[:, :], rhs=gw[:, d, :], start=True, stop=True),
                 reads=[lt, gw], writes=[(A, "g")])
            e = e1.next()
            S.op("act", lambda en: en.activation(out=e[:], in_=A[0:64, 192:320], func=AF.Exp, scale=-1.0), reads=[(A, "g")], writes=[e])
            s_ = sp.next()
            S.op("act", lambda en: en.activation(out=s_[:], in_=e[:], func=AF.Ln, bias=1.0, scale=1.0), reads=[e], writes=[s_])
            S.op("pe", lambda en: en.matmul(A[:, 128:192], lhsT=s_[:, :], rhs=mt[:, d, :], start=True, stop=True),
                 reads=[s_, mt], writes=[(A, "b")])
            ebt = eb.next(); enbt = enb.next()
            S.op("act", lambda en: en.activation(out=ebt[:], in_=A[:, 128:192], func=AF.Exp), reads=[(A, "b")], writes=[ebt])
            S.op("act", lambda en: en.activation(out=enbt[:], in_=A[:, 128:192], func=AF.Exp, scale=-1.0), reads=[(A, "b")], writes=[enbt])
            dec = ebt[:, 63:64] if d == 0 else ebt[:, 0:1]
            c_.update(ebt=ebt, enbt=enbt, dec=dec)

        def chunk_a3(c_):
            d, A, z, ebt, enbt, dec = (c_[k] for k in ("d", "A", "z", "ebt", "enbt", "dec"))
            qst = qs.next(); kst = ks.next(); ket = ke.next(); qbt = qb.next()
            S.op("dve", lambda en: en.scalar_tensor_tensor(out=qst[:], in0=A[:, 0:64], scalar=qscale, in1=ebt[:], op0=ALU.mult, op1=ALU.mult),
                 reads=[(A, "qk0"), ebt], writes=[qst])
            S.op("dve", lambda en: en.tensor_tensor(out=kst[:], in0=A[:, 64:128], in1=enbt[:], op=ALU.mult), reads=[(A, "qk1"), enbt], writes=[kst])
            S.op("dve", lambda en: en.scalar_tensor_tensor(out=ket[:], in0=A[:, 64:128], scalar=dec, in1=enbt[:], op0=ALU.mult, op1=ALU.mult),
                 reads=[(A, "qk1"), enbt, ebt], writes=[ket])
            S.op("pool", lambda en: en.tensor_tensor(out=qbt[:], in0=qst[:].unsqueeze(1).to_broadcast([128, 4, 64]), in1=blk[:], op=ALU.mult),
                 reads=[qst, blk], writes=[qbt])
            pk = pkT.next()
            S.op("pe", lambda en: en.transpose(out=pk[:], in_=ket[:], identity=identb[:]), reads=[ket, identb], writes=[pk])
            kt = kend.next()
            S.op("act", lambda en: en.copy(out=kt[:], in_=pk[:]), reads=[pk], writes=[kt])
            v = vbf.next()
            S.op("pool", lambda en: en.tensor_copy(out=v[:], in_=z[:, 256:512]), reads=[z], writes=[v])
            pa = pA.next()
            S.op("pe", lambda en: en.matmul(pa[:], lhsT=kst[:], rhs=qbt[:].rearrange("p h c -> p (h c)"), start=True, stop=True),
                 reads=[kst, qbt], writes=[pa])
            amt = am.next()
            S.op("dve", lambda en: en.tensor_tensor(out=amt[:], in0=pa[:].rearrange("p (h c) -> p h c", h=4),
                                                   in1=tri[:, d, :].unsqueeze(1).to_broadcast([64, 4, 64]), op=ALU.mult),
                 reads=[pa, tri], writes=[amt])
            pu = pU.next()
            S.op("pe", lambda en: en.matmul(pu[:], lhsT=kt[:], rhs=v[:], start=True, stop=True), reads=[kt, v], writes=[pu])
            tut = tu.next()
            S.op("dve", lambda en: en.tensor_tensor(out=tut[:], in0=pu[:], in1=blk[:].rearrange("p h c -> p (h c)"), op=ALU.mult),
                 reads=[pu, blk], writes=[tut])
            c_.update(qst=qst, kt=kt, amt=amt, v=v, tut=tut)

        def chunk_b(c_):
            tok0, d, want_out, z, ebt, dec, qst, kt, amt, v = (c_[k] for k in ("tok0", "d", "want_out", "z", "ebt", "dec", "qst", "kt", "amt", "v"))
            tut = c_["tut"]
            po = pO.next()
            S.op("pe", lambda en: en.matmul(po[:], lhsT=qst[:], rhs=Sbf[:], start=True, stop=False, skip_group_check=True),
                 reads=[qst, Sbf], writes=[po])
            for h in range(4):
                S.op("pe", lambda en: en.matmul(po[:, h * 64:(h + 1) * 64], lhsT=amt[:, h, :], rhs=v[:, h * 64:(h + 1) * 64],
                                               start=False, stop=True, skip_group_check=True), reads=[amt, v], writes=[po])
            S.op("dve", lambda en: en.scalar_tensor_tensor(out=Sblk[:], in0=Sblk[:], scalar=dec, in1=tut[:], op0=ALU.mult, op1=ALU.add),
                 reads=[Sblk, ebt, tut], writes=[Sblk])
            S.op("act", lambda en: en.copy(out=Sbf[:], in_=Sblk[:]), reads=[Sblk], writes=[Sbf])
            if not want_out:
                return
            if d == 0:
                o = of_.next()
                S.op("act", lambda en: en.copy(out=o[:], in_=po[:]), reads=[po], writes=[o])
                S.dma("sp", lambda en: en.dma_start(out=D["ofwd"][tok0:tok0 + 64, :], in_=o[:]), reads=[o])
                return
            o = c_["o_pre"]
            os_ = osum.next()
            S.op("dve", lambda en: en.tensor_tensor(out=os_[:], in0=po[:], in1=o[:], op=ALU.add), reads=[po, o], writes=[os_])
            q2 = sq.next()
            S.op("pool", lambda en: en.tensor_tensor(out=q2[:], in0=os_[:], in1=os_[:], op=ALU.mult), reads=[os_], writes=[q2])
            m = ms.next()
            S.op("dve", lambda en: en.tensor_reduce(out=m[:, 0:4], in_=q2[:].rearrange("p (h c) -> p h c", h=4), axis=AX.X, op=ALU.add),
                 reads=[q2], writes=[(m, 0)])
            S.op("dve", lambda en: en.tensor_scalar(out=m[:, 4:8], in0=m[:, 0:4], scalar1=1.0 / 64, scalar2=LN_EPS, op0=ALU.mult, op1=ALU.add),
                 reads=[(m, 0)], writes=[(m, 1)])
            S.op("act", lambda en: en.activation(out=m[:, 8:12], in_=m[:, 4:8], func=AF.Ln), reads=[(m, 1)], writes=[(m, 2)])
            S.op("act", lambda en: en.activation(out=m[:, 12:16], in_=m[:, 8:12], func=AF.Exp, scale=-0.5), reads=[(m, 2)], writes=[(m, 3)])
            S.op("dve", lambda en: en.tensor_tensor(out=os_[:].rearrange("p (h c) -> p h c", h=4), in0=os_[:].rearrange("p (h c) -> p h c", h=4),
                                                   in1=m[:, 12:16].unsqueeze(2).to_broadcast([64, 4, 64]), op=ALU.mult),
                 reads=[os_, (m, 3)], writes=[os_])
            S.op("pool", lambda en: en.tensor_tensor(out=os_[:], in0=os_[:], in1=ngb[0:64, :], op=ALU.mult), reads=[os_, ngb], writes=[os_])
            srt = sr.next()
            S.op("act", lambda en: en.activation(out=srt[:], in_=z[:, 512:768], func=AF.Exp, scale=-1.0), reads=[z], writes=[srt])
            S.op("pool", lambda en: en.tensor_scalar(out=srt[:], in0=srt[:], scalar1=1.0, scalar2=None, op0=ALU.add), reads=[srt], writes=[srt])
            S.op("dve", lambda en: en.reciprocal(out=srt[:], in_=srt[:]), reads=[srt], writes=[srt])
            S.op("pool", lambda en: en.tensor_tensor(out=srt[:], in0=srt[:], in1=z[:, 512:768], op=ALU.mult), reads=[srt, z], writes=[srt])
            y = yo.next()
            S.op("dve", lambda en: en.tensor_tensor(out=y[:], in0=os_[:], in1=srt[:], op=ALU.mult), reads=[os_, srt], writes=[y])
            S.dma("sp", lambda en: en.dma_start(out=D["ycat"][tok0:tok0 + 64, 256:512], in_=y[:]), reads=[y])

        for d in range(2):
            S.op("pool", lambda en: en.memset(Sblk[:], 0.0), writes=[Sblk])
            S.op("pool", lambda en: en.memset(Sbf[:], 0.0), writes=[Sbf])
            order = list(range(4)) if d == 0 else list(range(3, -1, -1))
            jobs = [(SEQ + 64 * i, False, 0, d, need_ctx) for i in order]
            order = list(range(lat_chunks)) if d == 0 else list(range(lat_chunks - 1, -1, -1))
            jobs += [(64 * i, True, i, d, True) for i in order]
            st_ = {}
            nj = len(jobs)
            PF = 4
            for i in range(nj + 3 + PF):
                if i < nj:
                    st_[i] = chunk_l(*jobs[i])
                if 0 <= i - PF < nj:
                    chunk_a1(st_[i - PF])
                if 0 <= i - PF - 1 < nj:
                    chunk_a2(st_[i - PF - 1])
                if 0 <= i - PF - 2 < nj:
                    chunk_a3(st_[i - PF - 2])
                if 0 <= i - PF - 3 < nj:
                    chunk_b(st_.pop(i - PF - 3))
            S.barrier()


def phase_peer_topk(S, D, l, tiles=range(NTT), xin="xs"):
    with S.phase():
        ident = D["ident_bf"]
        wq = S.tile([128, 8, 2048], BF16, "wq")
        S.dma("pool", lambda e: e.dma_start(out=wq[:], in_=D["peer_wq"][l].rearrange("(k p) n -> p k n", p=128)), writes=[wq])
        kT = S.tile([128, 16, 128], F32, "kT")
        S.dma("sp", lambda e: e.dma_start(out=kT[:], in_=D["peer_keysT"][l].rearrange("b d k -> d b k")), writes=[kT])
        iota = S.tile([128, 16, 16], F32, "iota")
        S.dma("sp", lambda e: e.dma_start(out=iota[:], in_=D["iota_kk"]), writes=[iota])
        scb = [bcast_load(S, D, l, j, 4096, 1024, "scb") for j in range(2)]
        shb = [bcast_load(S, D, l, j, 3072, 1024, "shb") for j in range(2)]
        xt = Rot(S, 2, [128, 1024], F32, name="xt")
        hf = Rot(S, 1, [128, 1024], F32, name="hf")
        hb = Rot(S, 2, [128, 1024], BF16, name="hb")
        hT = Rot(S, 2, [128, 8, 128], BF16, name="hT")
        qT = Rot(S, 2, [128, 16, 128], F32, name="qT")
        sc = Rot(S, 3, [128, 16, 128], F32, name="sc")
        wk16 = Rot(S, 1, [128, 16, 128], F32, name="wk16")
        wk8 = Rot(S, 1, [128, 8, 256], F32, name="wk8")
        t1 = Rot(S, 3, [128, 16, 16], F32, name="t1")
        ti = Rot(S, 3, [128, 16, 16], U32, name="ti")
        tif = Rot(S, 3, [128, 16, 16], F32, name="tif")
        cs = Rot(S, 2, [128, 8, 256], F32, name="cs")
        bs = Rot(S, 3, [128, 8, 16], F32, name="bs")
        bj = Rot(S, 3, [128, 8, 16], U32, name="bj")
        hi = Rot(S, 2, [128, 8, 16], U32, name="hi")
        lo = Rot(S, 2, [128, 8, 16], U32, name="lo")
        hif = Rot(S, 2, [128, 8, 16], F32, name="hif")
        lof = Rot(S, 2, [128, 8, 16], F32, name="lof")
        oh = Rot(S, 2, [128, 8, 16, 16], F32, name="oh")
        ee = Rot(S, 4, [128, 8, 16], F32, name="ee")
        ei = Rot(S, 2, [128, 128], I32, name="ei")
        gg = Rot(S, 2, [128, 8, 16], F32, name="gg")
        sm = Rot(S, 2, [128, 32], F32, name="sm")
        pT = Rot(S, 2, [128, 8, 128], BF16, psum=True, name="pT")
        pq = Rot(S, 3, [128, 4, 128], F32, psum=True, name="pq")
        def stage_a(tt):
            j = 0 if tt < 32 else 1
            r0 = tt * 128
            x = xt.next()
            S.dma("sp", lambda e: e.dma_start(out=x[:], in_=D[xin][r0:r0 + 128, :]), writes=[x])
            h1 = hf.next()
            S.op("pool", lambda e: e.tensor_tensor(out=h1[:], in0=x[:], in1=scb[j][:], op=ALU.mult), reads=[x, scb[j]], writes=[h1])
            h2 = hb.next()
            S.op("pool", lambda e: e.tensor_tensor(out=h2[:], in0=h1[:], in1=shb[j][:], op=ALU.add), reads=[h1, shb[j]], writes=[h2])
            p = pT.next()
            for k in range(8):
                S.op("pe", lambda e: e.transpose(out=p[:, k, :], in_=h2[:, k * 128:(k + 1) * 128], identity=ident[:]),
                     reads=[h2, ident], writes=[(p, k)])
            t = hT.next()
            S.op("act", lambda e: e.copy(out=t[:], in_=p[:]), reads=[p], writes=[t])
            q = qT.next()
            for g in range(4):
                pp = pq.next()
                for b in range(4):
                    blk = g * 4 + b
                    for k in range(8):
                        S.op("pe", lambda e: e.matmul(pp[:, b, :], lhsT=wq[:, k, blk * 128:(blk + 1) * 128], rhs=t[:, k, :],
                                                      start=(k == 0), stop=(k == 7)), reads=[wq, t], writes=[(pp, b)])
                S.op("act", lambda e: e.copy(out=q[:, g * 4:(g + 1) * 4, :], in_=pp[:]), reads=[pp], writes=[(q, g)])
            s = sc.next()
            for g in range(4):
                pp = pq.next()
                for b in range(4):
                    blk = g * 4 + b
                    S.op("pe", lambda e: e.matmul(pp[:, b, :], lhsT=q[:, blk, :], rhs=kT[:, blk, :], start=True, stop=True),
                         reads=[q, kT], writes=[(pp, b)])
                S.op("act", lambda e: e.copy(out=s[:, g * 4:(g + 1) * 4, :], in_=pp[:]), reads=[pp], writes=[(s, g)])
            return dict(r0=r0, s=s)

        def stage_b1(st_):
            r0, s = st_["r0"], st_["s"]
            tv = t1.next(); tix = ti.next()
            w16 = wk16.next()
            for blk in range(16):
                S.op("dve", lambda e: e.max(out=tv[:, blk, 0:8], in_=s[:, blk, :]), reads=[s], writes=[(tv, (blk, 0))])
            for blk in range(16):
                S.op("dve", lambda e: e.match_replace(out=w16[:, blk, :], in_to_replace=tv[:, blk, 0:8], in_values=s[:, blk, :], imm_value=-1e30),
                     reads=[s, (tv, (blk, 0))], writes=[(w16, blk)])
            for blk in range(16):
                S.op("dve", lambda e: e.max(out=tv[:, blk, 8:16], in_=w16[:, blk, :]), reads=[(w16, blk)], writes=[(tv, (blk, 1))])
            for blk in range(16):
                S.op("dve", lambda e: e.max_index(out=tix[:, blk, 0:8], in_max=tv[:, blk, 0:8], in_values=s[:, blk, :]),
                     reads=[s, (tv, (blk, 0))], writes=[(tix, (blk, 0))])
            for blk in range(16):
                S.op("dve", lambda e: e.max_index(out=tix[:, blk, 8:16], in_max=tv[:, blk, 8:16], in_values=w16[:, blk, :]),
                     reads=[(w16, blk), (tv, (blk, 1))], writes=[(tix, (blk, 1))])
            st_.update(tv=tv, tix=tix)

        def stage_b2(st_):
            tv, tix = st_["tv"], st_["tix"]
            tf = tif.next()
            S.op("pool", lambda e: e.tensor_copy(out=tf[:], in_=tix[:]), reads=[tix], writes=[tf])
            c = cs.next()
            tv4 = tv[:].rearrange("p (h s) k -> p h s k", s=2)
            S.op("dve", lambda e: e.tensor_tensor(out=c[:].rearrange("p h (i j) -> p h i j", i=16),
                                                  in0=tv4[:, :, 0, :].unsqueeze(3).to_broadcast([128, 8, 16, 16]),
                                                  in1=tv4[:, :, 1, :].unsqueeze(2).to_broadcast([128, 8, 16, 16]), op=ALU.add),
                 reads=[tv], writes=[c])
            b_ = bs.next(); bjx = bj.next()
            w8 = wk8.next()
            for h in range(8):
                S.op("dve", lambda e: e.max(out=b_[:, h, 0:8], in_=c[:, h, :]), reads=[c], writes=[(b_, (h, 0))])
            for h in range(8):
                S.op("dve", lambda e: e.match_replace(out=w8[:, h, :], in_to_replace=b_[:, h, 0:8], in_values=c[:, h, :], imm_value=-1e30),
                     reads=[c, (b_, (h, 0))], writes=[(w8, h)])
            for h in range(8):
                S.op("dve", lambda e: e.max(out=b_[:, h, 8:16], in_=w8[:, h, :]), reads=[(w8, h)], writes=[(b_, (h, 1))])
            for h in range(8):
                S.op("dve", lambda e: e.max_index(out=bjx[:, h, 0:8], in_max=b_[:, h, 0:8], in_values=c[:, h, :]),
                     reads=[c, (b_, (h, 0))], writes=[(bjx, (h, 0))])
            for h in range(8):
                S.op("dve", lambda e: e.max_index(out=bjx[:, h, 8:16], in_max=b_[:, h, 8:16], in_values=w8[:, h, :]),
                     reads=[(w8, h), (b_, (h, 1))], writes=[(bjx, (h, 1))])
            st_.update(tf=tf, b_=b_, bjx=bjx)

        def stage_b3(st_):
            r0, tf, b_, bjx = (st_[k] for k in ("r0", "tf", "b_", "bjx"))
            hx = hi.next(); lx = lo.next(); hfx = hif.next(); lfx = lof.next()
            S.op("dve", lambda e: e.tensor_single_scalar(out=hx[:], in_=bjx[:], scalar=4, op=ALU.logical_shift_right), reads=[bjx], writes=[hx])
            S.op("dve", lambda e: e.tensor_single_scalar(out=lx[:], in_=bjx[:], scalar=15, op=ALU.bitwise_and), reads=[bjx], writes=[lx])
            S.op("pool", lambda e: e.tensor_copy(out=hfx[:], in_=hx[:]), reads=[hx], writes=[hfx])
            S.op("pool", lambda e: e.tensor_copy(out=lfx[:], in_=lx[:]), reads=[lx], writes=[lfx])
            tf4 = tf[:].rearrange("p (h s) k -> p h s k", s=2)
            es = []
            for (sel, half) in ((hfx, 0), (lfx, 1)):
                o = oh.next()
                S.op("dve", lambda e: e.tensor_tensor(out=o[:], in0=sel[:].unsqueeze(3).to_broadcast([128, 8, 16, 16]),
                                                      in1=iota[:].unsqueeze(1).to_broadcast([128, 8, 16, 16]), op=ALU.is_equal),
                     reads=[sel, iota], writes=[o])
                S.op("pool", lambda e: e.tensor_tensor(out=o[:], in0=o[:], in1=tf4[:, :, half, :].unsqueeze(2).to_broadcast([128, 8, 16, 16]),
                                                       op=ALU.mult), reads=[o, tf], writes=[o])
                ex = ee.next()
                S.op("dve", lambda e: e.tensor_reduce(out=ex[:].rearrange("p h k -> p (h k)"), in_=o[:].rearrange("p h k i -> p (h k) i"),
                                                      axis=AX.X, op=ALU.add), reads=[o], writes=[ex])
                es.append(ex)
            ef = ee.next()
            S.op("dve", lambda e: e.scalar_tensor_tensor(out=ef[:], in0=es[0][:], scalar=128.0, in1=es[1][:], op0=ALU.mult, op1=ALU.add),
                 reads=[es[0], es[1]], writes=[ef])
            eix = ei.next()
            S.op("dve", lambda e: e.tensor_copy(out=eix[:], in_=ef[:].rearrange("p h k -> p (h k)")), reads=[ef], writes=[eix])
            identf = D["ident_f"]
            ptr = pq.next()
            S.op("pe", lambda e: e.transpose(out=ptr[:, 0, :], in_=ef[:].rearrange("p h k -> p (h k)"), identity=identf[:]),
                 reads=[ef, identf], writes=[(ptr, 0)])
            S.op("dve", lambda e: e.tensor_copy(out=eix[:], in_=ptr[:, 0, :]), reads=[(ptr, 0)], writes=[eix])
            S.dma("sp", lambda e: e.dma_start(out=D["pidx"][r0:r0 + 128, :], in_=eix[:]), reads=[eix])
            g_ = gg.next(); m = sm.next()
            S.op("dve", lambda e: e.tensor_tensor(out=g_[:], in0=b_[:], in1=b_[:, :, 0:1].to_broadcast([128, 8, 16]), op=ALU.subtract),
                 reads=[b_], writes=[g_])
            S.op("act", lambda e: e.activation(out=g_[:], in_=g_[:], func=AF.Exp), reads=[g_], writes=[g_])
            S.op("dve", lambda e: e.tensor_reduce(out=m[:, 0:8], in_=g_[:], axis=AX.X, op=ALU.add), reads=[g_], writes=[(m, 0)])
            S.op("dve", lambda e: e.reciprocal(out=m[:, 8:16], in_=m[:, 0:8]), reads=[(m, 0)], writes=[(m, 1)])
            S.op("dve", lambda e: e.tensor_tensor(out=g_[:], in0=g_[:], in1=m[:, 8:16].unsqueeze(2).to_broadcast([128, 8, 16]), op=ALU.mult),
                 reads=[g_, (m, 1)], writes=[g_])
            S.op("pe", lambda e: e.transpose(out=ptr[:, 1, :], in_=g_[:].rearrange("p h k -> p (h k)"), identity=identf[:]),
                 reads=[g_, identf], writes=[(ptr, 1)])
            gT_ = qT.next()
            S.op("act", lambda e: e.copy(out=gT_[:, 0, :], in_=ptr[:, 1, :]), reads=[(ptr, 1)], writes=[gT_])
            S.dma("sp", lambda e: e.dma_start(out=D["pgt"][r0:r0 + 128, :], in_=gT_[:, 0, :]), reads=[gT_])


        tiles = list(tiles)
        stt = {}
        nj = len(tiles)
        for i in range(nj + 3):
            if i < nj:
                stt[i] = stage_a(tiles[i])
            if 0 <= i - 1 < nj:
                stage_b1(stt[i - 1])
            if 0 <= i - 2 < nj:
                stage_b2(stt[i - 2])
            if 0 <= i - 3 < nj:
                stage_b3(stt.pop(i - 3))


TAB_CHUNK = 512


def start_table_convert(S, D, l):
    vals = []
    for ti, nm in enumerate(("peer_u", "peer_v")):
        sem = S.bg[2 * l + ti]
        n = 0
        for r0 in range(0, 16384, TAB_CHUNK):
            v = S.bg_dma("pool", lambda e: e.dma_start(out=D["peer_uvb%d" % l][r0:r0 + TAB_CHUNK, ti * 1024:(ti + 1) * 1024],
                                                       in_=D["%s%d" % (nm, l)][r0:r0 + TAB_CHUNK, :]), sem, n)
            n += 1
        vals.append(v)
    return vals


def phase_peer_ffn(S, D, l, tiles=range(NTT), xin="xs", xout="xs", conv_vals=None):
    tiles = list(tiles)
    if conv_vals is not None:
        for ti in range(2):
            S.wait_sem(("pool", "sp"), S.bg[2 * l + ti], conv_vals[ti])
    with S.phase():
        ident = D["ident_bf"]
        uvb = D["peer_uvb%d" % l]
        scb = [bcast_load(S, D, l, j, 4096, 1024, "scb") for j in range(2)]
        shb = [bcast_load(S, D, l, j, 3072, 1024, "shb") for j in range(2)]
        g2b = [bcast_load(S, D, l, j, 5120, 1024, "g2b") for j in range(2)]
        lng = vec_bcast(S, D["ln2_g"][l], 1024, "lng")
        lnb = vec_bcast(S, D["ln2_b"][l], 1024, "lnb")
        xt = Rot(S, 3, [128, 1024], F32, name="xt")
        hf = Rot(S, 2, [128, 1024], F32, name="hf")
        hb = Rot(S, 3, [128, 1024], BF16, name="hb")
        idx = Rot(S, 3, [128, 128], I32, name="idx")
        gt = Rot(S, 3, [128, 128], F32, name="gt")
        gb = Rot(S, 10, [128, 2048], BF16, name="gb")
        junk = Rot(S, 2, [128, 1024], BF16, name="junk")
        aT = Rot(S, 2, [128, 128], F32, name="aT")
        cf = Rot(S, 2, [128, 128], F32, name="cf")
        zb = Rot(S, 6, [128, 255], BF16, name="zb")
        for b in zb.bufs:
            S.op("pool", lambda e: e.memset(b[:], 0.0), writes=[b])
        tm = Rot(S, 2, [128, 1024], F32, name="tm")
        sm = Rot(S, 2, [128, 32], F32, name="sm")
        xo = Rot(S, 2, [128, 1024], F32, name="xo")
        pb = Rot(S, 3, [128, 1024], F32, psum=True, name="pb")
        po_r = Rot(S, 1, [128, 1024], F32, psum=True, name="po")
        def load_stage(tt):
            j = 0 if tt < 32 else 1
            r0 = tt * 128
            x = xt.next(); ix = idx.next(); g = gt.next()
            S.dma("sp", lambda e: e.dma_start(out=ix[:], in_=D["pidx"][r0:r0 + 128, :]), writes=[ix])
            S.dma("sp", lambda e: e.dma_start(out=x[:], in_=D[xin][r0:r0 + 128, :]), writes=[x])
            S.dma("sp", lambda e: e.dma_start(out=g[:], in_=D["pgt"][r0:r0 + 128, :]), writes=[g])
            h1 = hf.next()
            S.op("dve", lambda e: e.tensor_tensor(out=h1[:], in0=x[:], in1=scb[j][:], op=ALU.mult), reads=[x, scb[j]], writes=[h1])
            h = hb.next()
            S.op("dve", lambda e: e.tensor_tensor(out=h[:], in0=h1[:], in1=shb[j][:], op=ALU.add), reads=[h1, shb[j]], writes=[h])
            return (j, r0, x, ix, g, h)

        nxt = load_stage(tiles[0]) if tiles else None
        for ti_, tt in enumerate(tiles):
            j, r0, x, ix, g, h = nxt
            nxt = load_stage(tiles[ti_ + 1]) if ti_ + 1 < len(tiles) else None
            at = aT.next(); c = cf.next(); po = po_r.next()
            LAG = 3
            uvs = {}

            pbs = {}

            def stage_a0(t):
                uv = gb.next()
                uvs[t] = uv
                S.dma("pool", lambda e: e.indirect_dma_start(out=uv[:], out_offset=None, in_=uvb,
                                                             in_offset=bass.IndirectOffsetOnAxis(ap=ix[:, t:t + 1], axis=0)),
                      reads=[ix], writes=[uv])
                p = pb.next()
                pbs[t] = p
                for half in range(2):
                    S.op("pe", lambda e: e.matmul(p[:, half * 512:(half + 1) * 512], lhsT=ident[:, t:t + 1].to_broadcast([128, 128]),
                                                  rhs=h[:, half * 512:(half + 1) * 512], start=True, stop=True),
                         reads=[ident, h], writes=[(p, half)])

            def stage_a1(t):
                uv = uvs[t]
                p = pbs.pop(t)
                jk = junk.next()
                S.op("dve", lambda e: e.scalar_tensor_tensor(out=jk[:], in0=uv[:, 0:1024], scalar=1.0, in1=p[:], op0=ALU.mult, op1=ALU.mult,
                                                             accum_out=at[:, t:t + 1]), reads=[(uv, "u"), p], writes=[jk, (at, t)])
                S.op("act", lambda e: e.activation(out=c[:, t:t + 1], in_=at[:, t:t + 1], func=AF.Gelu_apprx_tanh),
                     reads=[(at, t)], writes=[(c, t)])

            def stage_b(t):
                uv = uvs.pop(t)
                z = zb.next()
                S.op("dve", lambda e: e.tensor_tensor(out=z[:, 127:128], in0=c[:, t:t + 1], in1=g[:, t:t + 1], op=ALU.mult),
                     reads=[(c, t), g], writes=[z])
                for half in range(2):
                    S.op("pe", lambda e: e.matmul(po[:, half * 512:(half + 1) * 512], lhsT=z[:, 127 - t:255 - t],
                                                  rhs=uv[:, 1024 + half * 512:1024 + (half + 1) * 512], start=(t == 0), stop=(t == 127)),
                         reads=[z, (uv, "v")], writes=[(po, half)])

            for s_i in range(128 + LAG + 1):
                if s_i < 128:
                    stage_a0(s_i)
                if 0 <= s_i - 1 < 128:
                    stage_a1(s_i - 1)
                if 0 <= s_i - 1 - LAG < 128:
                    stage_b(s_i - 1 - LAG)
            t_ = tm.next()
            S.op("dve", lambda e: e.tensor_tensor(out=t_[:], in0=po[:], in1=g2b[j][:], op=ALU.mult), reads=[po, g2b[j]], writes=[t_])
            S.op("dve", lambda e: e.scalar_tensor_tensor(out=t_[:], in0=x[:], scalar=ALPHA, in1=t_[:], op0=ALU.mult, op1=ALU.add),
                 reads=[x, t_], writes=[t_])
            t2 = tm.next(); s_ = sm.next(); o = xo.next()
            layer_norm(S, t_[:, :], [t_], 1024, lng, lnb, o, t2, s_)
            S.dma("sp", lambda e: e.dma_start(out=D[xout][r0:r0 + 128, :], in_=o[:]), reads=[o])


D_CONST = {}


def make_consts(S, D):
    mh = S.tile([128, 1], F32, "mhalf")
    S.op("pool", lambda e: e.memset(mh[:], -0.5), writes=[mh])
    D_CONST["mhalf"] = mh
    ib = S.tile([128, 128], BF16, "ident_bf")
    S.op("pool", lambda e: e.memset(ib[:], 0.0), writes=[ib])
    S.op("pool", lambda e: e.affine_select(out=ib[:], in_=ib[:], pattern=[[-1, 128]], compare_op=ALU.not_equal,
                                           fill=1.0, base=0, channel_multiplier=1), reads=[ib], writes=[ib])
    D["ident_bf"] = ib
    i32 = S.tile([128, 128], F32, "ident_f")
    S.op("pool", lambda e: e.memset(i32[:], 0.0), writes=[i32])
    S.op("pool", lambda e: e.affine_select(out=i32[:], in_=i32[:], pattern=[[-1, 128]], compare_op=ALU.not_equal,
                                           fill=1.0, base=0, channel_multiplier=1), reads=[i32], writes=[i32])
    D["ident_f"] = i32


INPUT_SPECS = {
    "xs": ([NTOK, 1024], F32),
    "cvecT": ([128, 8, 2], F32),
    "ada_w": ([2, 1024, 6144], F32),
    "ada_b": ([2, 6144], F32),
    "w_in": ([2, 1024, W_IN_COLS], F32),
    "w_branch": ([2, 1024, 1024], F32),
    "w_out": ([2, 1024, 1024], F32),
    "ln1_g": ([2, 1024], F32),
    "ln1_b": ([2, 1024], F32),
    "na_bias": ([2, 4, 8, 64, 512], F32),
    "ropeC": ([64, 64, 2, 8], F32),
    "ropeS": ([64, 64, 2, 8], F32),
    "gla_mt": ([64, 2, 64], F32),
    "gla_tri": ([64, 2, 64], F32),
    "gla_blk": ([128, 4, 64], F32),
    "gla_gw": ([2, 2, 33, 128], F32),
    "gla_norm_g": ([2, 256], F32),
    "peer_wq": ([2, 1024, 2048], F32),
    "peer_keysT": ([2, 16, 128, 128], F32),
    "peer_u0": ([16384, 1024], F32),
    "peer_u1": ([16384, 1024], F32),
    "peer_v0": ([16384, 1024], F32),
    "peer_v1": ([16384, 1024], F32),
    "ln2_g": ([2, 1024], F32),
    "ln2_b": ([2, 1024], F32),
    "iota_kk": ([128, 16, 16], F32),
    "conv_dwT": ([2, 256, 31], F32),
    "conv_bT": ([2, 128, 2], F32),
    "conv_ln_g": ([2, 256], F32),
    "conv_ln_b": ([2, 256], F32),
    "sgu_ln_g": ([2, 256], F32),
    "sgu_ln_b": ([2, 256], F32),
    "sgu_wsT": ([2, 4, 128, 128], F32),
    "sgu_bsT": ([2, 128, 4], F32),
}
SCRATCH_SPECS = {
    "modv": ([2, 2, 6144], F32),
    "z": ([NTOK, W_IN_COLS], F32),
    "ycat": ([NTOK, 1024], F32),
    "ofwd": ([NTOK, 256], F32),
    "pidx": ([NTOK, 128], I32),
    "pgt": ([NTOK, 128], F32),
    "xa": ([NTOK, 1024], F32),
    "peer_uvb0": ([16384, 2048], BF16),
    "peer_uvb1": ([16384, 2048], BF16),
    "out": ([SEQ, 1024], F32),
}


def build_program(plan, ext_in=(), ext_out=(), inputs=None):
    nc = bass.Bass("TRN2", target_bir_lowering=False)
    D = {}
    for name, (shape, dt) in INPUT_SPECS.items():
        if inputs is not None and name not in inputs:
            continue
        D[name] = nc.dram_tensor(name, shape, dt, kind="ExternalInput").ap()
    for name, (shape, dt) in SCRATCH_SPECS.items():
        kind = "ExternalInput" if name in ext_in else ("ExternalOutput" if name in ext_out else "Internal")
        D[name] = nc.dram_tensor(name, shape, dt, kind=kind).ap()
    with ExitStack() as st:
        S = Sync(nc, st)
        make_consts(S, D)
        plan(S, D)
        S.barrier()
    return nc


def na_bias_table(rpb):
    L = rpb.shape[0]
    W = 64
    col = np.arange(W)
    c0 = np.clip(col - 8, 0, W - 16)
    in_win = (col[None, :] >= c0[:, None]) & (col[None, :] < c0[:, None] + 16)
    dc = np.clip(col[None, :] - col[:, None], -15, 15) + 15
    out = np.empty((L, 4, 8, 64, 512), np.float32)
    reps = [0, 1, 2, 3, 30, 61, 62, 63]
    for ci, r in enumerate(reps):
        r0 = min(max(r - 4, 0), 56)
        for k in range(8):
            dr = r0 + k - r + 7
            b = rpb[:, :, dr][:, :, dc]
            out[:, :, ci, :, k * 64:(k + 1) * 64] = np.where(in_win[None, None], b, np.float32(-1e30))
    return out


def gla_consts():
    inv = (1.0 / (np.float32(100.0) ** (np.arange(8, dtype=np.float32) / np.float32(8)))).astype(np.float32)
    pos = np.arange(64, dtype=np.float32)
    ang = (pos[:, None] * inv[None, :]).astype(np.float32)
    C = np.cos(ang).astype(np.float32)
    Sn = np.sin(ang).astype(np.float32)
    ropeC = np.empty((64, 64, 2, 8), np.float32)
    ropeS = np.empty((64, 64, 2, 8), np.float32)
    ropeC[:, :, 0, :] = C[None, :, :]
    ropeS[:, :, 0, :] = Sn[None, :, :]
    ropeC[:, :, 1, :] = C[:, None, :]
    ropeS[:, :, 1, :] = Sn[:, None, :]
    s = np.arange(64)[:, None]
    c = np.arange(64)[None, :]
    tri = np.stack([(s <= c), (s >= c)], 1).astype(np.float32)
    mt = (tri * np.float32(-1.0 / 16)).astype(np.float32)
    blk = np.zeros((128, 4, 64), np.float32)
    for h in range(4):
        blk[h * 32:(h + 1) * 32, h, :] = 1
    return dict(ropeC=ropeC, ropeS=ropeS, gla_mt=mt, gla_tri=tri, gla_blk=blk)


def gla_gw_layout(gate_up, gate_b):
    L = gate_up.shape[0]
    out = np.zeros((L, 2, 33, 128), np.float32)
    for d in range(2):
        out[:, d, d * 16:(d + 1) * 16, :] = gate_up[:, d]
        out[:, d, 32, :] = gate_b[:, d]
    return out


def host_weights(inp):
    f = lambda a: np.ascontiguousarray(np.asarray(a, dtype=np.float32))
    W = {}
    for l in range(2):
        W["peer_u%d" % l] = f(np.asarray(inp["peer_u"])[l])
        W["peer_v%d" % l] = f(np.asarray(inp["peer_v"])[l])
    for k in ("ada_w", "ada_b", "w_in", "w_out", "ln1_g", "ln1_b", "peer_wq", "ln2_g", "ln2_b",
              "conv_ln_g", "conv_ln_b", "sgu_ln_g", "sgu_ln_b"):
        W[k] = f(inp[k])
    W["w_branch"] = f(np.asarray(inp["w_branch"]).reshape(2, 1024, 1024))
    W["na_bias"] = na_bias_table(f(inp["na_rpb"]))
    W.update(gla_consts())
    W["gla_gw"] = gla_gw_layout(f(inp["gla_gate_up"]), f(inp["gla_gate_b"]))
    W["gla_norm_g"] = f(np.asarray(inp["gla_norm_g"]).reshape(2, 256))
    W["conv_dwT"] = f(np.asarray(inp["conv_dw"]).transpose(0, 2, 1))
    W["conv_bT"] = f(np.asarray(inp["conv_b"]).reshape(2, 2, 128).transpose(0, 2, 1))
    W["sgu_wsT"] = f(np.asarray(inp["sgu_ws"]).transpose(0, 1, 3, 2))
    W["sgu_bsT"] = f(np.asarray(inp["sgu_bs"]).transpose(0, 2, 1))
    W["peer_keysT"] = f(np.asarray(inp["peer_keys"]).reshape(2, 16, 128, 128).transpose(0, 1, 3, 2))
    W["iota_kk"] = f(np.broadcast_to(np.arange(16, dtype=np.float32)[None, None, :], (128, 16, 16)))
    return W


def full_plan(S, D):
    cv = {}
    for l in range(DEPTH):
        need_ctx = l < DEPTH - 1
        tl = range(NTT) if need_ctx else range(32)
        xin = "xs" if l == 0 else "xa"
        phase_mod(S, D, l)
        phase_win(S, D, l, xin=xin, post_weights=(lambda l_=l: cv.__setitem__(l_, start_table_convert(S, D, l_))))
        phase_na(S, D, l, need_ctx=need_ctx)
        phase_gla(S, D, l, need_ctx=need_ctx)
        phase_conv(S, D, l, do_ctx=need_ctx)
        phase_sgu(S, D, l, tiles=tl)
        phase_merge(S, D, l, tiles=tl, xin=xin, xout="xa")
        phase_peer_topk(S, D, l, tiles=tl, xin="xa")
        phase_peer_ffn(S, D, l, tiles=tl, xin="xa", xout=("xa" if need_ctx else "out"), conv_vals=cv[l])


_CACHE = {}


def kernel(**inputs):
    x = np.asarray(inputs["x"], dtype=np.float32)
    c = np.asarray(inputs["c"], dtype=np.float32)
    ctx = np.asarray(inputs["ctx"], dtype=np.float32)
    c_ctx = np.asarray(inputs["c_ctx"], dtype=np.float32)
    B = x.shape[0]
    W = host_weights(inputs)
    if "nc" not in _CACHE:
        _CACHE["nc"] = build_program(full_plan, ext_out=("out",))
    nc = _CACHE["nc"]
    in_maps = []
    for b in range(B):
        m = dict(W)
        m["xs"] = np.ascontiguousarray(np.concatenate([x[b], ctx[b]], 0))
        cvec = np.stack([c[b], c_ctx], 0)
        m["cvecT"] = np.ascontiguousarray(cvec.reshape(2, 8, 128).transpose(2, 1, 0))
        in_maps.append(m)
    res = run_bass_kernel_spmd(nc, in_maps, core_ids=list(range(B)))
    return np.stack([np.asarray(r["out"], dtype=np.float32) for r in res.results], 0)
```

```python
import numpy as np
from contextlib import ExitStack, contextmanager
import concourse.bass as bass
import concourse.mybir as mybir
from concourse.bass_utils import run_bass_kernel_spmd

F32 = mybir.dt.float32
BF16 = mybir.dt.bfloat16
I32 = mybir.dt.int32
U32 = mybir.dt.uint32
AF = mybir.ActivationFunctionType
ALU = mybir.AluOpType
AX = mybir.AxisListType

D_MODEL = 1024
SEQ = 4096
CTX = 256
NTOK = SEQ + CTX
NTT = NTOK // 128
DEPTH = 2
W_IN_COLS = 6688
ALPHA = (2 * DEPTH) ** 0.25
LN_EPS = 1e-6
C_QKV, C_GQ, C_GK, C_GV, C_GR, C_GLO, C_CONV, C_SGU, C_GATE = 0, 768, 896, 1024, 1280, 1536, 1568, 2080, 2592


class Buf:
    def __init__(self, h):
        self.h = h
        self.st = {}

    def __getitem__(self, idx):
        return self.h[idx]


class Sync:
    NDMA = 8

    def __init__(self, nc, stack):
        self.nc = nc
        self.stack = stack
        self.E = {}
        self.semobj = {}
        for name, eng in (("pe", nc.tensor), ("act", nc.scalar), ("dve", nc.vector),
                          ("pool", nc.gpsimd), ("sp", nc.sync)):
            sem = stack.enter_context(nc.semaphore("s_" + name))
            self.E[name] = dict(eng=eng, sem=sem, cnt=0, seen={}, dq=[], dn=0)
            self.semobj[id(sem)] = sem
        for q in ("sp", "pool", "act"):
            e = self.E[q]
            e["dq"] = [stack.enter_context(nc.semaphore("d_%s%d" % (q, i))) for i in range(self.NDMA)]
            for s in e["dq"]:
                self.semobj[id(s)] = s
        self.bg = [stack.enter_context(nc.semaphore("bg%d" % i)) for i in range(4)]
        for b in self.bg:
            self.semobj[id(b)] = b
        self.pending_dma = {}
        self.cur = stack
        self.uid = 0

    def tile(self, shape, dt, name=None):
        self.uid += 1
        return Buf(self.cur.enter_context(self.nc.sbuf_tensor("%s_%d" % (name or "t", self.uid), list(shape), dt)))

    def psum(self, shape, dt=F32, name=None):
        self.uid += 1
        return Buf(self.cur.enter_context(self.nc.psum_tensor("%s_%d" % (name or "p", self.uid), list(shape), dt)))

    @contextmanager
    def phase(self):
        prev = self.cur
        with ExitStack() as st:
            self.cur = st
            yield
            self.barrier()
        self.cur = prev

    @staticmethod
    def _merge(out, d):
        for k, v in d.items():
            if out.get(k, 0) < v:
                out[k] = v

    def _deps(self, reads, writes):
        out = {}
        for b, key in reads:
            keys = list(b.st.keys()) if key is None else [key, None]
            for k in keys:
                st = b.st.get(k)
                if st:
                    self._merge(out, st[0])
        for b, key in writes:
            keys = list(b.st.keys()) if key is None else [key, None]
            for k in keys:
                st = b.st.get(k)
                if st:
                    self._merge(out, st[0])
                    self._merge(out, st[1])
        return out

    def _wait(self, ename, deps):
        e = self.E[ename]
        own = id(e["sem"])
        for sid, val in deps.items():
            if ename == "pe" and sid == own:
                continue
            if e["seen"].get(sid, 0) >= val:
                continue
            e["eng"].wait_ge(self.semobj[sid], val)
            e["seen"][sid] = val

    def _mark(self, reads, writes, sid, val):
        for b, key in reads:
            st = b.st.setdefault(key, [{}, {}])
            if st[1].get(sid, 0) < val:
                st[1][sid] = val
        for b, key in writes:
            if key is None:
                b.st = {None: [{sid: val}, {}]}
            else:
                b.st[key] = [{sid: val}, {}]

    @staticmethod
    def _norm(lst):
        return [(x, None) if isinstance(x, Buf) else x for x in lst]

    def op(self, ename, fn, reads=(), writes=()):
        reads = self._norm(reads)
        writes = self._norm(writes)
        e = self.E[ename]
        self._wait(ename, self._deps(reads, writes))
        ins = fn(e["eng"])
        e["cnt"] += 1
        ins.then_inc(e["sem"], 1)
        self._mark(reads, writes, id(e["sem"]), e["cnt"])
        return ins

    def dma(self, qname, fn, reads=(), writes=()):
        reads = self._norm(reads)
        writes = self._norm(writes)
        e = self.E[qname]
        slot = e["dn"] % self.NDMA
        val = (e["dn"] // self.NDMA + 1) * 16
        sem = e["dq"][slot]
        deps = self._deps(reads, writes)
        if val > 16:
            self._merge(deps, {id(sem): val - 16})
        self._wait(qname, deps)
        ins = fn(e["eng"])
        ins.then_inc(sem, 16)
        e["dn"] += 1
        self._mark(reads, writes, id(sem), val)
        self._merge(self.pending_dma, {id(sem): val})
        return ins

    def bg_dma(self, qname, fn, sem, n_prev):
        e = self.E[qname]
        ins = fn(e["eng"])
        ins.then_inc(sem, 16)
        return (n_prev + 1) * 16

    def wait_sem(self, enames, sem, val):
        for n in enames:
            self._wait(n, {id(sem): val})

    def barrier(self):
        allv = dict(self.pending_dma)
        for n, e in self.E.items():
            if e["cnt"]:
                allv[id(e["sem"])] = e["cnt"]
        for n in self.E:
            self._wait(n, allv)

    def load(self, t, src, q="sp", key=None):
        return self.dma(q, lambda e: e.dma_start(out=t, in_=src), writes=[(self._b, key)] if False else [])


class Rot:
    def __init__(self, S, n, shape, dt, psum=False, name=None):
        self.bufs = [(S.psum(shape, dt, name) if psum else S.tile(shape, dt, name)) for _ in range(n)]
        self.i = 0

    def next(self):
        b = self.bufs[self.i % len(self.bufs)]
        self.i += 1
        return b


def phase_mod(S, D, l):
    with S.phase():
        cT = S.tile([128, 8, 2], F32, "cT")
        cs = S.tile([128, 8, 2], F32, "cs")
        ones = S.tile([1, 2], F32, "ones")
        ab = S.tile([1, 6144], F32, "ab")
        wa = Rot(S, 4, [128, 8, 512], F32, name="wa")
        pm = Rot(S, 4, [2, 512], F32, psum=True, name="pm")
        mr = Rot(S, 4, [2, 512], F32, name="mr")
        S.dma("sp", lambda e: e.dma_start(out=cT[:], in_=D["cvecT"]), writes=[cT])
        S.dma("sp", lambda e: e.dma_start(out=ab[:], in_=D["ada_b"][l:l + 1, :]), writes=[ab])
        S.op("dve", lambda e: e.memset(ones[:], 1.0), writes=[ones])
        S.op("act", lambda e: e.activation(out=cs[:], in_=cT[:], func=AF.Silu), reads=[cT], writes=[cs])
        aw = D["ada_w"][l].rearrange("(k p) n -> p k n", p=128)
        wl = {}

        def ld(n):
            w_ = wa.next()
            S.dma("sp", lambda e: e.dma_start(out=w_[:], in_=aw[:, :, n * 512:(n + 1) * 512]), writes=[w_])
            wl[n] = w_

        for n in range(3):
            ld(n)
        for n in range(12):
            if n + 3 < 12:
                ld(n + 3)
            w = wl.pop(n)
            p = pm.next()
            for k in range(8):
                S.op("pe", lambda e: e.matmul(p[:], lhsT=cs[:, k, :], rhs=w[:, k, :], start=(k == 0), stop=False),
                     reads=[cs, w], writes=[p])
            S.op("pe", lambda e: e.matmul(p[:], lhsT=ones[:], rhs=ab[:, n * 512:(n + 1) * 512], start=False, stop=True),
                 reads=[ones, ab], writes=[p])
            m = mr.next()
            plus1 = 1.0 if n in (2, 3, 8, 9) else 0.0
            S.op("dve", lambda e: e.tensor_scalar(out=m[:], in0=p[:], scalar1=plus1, scalar2=None, op0=ALU.add),
                 reads=[p], writes=[m])
            S.dma("sp", lambda e: e.dma_start(out=D["modv"][l, :, n * 512:(n + 1) * 512], in_=m[:]), reads=[m])


def bcast_load(S, D, l, j, c0, n, name):
    t = S.tile([128, n], F32, name)
    S.dma("sp", lambda e: e.dma_start(out=t[:], in_=D["modv"][l, j, c0:c0 + n].partition_broadcast(128)), writes=[t])
    return t


def vec_bcast(S, src, n, name):
    t = S.tile([128, n], F32, name)
    S.dma("sp", lambda e: e.dma_start(out=t[:], in_=src.partition_broadcast(128)), writes=[t])
    return t


def phase_win(S, D, l, tiles=range(NTT), xin="xs", post_weights=None, pre=None):
    with S.phase():
        wb = S.tile([128, 8, W_IN_COLS], BF16, "wb")
        wv = D["w_in"][l].rearrange("(k p) n -> p k n", p=128)
        for k in range(8):
            S.dma("pool", lambda e: e.dma_start(out=wb[:, k, :], in_=wv[:, k, :]), writes=[(wb, k)])
        if post_weights is not None:
            post_weights()
        if pre is not None:
            pre()
        scb = [bcast_load(S, D, l, j, 1024, 1024, "scb") for j in range(2)]
        shb = [bcast_load(S, D, l, j, 0, 1024, "shb") for j in range(2)]
        xt = Rot(S, 3, [128, 1024], F32, name="xt")
        hf = Rot(S, 2, [128, 1024], F32, name="hf")
        hb = Rot(S, 2, [128, 1024], BF16, name="hb")
        hT = Rot(S, 3, [128, 8, 128], BF16, name="hT")
        pT = Rot(S, 2, [128, 8, 128], BF16, psum=True, name="pT")
        pz = Rot(S, 4, [128, 512], F32, psum=True, name="pz")
        zs = Rot(S, 3, [128, 2048], F32, name="zs")
        ident = D["ident_bf"]
        cnt = 0
        def st_l(tt):
            j = 0 if tt < 32 else 1
            x = xt.next()
            S.dma("sp", lambda e: e.dma_start(out=x[:], in_=D[xin][tt * 128:(tt + 1) * 128, :]), writes=[x])
            return dict(tt=tt, j=j, x=x)

        def st_a(c_):
            tt, j, x = c_["tt"], c_["j"], c_["x"]
            h1 = hf.next()
            S.op("dve", lambda e: e.tensor_tensor(out=h1[:], in0=x[:], in1=scb[j][:], op=ALU.mult), reads=[x, scb[j]], writes=[h1])
            h2 = hb.next()
            S.op("dve", lambda e: e.tensor_tensor(out=h2[:], in0=h1[:], in1=shb[j][:], op=ALU.add), reads=[h1, shb[j]], writes=[h2])
            p = pT.next()
            for k in range(8):
                S.op("pe", lambda e: e.transpose(out=p[:, k, :], in_=h2[:, k * 128:(k + 1) * 128], identity=ident[:]),
                     reads=[h2, ident], writes=[(p, k)])
            t = hT.next()
            S.op("act", lambda e: e.copy(out=t[:], in_=p[:]), reads=[p], writes=[t])
            c_.update(t=t)

        def st_b(c_):
            nonlocal cnt
            tt, t = c_["tt"], c_["t"]
            for g0 in range(0, W_IN_COLS, 2048):
                gw = min(2048, W_IN_COLS - g0)
                zt = zs.next()
                for c0 in range(g0, g0 + gw, 512):
                    cw = min(512, W_IN_COLS - c0)
                    pp = pz.next()
                    for k in range(8):
                        S.op("pe", lambda e: e.matmul(pp[:, :cw], lhsT=t[:, k, :], rhs=wb[:, k, c0:c0 + cw],
                                                      start=(k == 0), stop=(k == 7)), reads=[t, wb], writes=[pp])
                    eng = "act" if cnt % 2 == 0 else "dve"
                    cnt += 1
                    if eng == "act":
                        S.op("act", lambda e: e.copy(out=zt[:, c0 - g0:c0 - g0 + cw], in_=pp[:, :cw]), reads=[pp], writes=[(zt, c0)])
                    else:
                        S.op("dve", lambda e: e.tensor_copy(out=zt[:, c0 - g0:c0 - g0 + cw], in_=pp[:, :cw]), reads=[pp], writes=[(zt, c0)])
                S.dma("sp", lambda e: e.dma_start(out=D["z"][tt * 128:(tt + 1) * 128, g0:g0 + gw], in_=zt[:, :gw]), reads=[zt])

        tiles = list(tiles)
        st_ = {}
        nj = len(tiles)
        for i in range(nj + 2):
            if i < nj:
                st_[i] = st_l(tiles[i])
            if 0 <= i - 1 < nj:
                st_a(st_[i - 1])
            if 0 <= i - 2 < nj:
                st_b(st_.pop(i - 2))


def layer_norm(S, xin, xdeps, n, gb, bb, out, tmp, small):
    nch = (n + 511) // 512
    for i in range(nch):
        w = min(512, n - i * 512)
        S.op("dve", lambda e: e.bn_stats(out=small[:, 8 + 6 * i:8 + 6 * i + 6], in_=xin[:, i * 512:i * 512 + w]),
             reads=xdeps, writes=[(small, "st%d" % i)])
    S.op("dve", lambda e: e.bn_aggr(out=small[:, 0:2], in_=small[:, 8:8 + 6 * nch]), reads=[small], writes=[(small, "mv")])
    mh = D_CONST["mhalf"]
    S.op("pool", lambda e: e.tensor_scalar(out=small[:, 2:3], in0=small[:, 1:2], scalar1=LN_EPS, scalar2=None, op0=ALU.add),
         reads=[(small, "mv")], writes=[(small, "ve")])
    S.op("pool", lambda e: e.tensor_tensor(out=small[:, 4:5], in0=small[:, 2:3], in1=mh[:, 0:1], op=ALU.pow),
         reads=[(small, "ve"), mh], writes=[(small, "rs")])
    S.op("dve", lambda e: e.tensor_scalar(out=tmp[:, :n], in0=xin, scalar1=small[:, 0:1], scalar2=small[:, 4:5],
                                          op0=ALU.subtract, op1=ALU.mult), reads=list(xdeps) + [small], writes=[tmp])
    S.op("pool", lambda e: e.tensor_tensor(out=tmp[:, :n], in0=tmp[:, :n], in1=gb[:, :n], op=ALU.mult), reads=[tmp, gb], writes=[tmp])
    S.op("pool", lambda e: e.tensor_tensor(out=out[:, :n], in0=tmp[:, :n], in1=bb[:, :n], op=ALU.add), reads=[tmp, bb], writes=[out])


def phase_conv(S, D, l, do_ctx=True):
    PAD = 15
    with S.phase():
        dw = S.tile([128, 2, 31], F32, "dw")
        cb = S.tile([128, 2], F32, "cb")
        S.dma("sp", lambda e: e.dma_start(out=dw[:], in_=D["conv_dwT"][l].rearrange("(c p) j -> p c j", p=128)), writes=[dw])
        S.dma("sp", lambda e: e.dma_start(out=cb[:], in_=D["conv_bT"][l]), writes=[cb])
        lng = vec_bcast(S, D["conv_ln_g"][l], 256, "lng")
        lnb = vec_bcast(S, D["conv_ln_b"][l], 256, "lnb")
        identf = D["ident_f"]
        yT = S.tile([128, 2, PAD + SEQ + PAD], F32, "yT")
        cv = S.tile([128, 2, SEQ], F32, "cv")
        zt = Rot(S, 4, [128, 512], F32, name="zt")
        sg = Rot(S, 4, [128, 256], F32, name="sg")
        yy = Rot(S, 4, [128, 256], F32, name="yy")
        pcs = Rot(S, 4, [128, 256], F32, name="pcs")
        pt = Rot(S, 2, [128, 2, 128], F32, psum=True, name="pt")
        pb = Rot(S, 2, [128, 256], F32, psum=True, name="pb")
        tmp = Rot(S, 2, [128, 256], F32, name="tmp")
        sm = Rot(S, 2, [128, 32], F32, name="sm")
        ln = Rot(S, 2, [128, 256], F32, name="ln")
        yo = Rot(S, 2, [128, 256], F32, name="yo")
        for (base, T) in ((0, SEQ), (SEQ, CTX)) if do_ctx else ((0, SEQ),):
            S.op("pool", lambda e: e.memset(yT[:, :, 0:PAD], 0.0), writes=[(yT, "padl")])
            S.op("pool", lambda e: e.memset(yT[:, :, PAD + T:PAD + T + PAD], 0.0), writes=[(yT, "padr")])
            ys_ = {}

            def a_front(i):
                r0 = base + i * 128
                z = zt.next()
                S.dma("sp", lambda e: e.dma_start(out=z[:], in_=D["z"][r0:r0 + 128, C_CONV:C_CONV + 512]), writes=[z])
                s = sg.next()
                S.op("act", lambda e: e.activation(out=s[:], in_=z[:, 256:512], func=AF.Sigmoid), reads=[z], writes=[s])
                y = yy.next()
                S.op("dve", lambda e: e.tensor_tensor(out=y[:], in0=z[:, 0:256], in1=s[:], op=ALU.mult), reads=[z, s], writes=[y])
                ys_[i] = y

            NT_ = T // 128
            for i in range(min(2, NT_)):
                a_front(i)
            for i in range(NT_):
                if i + 2 < NT_:
                    a_front(i + 2)
                y = ys_.pop(i)
                p = pt.next()
                for c in range(2):
                    S.op("pe", lambda e: e.transpose(out=p[:, c, :], in_=y[:, c * 128:(c + 1) * 128], identity=identf[:]),
                         reads=[y, identf], writes=[(p, c)])
                S.op("act", lambda e: e.copy(out=yT[:, :, PAD + i * 128:PAD + (i + 1) * 128], in_=p[:]), reads=[p], writes=[(yT, i)])
            CH = 1024 if T >= 1024 else T
            for t0 in range(0, T, CH):
                for c in range(2):
                    key = ("cv", t0, c)
                    S.op("dve", lambda e: e.tensor_scalar(out=cv[:, c, t0:t0 + CH], in0=yT[:, c, t0:t0 + CH], scalar1=dw[:, c, 0:1],
                                                          scalar2=cb[:, c:c + 1], op0=ALU.mult, op1=ALU.add),
                         reads=[yT, dw, cb], writes=[(cv, key)])
                    for j in range(1, 31):
                        S.op("dve", lambda e: e.scalar_tensor_tensor(out=cv[:, c, t0:t0 + CH], in0=yT[:, c, t0 + j:t0 + j + CH],
                                                                     scalar=dw[:, c, j:j + 1], in1=cv[:, c, t0:t0 + CH],
                                                                     op0=ALU.mult, op1=ALU.add),
                             reads=[yT, dw, (cv, key)], writes=[(cv, key)])
            ps_ = {}

            def c_front(i):
                p_ = pb.next()
                for c in range(2):
                    S.op("pe", lambda e: e.transpose(out=p_[:, c * 128:(c + 1) * 128], in_=cv[:, c, i * 128:(i + 1) * 128], identity=identf[:]),
                         reads=[cv, identf], writes=[(p_, c)])
                pc_ = pcs.next()
                S.op("act", lambda e: e.copy(out=pc_[:], in_=p_[:]), reads=[p_], writes=[pc_])
                ps_[i] = pc_

            for i in range(min(2, NT_)):
                c_front(i)
            for i in range(NT_):
                if i + 2 < NT_:
                    c_front(i + 2)
                r0 = base + i * 128
                p = ps_.pop(i)
                t = tmp.next(); s = sm.next(); o = ln.next()
                layer_norm(S, p[:, :], [p], 256, lng, lnb, o, t, s)
                y = yo.next()
                S.op("act", lambda e: e.activation(out=y[:], in_=o[:], func=AF.Silu), reads=[o], writes=[y])
                S.dma("sp", lambda e: e.dma_start(out=D["ycat"][r0:r0 + 128, 512:768], in_=y[:]), reads=[y])
            S.barrier()


def phase_sgu(S, D, l, tiles=range(NTT)):
    with S.phase():
        ws = S.tile([128, 4, 128], F32, "ws")
        bs = S.tile([128, 4], F32, "bs")
        S.dma("sp", lambda e: e.dma_start(out=ws[:], in_=D["sgu_wsT"][l].rearrange("g q p -> q g p")), writes=[ws])
        S.dma("sp", lambda e: e.dma_start(out=bs[:], in_=D["sgu_bsT"][l]), writes=[bs])
        lng = vec_bcast(S, D["sgu_ln_g"][l], 256, "lng")
        lnb = vec_bcast(S, D["sgu_ln_b"][l], 256, "lnb")
        zt = Rot(S, 4, [128, 512], F32, name="zt")
        ge = Rot(S, 4, [128, 512], F32, name="ge")
        tmp = Rot(S, 2, [128, 256], F32, name="tmp")
        sm = Rot(S, 2, [128, 32], F32, name="sm")
        vn = Rot(S, 2, [128, 256], F32, name="vn")
        ps = Rot(S, 2, [128, 256], F32, psum=True, name="ps")
        sb = Rot(S, 2, [128, 256], F32, name="sb")
        yo = Rot(S, 2, [128, 256], F32, name="yo")
        def st_a(tt):
            r0 = tt * 128
            z = zt.next()
            S.dma("sp", lambda e: e.dma_start(out=z[:], in_=D["z"][r0:r0 + 128, C_SGU:C_SGU + 512]), writes=[z])
            g = ge.next()
            S.op("act", lambda e: e.activation(out=g[:], in_=z[:], func=AF.Gelu_apprx_tanh), reads=[z], writes=[g])
            return (r0, g)

        tiles = list(tiles)
        pend = {}
        for i in range(len(tiles) + 2):
            if i < len(tiles):
                pend[i] = st_a(tiles[i])
            if i - 2 < 0:
                continue
            r0, g = pend.pop(i - 2)
            t = tmp.next(); s = sm.next(); v = vn.next()
            layer_norm(S, g[:, 256:512], [g], 256, lng, lnb, v, t, s)
            p = ps.next()
            for gi in range(4):
                S.op("pe", lambda e: e.matmul(p[:, gi * 64:(gi + 1) * 64], lhsT=ws[:, gi, :], rhs=v[:, gi * 64:(gi + 1) * 64],
                                              start=True, stop=True), reads=[ws, v], writes=[(p, gi)])
            sbt = sb.next()
            S.op("dve", lambda e: e.tensor_tensor(out=sbt[:].rearrange("p (g c) -> p g c", g=4), in0=p[:].rearrange("p (g c) -> p g c", g=4),
                                                  in1=bs[:].unsqueeze(2).to_broadcast([128, 4, 64]), op=ALU.add),
                 reads=[p, bs], writes=[sbt])
            y = yo.next()
            S.op("dve", lambda e: e.tensor_tensor(out=y[:], in0=sbt[:], in1=g[:, 0:256], op=ALU.mult), reads=[sbt, g], writes=[y])
            S.dma("sp", lambda e: e.dma_start(out=D["ycat"][r0:r0 + 128, 768:1024], in_=y[:]), reads=[y])


def phase_merge(S, D, l, tiles=range(NTT), xin="xs", xout="xs"):
    with S.phase():
        wbr = S.tile([128, 8, 1024], BF16, "wbr")
        wo = S.tile([128, 8, 1024], BF16, "wo")
        S.dma("pool", lambda e: e.dma_start(out=wbr[:], in_=D["w_branch"][l].rearrange("(k p) n -> p k n", p=128)), writes=[wbr])
        S.dma("pool", lambda e: e.dma_start(out=wo[:], in_=D["w_out"][l].rearrange("(k p) n -> p k n", p=128)), writes=[wo])
        g1b = [bcast_load(S, D, l, j, 2048, 1024, "g1b") for j in range(2)]
        lng = vec_bcast(S, D["ln1_g"][l], 1024, "lng")
        lnb = vec_bcast(S, D["ln1_b"][l], 1024, "lnb")
        ident = D["ident_bf"]
        yt = Rot(S, 3, [128, 1024], F32, name="yt")
        yb = Rot(S, 2, [128, 1024], BF16, name="yb")
        yT = Rot(S, 2, [128, 8, 128], BF16, name="yT")
        gt = Rot(S, 3, [128, 4096], F32, name="gt")
        sg = Rot(S, 2, [128, 1024], F32, name="sg")
        tm = Rot(S, 2, [128, 1024], F32, name="tm")
        mg = Rot(S, 1, [128, 1024], F32, name="mg")
        mb = Rot(S, 3, [128, 1024], BF16, name="mb")
        mT = Rot(S, 2, [128, 8, 128], BF16, name="mT")
        xt = Rot(S, 4, [128, 1024], F32, name="xt")
        rr = Rot(S, 2, [128, 1024], F32, name="rr")
        sm = Rot(S, 2, [128, 32], F32, name="sm")
        xo = Rot(S, 2, [128, 1024], F32, name="xo")
        pT = Rot(S, 2, [128, 8, 128], BF16, psum=True, name="pT")
        pP = Rot(S, 2, [128, 1024], F32, psum=True, name="pP")
        pO = Rot(S, 1, [128, 1024], F32, psum=True, name="pO")
        def st_a(tt):
            j = 0 if tt < 32 else 1
            r0 = tt * 128
            y = yt.next()
            S.dma("sp", lambda e: e.dma_start(out=y[:], in_=D["ycat"][r0:r0 + 128, :]), writes=[y])
            g = gt.next()
            S.dma("sp", lambda e: e.dma_start(out=g[:], in_=D["z"][r0:r0 + 128, C_GATE:C_GATE + 4096]), writes=[g])
            x = xt.next()
            S.dma("sp", lambda e: e.dma_start(out=x[:], in_=D[xin][r0:r0 + 128, :]), writes=[x])
            return dict(j=j, r0=r0, y=y, g=g, x=x)

        def st_b(c_):
            j, r0, y, g, x = (c_[k] for k in ("j", "r0", "y", "g", "x"))
            b = yb.next()
            S.op("dve", lambda e: e.tensor_copy(out=b[:], in_=y[:]), reads=[y], writes=[b])
            p = pT.next()
            for k in range(8):
                S.op("pe", lambda e: e.transpose(out=p[:, k, :], in_=b[:, k * 128:(k + 1) * 128], identity=ident[:]),
                     reads=[b, ident], writes=[(p, k)])
            t = yT.next()
            S.op("act", lambda e: e.copy(out=t[:], in_=p[:]), reads=[p], writes=[t])
            m = mg.next()
            mbt = mb.next()
            for i in range(4):
                pp = pP.next()
                for half in range(2):
                    for kk in range(2):
                        S.op("pe", lambda e: e.matmul(pp[:, half * 512:(half + 1) * 512], lhsT=t[:, i * 2 + kk, :],
                                                      rhs=wbr[:, i * 2 + kk, half * 512:(half + 1) * 512], start=(kk == 0), stop=(kk == 1)),
                             reads=[t, wbr], writes=[(pp, half)])
                s = sg.next()
                S.op("act", lambda e: e.activation(out=s[:], in_=g[:, i * 1024:(i + 1) * 1024], func=AF.Sigmoid), reads=[g], writes=[s])
                if i == 0:
                    S.op("dve", lambda e: e.tensor_tensor(out=m[:], in0=pp[:], in1=s[:], op=ALU.mult), reads=[pp, s], writes=[m])
                else:
                    tmp = tm.next()
                    S.op("dve", lambda e: e.tensor_tensor(out=tmp[:], in0=pp[:], in1=s[:], op=ALU.mult), reads=[pp, s], writes=[tmp])
                    dst = mbt if i == 3 else m
                    S.op("pool", lambda e: e.tensor_tensor(out=dst[:], in0=m[:], in1=tmp[:], op=ALU.add), reads=[m, tmp], writes=[dst])
            c_.update(mbt=mbt)

        def st_c(c_):
            j, r0, x, mbt = (c_[k] for k in ("j", "r0", "x", "mbt"))
            p = pT.next()
            for k in range(8):
                S.op("pe", lambda e: e.transpose(out=p[:, k, :], in_=mbt[:, k * 128:(k + 1) * 128], identity=ident[:]),
                     reads=[mbt, ident], writes=[(p, k)])
            t2 = mT.next()
            S.op("act", lambda e: e.copy(out=t2[:], in_=p[:]), reads=[p], writes=[t2])
            po = pO.next()
            for half in range(2):
                for k in range(8):
                    S.op("pe", lambda e: e.matmul(po[:, half * 512:(half + 1) * 512], lhsT=t2[:, k, :],
                                                  rhs=wo[:, k, half * 512:(half + 1) * 512], start=(k == 0), stop=(k == 7)),
                         reads=[t2, wo], writes=[(po, half)])
            r = rr.next()
            S.op("dve", lambda e: e.tensor_tensor(out=r[:], in0=po[:], in1=g1b[j][:], op=ALU.mult), reads=[po, g1b[j]], writes=[r])
            S.op("dve", lambda e: e.scalar_tensor_tensor(out=r[:], in0=x[:], scalar=ALPHA, in1=r[:], op0=ALU.mult, op1=ALU.add),
                 reads=[x, r], writes=[r])
            tmp = tm.next(); s = sm.next(); o = xo.next()
            layer_norm(S, r[:, :], [r], 1024, lng, lnb, o, tmp, s)
            S.dma("sp", lambda e: e.dma_start(out=D[xout][r0:r0 + 128, :], in_=o[:]), reads=[o])

        tiles = list(tiles)
        st_ = {}
        nj = len(tiles)
        for i in range(nj + 2):
            if i < nj:
                st_[i] = st_a(tiles[i])
            if 0 <= i - 1 < nj:
                st_b(st_[i - 1])
            if 0 <= i - 2 < nj:
                st_c(st_.pop(i - 2))


def phase_na(S, D, l, need_ctx=True, rows=range(64), heads=range(4)):
    scale = 64 ** -0.5
    with S.phase():
        ident = D["ident_bf"]
        qkT = S.tile([128, 4, NTOK], BF16, "qkT")
        va = S.tile([128, 32, 256], BF16, "va")
        vb = S.tile([128, 31, 256], BF16, "vb")
        vc = S.tile([128, 2, 256], BF16, "vc")
        zv = D["z"]
        S.dma("pool", lambda e: e.dma_start(out=va[:], in_=zv[0:4096, 512:768].rearrange("(t p) c -> p t c", p=128)), writes=[va])
        S.dma("pool", lambda e: e.dma_start(out=vb[:], in_=zv[64:64 + 31 * 128, 512:768].rearrange("(t p) c -> p t c", p=128)), writes=[vb])
        S.dma("pool", lambda e: e.dma_start(out=vc[:], in_=zv[4096:4352, 512:768].rearrange("(t p) c -> p t c", p=128)), writes=[vc])
        with S.phase():
            qk = Rot(S, 3, [128, 512], BF16, name="qk")
            pq = Rot(S, 2, [128, 4, 128], BF16, psum=True, name="pq")
            for tt in range(NTT):
                t = qk.next()
                S.dma("pool", lambda e: e.dma_start(out=t[:], in_=zv[tt * 128:(tt + 1) * 128, 0:512]), writes=[t])
                p = pq.next()
                for c in range(4):
                    S.op("pe", lambda e: e.transpose(out=p[:, c, :], in_=t[:, c * 128:(c + 1) * 128], identity=ident[:]),
                         reads=[t, ident], writes=[(p, c)])
                S.op("act", lambda e: e.copy(out=qkT[:, :, tt * 128:(tt + 1) * 128], in_=p[:]), reads=[p], writes=[(qkT, tt)])
        bias = Rot(S, 4, [64, 8, 512], F32, name="bias")
        ps1 = Rot(S, 2, [128, 512], F32, psum=True, name="ps1")
        ps2 = Rot(S, 2, [128, 256], F32, psum=True, name="ps2")
        ppT = Rot(S, 2, [128, 6, 128], BF16, psum=True, name="ppT")
        po = Rot(S, 2, [128, 64], F32, psum=True, name="po")
        sc = Rot(S, 3, [128, 768], F32, name="sc")
        pe_ = Rot(S, 3, [128, 768], BF16, name="pexp")
        pT = Rot(S, 3, [128, 6, 128], BF16, name="pT")
        sm = Rot(S, 6, [128, 4], F32, name="sm")
        yo = Rot(S, 4, [128, 64], F32, name="yo")

        def att_s1(h, M, qcols, kwin, cls, vchunks, bt, out_ap):
            hp, pb = h // 2, (h % 2) * 64
            qTs = qkT[pb:pb + 64, hp, qcols[0]:qcols[1]]
            s = sc.next(); nk = 0
            if kwin is not None:
                p1 = ps1.next()
                S.op("pe", lambda e: e.matmul(p1[:M, :], lhsT=qTs, rhs=qkT[pb:pb + 64, 2 + hp, kwin:kwin + 512], start=True, stop=True),
                     reads=[qkT], writes=[p1])
                S.op("dve", lambda e: e.scalar_tensor_tensor(out=s[:M, 0:512], in0=p1[:M, :], scalar=scale, in1=bt[:M, cls, :],
                                                             op0=ALU.mult, op1=ALU.add), reads=[p1, bt], writes=[(s, "w")])
                nk = 512
            p2 = ps2.next()
            S.op("pe", lambda e: e.matmul(p2[:M, :], lhsT=qTs, rhs=qkT[pb:pb + 64, 2 + hp, 4096:4352], start=True, stop=True),
                 reads=[qkT], writes=[p2])
            S.op("act", lambda e: e.mul(out=s[:M, nk:nk + 256], in_=p2[:M, :], mul=scale), reads=[p2], writes=[(s, "c")])
            nk += 256
            m = sm.next()
            S.op("dve", lambda e: e.tensor_reduce(out=m[:M, 0:1], in_=s[:M, :nk], axis=AX.X, op=ALU.max, negate=True),
                 reads=[s], writes=[(m, 0)])
            return dict(h=h, M=M, vchunks=vchunks, out_ap=out_ap, s=s, m=m, nk=nk)

        def att_s2(c_):
            M, s, m, nk = c_["M"], c_["s"], c_["m"], c_["nk"]
            pe = pe_.next()
            S.op("act", lambda e: e.activation(out=pe[:M, :nk], in_=s[:M, :nk], func=AF.Exp, bias=m[:M, 0:1], scale=1.0,
                                               accum_out=m[:M, 1:2]), reads=[s, (m, 0)], writes=[pe, (m, 1)])
            nch = nk // 128
            pp = ppT.next()
            for kc in range(nch):
                S.op("pe", lambda e: e.transpose(out=pp[:, kc, :M], in_=pe[:M, kc * 128:(kc + 1) * 128], identity=ident[:M, :M]),
                     reads=[pe, ident], writes=[(pp, kc)])
            pt = pT.next()
            S.op("dve", lambda e: e.tensor_copy(out=pt[:, :nch, :M], in_=pp[:, :nch, :M]), reads=[pp], writes=[pt])
            c_.update(pt=pt, nch=nch)

        def att_s3(c_):
            h, M, vchunks, out_ap, m, pt, nch = (c_[k] for k in ("h", "M", "vchunks", "out_ap", "m", "pt", "nch"))
            o = po.next()
            for kc in range(nch):
                vbuf, vi = vchunks[kc]
                S.op("pe", lambda e: e.matmul(o[:M, :], lhsT=pt[:, kc, :M], rhs=vbuf[:, vi, h * 64:(h + 1) * 64],
                                              start=(kc == 0), stop=(kc == nch - 1)), reads=[pt, vbuf], writes=[o])
            S.op("dve", lambda e: e.reciprocal(out=m[:M, 2:3], in_=m[:M, 1:2]), reads=[(m, 1)], writes=[(m, 2)])
            y = yo.next()
            S.op("dve", lambda e: e.tensor_scalar(out=y[:M, :], in0=o[:M, :], scalar1=m[:M, 2:3], scalar2=None, op0=ALU.mult),
                 reads=[o, (m, 2)], writes=[y])
            S.dma("sp", lambda e: e.dma_start(out=out_ap, in_=y[:M, :]), reads=[y])

        jobs = []
        for h in heads:
            bt = bias.next()
            S.dma("sp", lambda e: e.dma_start(out=bt[:], in_=D["na_bias"][l, h].rearrange("c q k -> q c k")), writes=[bt])
            for r in rows:
                r0 = min(max(r - 4, 0), 56)
                cls = r if r < 4 else (4 if r <= 60 else r - 56)
                if r0 % 2 == 0:
                    vch = [(va, r0 // 2 + jj) for jj in range(4)]
                else:
                    vch = [(vb, (r0 - 1) // 2 + jj) for jj in range(4)]
                vch += [(vc, 0), (vc, 1)]
                jobs.append((h, 64, (r * 64, (r + 1) * 64), r0 * 64, cls, vch, bt,
                             D["ycat"][r * 64:(r + 1) * 64, h * 64:(h + 1) * 64]))
            if need_ctx:
                for ct in range(2):
                    jobs.append((h, 128, (4096 + ct * 128, 4096 + (ct + 1) * 128), None, None, [(vc, 0), (vc, 1)], None,
                                 D["ycat"][4096 + ct * 128:4096 + (ct + 1) * 128, h * 64:(h + 1) * 64]))
        st_ = {}
        nj = len(jobs)
        for i in range(nj + 2):
            if i < nj:
                st_[i] = att_s1(*jobs[i])
            if 0 <= i - 1 < nj:
                att_s2(st_[i - 1])
            if 0 <= i - 2 < nj:
                att_s3(st_.pop(i - 2))


def phase_gla(S, D, l, need_ctx=True, lat_chunks=64):
    qscale = 32 ** -0.5
    with S.phase():
        identf = D["ident_f"]
        identb = D["ident_bf"]
        ropeC = S.tile([64, 64, 2, 8], F32, "ropeC")
        ropeS = S.tile([64, 64, 2, 8], F32, "ropeS")
        mt = S.tile([64, 2, 64], F32, "mt")
        tri = S.tile([64, 2, 64], F32, "tri")
        blk = S.tile([128, 4, 64], F32, "blk")
        gw = S.tile([33, 2, 128], F32, "gw")
        ngb = vec_bcast(S, D["gla_norm_g"][l], 256, "ngb")
        for t, src in ((ropeC, D["ropeC"]), (ropeS, D["ropeS"]), (mt, D["gla_mt"]), (tri, D["gla_tri"]), (blk, D["gla_blk"]),
                       (gw, D["gla_gw"][l].rearrange("d k n -> k d n"))):
            S.dma("sp", lambda e: e.dma_start(out=t[:], in_=src), writes=[t])
        Sblk = S.tile([128, 256], F32, "Sblk")
        Sbf = S.tile([128, 256], BF16, "Sbf")
        zc_r = Rot(S, 10, [64, 800], F32, name="zc")
        qkr = Rot(S, 2, [64, 256], F32, name="qkr")
        rt = Rot(S, 4, [64, 8, 2, 8], F32, name="rt")
        loT = Rot(S, 3, [33, 64], F32, name="loT")
        for b in loT.bufs:
            S.op("pool", lambda e: e.memset(b[32:33, :], 1.0), writes=[(b, "one")])
        e1 = Rot(S, 2, [64, 128], F32, name="e1")
        sp = Rot(S, 2, [64, 128], F32, name="sp")
        eb = Rot(S, 5, [128, 64], F32, name="eb")
        enb = Rot(S, 3, [128, 64], F32, name="enb")
        qs = Rot(S, 3, [128, 64], BF16, name="qs")
        ks = Rot(S, 2, [128, 64], BF16, name="ks")
        ke = Rot(S, 2, [128, 64], BF16, name="ke")
        qb = Rot(S, 2, [128, 4, 64], BF16, name="qb")
        kend = Rot(S, 3, [64, 128], BF16, name="kend")
        am = Rot(S, 3, [64, 4, 64], BF16, name="am")
        vbf = Rot(S, 3, [64, 256], BF16, name="vbf")
        tu = Rot(S, 4, [128, 256], F32, name="tu")
        of_ = Rot(S, 10, [64, 256], F32, name="of")
        osum = Rot(S, 2, [64, 256], F32, name="osum")
        sq = Rot(S, 2, [64, 256], F32, name="sq")
        ms = Rot(S, 2, [64, 16], F32, name="ms")
        sr = Rot(S, 2, [64, 256], F32, name="sr")
        yo = Rot(S, 2, [64, 256], F32, name="yo")
        bankA = Rot(S, 3, [128, 512], F32, psum=True, name="bankA")
        pkT = Rot(S, 1, [64, 128], BF16, psum=True, name="pkT")
        pA = Rot(S, 1, [64, 256], F32, psum=True, name="pA")
        pO = Rot(S, 2, [64, 256], F32, psum=True, name="pO")
        pU = Rot(S, 1, [128, 256], F32, psum=True, name="pU")

        def chunk_l(tok0, latent, n, d, want_out):
            z = zc_r.next()
            S.dma("sp", lambda e: e.dma_start(out=z[:], in_=D["z"][tok0:tok0 + 64, C_GQ:C_GQ + 800]), writes=[z])
            o_pre = None
            if want_out and d == 1:
                o_pre = of_.next()
                S.dma("sp", lambda en: en.dma_start(out=o_pre[:], in_=D["ofwd"][tok0:tok0 + 64, :]), writes=[o_pre])
            return dict(tok0=tok0, latent=latent, n=n, d=d, want_out=want_out, z=z, o_pre=o_pre)

        def chunk_a1(c_):
            tok0, latent, n, d, want_out, z = (c_[k] for k in ("tok0", "latent", "n", "d", "want_out", "z"))
            if latent:
                q = qkr.next()
                zv = z[:, 0:256].rearrange("p (h a b f) -> p h a b f", h=8, a=2, b=2)
                qv = q[:].rearrange("p (h a b f) -> p h a b f", h=8, a=2, b=2)
                xa, xb_ = zv[:, :, :, 0, :], zv[:, :, :, 1, :]
                cosb = ropeC[:, n, :, :].unsqueeze(1).to_broadcast([64, 8, 2, 8])
                sinb = ropeS[:, n, :, :].unsqueeze(1).to_broadcast([64, 8, 2, 8])
                t1, t2, t3, t4 = rt.next(), rt.next(), rt.next(), rt.next()
                S.op("dve", lambda e: e.tensor_tensor(out=t1[:], in0=xa, in1=cosb, op=ALU.mult), reads=[z, ropeC], writes=[t1])
                S.op("pool", lambda e: e.tensor_tensor(out=t2[:], in0=xb_, in1=sinb, op=ALU.mult), reads=[z, ropeS], writes=[t2])
                S.op("pool", lambda e: e.tensor_tensor(out=t3[:], in0=xa, in1=sinb, op=ALU.mult), reads=[z, ropeS], writes=[t3])
                S.op("dve", lambda e: e.tensor_tensor(out=t4[:], in0=xb_, in1=cosb, op=ALU.mult), reads=[z, ropeC], writes=[t4])
                S.op("dve", lambda e: e.tensor_tensor(out=qv[:, :, :, 0, :], in0=t1[:], in1=t2[:], op=ALU.subtract), reads=[t1, t2], writes=[(q, "a")])
                S.op("pool", lambda e: e.tensor_tensor(out=qv[:, :, :, 1, :], in0=t3[:], in1=t4[:], op=ALU.add), reads=[t3, t4], writes=[(q, "b")])
                qsrc, qdep = q, q
            else:
                qsrc, qdep = z, z
            A = bankA.next()
            for c in range(2):
                S.op("pe", lambda e: e.transpose(out=A[:, c * 64:(c + 1) * 64], in_=qsrc[:, c * 128:(c + 1) * 128], identity=identf[:64, :64]),
                     reads=[qdep, identf], writes=[(A, "qk%d" % c)])
            S.op("pe", lambda e: e.transpose(out=A[0:32, 320:384], in_=z[:, 768:800], identity=identf[:64, :64]),
                 reads=[z, identf], writes=[(A, "lo")])
            lt = loT.next()
            S.op("act", lambda e: e.copy(out=lt[0:32, :], in_=A[0:32, 320:384]), reads=[(A, "lo")], writes=[(lt, "d")])
            c_.update(A=A, lt=lt)

        def chunk_a2(c_):
            d, A, lt = c_["d"], c_["A"], c_["lt"]
            S.op("pe", lambda e: e.matmul(A[0:64, 192:320], lhsT=lt[:, :], rhs=gw[:, d, :], start=True, stop=True),
                 reads=[lt, gw], writes=[(A, "g")])
            e = e1.next()
            S.op("act", lambda en: en.activation(out=e[:], in_=A[0:64, 192:320], func=AF.Exp, scale=-1.0), reads=[(A, "g")], writes=[e])
            s_ = sp.next()
            S.op("act", lambda en: en.activation(out=s_[:], in_=e[:], func=AF.Ln, bias=1.0, scale=1.0), reads=[e], writes=[s_])
            S.op("pe", lambda en: en.matmul(A[:, 128:192], lhsT=s_[:, :], rhs=mt[:, d, :], start=True, stop=True),
                 reads=[s_, mt], writes=[(A, "b")])
            ebt = eb.next(); enbt = enb.next()
            S.op("act", lambda en: en.activation(out=ebt[:], in_=A[:, 128:192], func=AF.Exp), reads=[(A, "b")], writes=[ebt])
            S.op("act", lambda en: en.activation(out=enbt[:], in_=A[:, 128:192], func=AF.Exp, scale=-1.0), reads=[(A, "b")], writes=[enbt])
            dec = ebt[:, 63:64] if d == 0 else ebt[:, 0:1]
            c_.update(ebt=ebt, enbt=enbt, dec=dec)

        def chunk_a3(c_):
            d, A, z, ebt, enbt, dec = (c_[k] for k in ("d", "A", "z", "ebt", "enbt", "dec"))
            qst = qs.next(); kst = ks.next(); ket = ke.next(); qbt = qb.next()
            S.op("dve", lambda en: en.scalar_tensor_tensor(out=qst[:], in0=A[:, 0:64], scalar=qscale, in1=ebt[:], op0=ALU.mult, op1=ALU.mult),
                 reads=[(A, "qk0"), ebt], writes=[qst])
            S.op("dve", lambda en: en.tensor_tensor(out=kst[:], in0=A[:, 64:128], in1=enbt[:], op=ALU.mult), reads=[(A, "qk1"), enbt], writes=[kst])
            S.op("dve", lambda en: en.scalar_tensor_tensor(out=ket[:], in0=A[:, 64:128], scalar=dec, in1=enbt[:], op0=ALU.mult, op1=ALU.mult),
                 reads=[(A, "qk1"), enbt, ebt], writes=[ket])
            S.op("pool", lambda en: en.tensor_tensor(out=qbt[:], in0=qst[:].unsqueeze(1).to_broadcast([128, 4, 64]), in1=blk[:], op=ALU.mult),
                 reads=[qst, blk], writes=[qbt])
            pk = pkT.next()
            S.op("pe", lambda en: en.transpose(out=pk[:], in_=ket[:], identity=identb[:]), reads=[ket, identb], writes=[pk])
            kt = kend.next()
            S.op("act", lambda en: en.copy(out=kt[:], in_=pk[:]), reads=[pk], writes=[kt])
            v = vbf.next()
            S.op("pool", lambda en: en.tensor_copy(out=v[:], in_=z[:, 256:512]), reads=[z], writes=[v])
            pa = pA.next()
            S.op("pe", lambda en: en.matmul(pa[:], lhsT=kst[:], rhs=qbt[:].rearrange("p h c -> p (h c)"), start=True, stop=True),
                 reads=[kst, qbt], writes=[pa])
            amt = am.next()
            S.op("dve", lambda en: en.tensor_tensor(out=amt[:], in0=pa[:].rearrange("p (h c) -> p h c", h=4),
                                                   in1=tri[:, d, :].unsqueeze(1).to_broadcast([64, 4, 64]), op=ALU.mult),
                 reads=[pa, tri], writes=[amt])
            pu = pU.next()
            S.op("pe", lambda en: en.matmul(pu[:], lhsT=kt[:], rhs=v[:], start=True, stop=True), reads=[kt, v], writes=[pu])
            tut = tu.next()
            S.op("dve", lambda en: en.tensor_tensor(out=tut[:], in0=pu[:], in1=blk[:].rearrange("p h c -> p (h c)"), op=ALU.mult),
                 reads=[pu, blk], writes=[tut])
            c_.update(qst=qst, kt=kt, amt=amt, v=v, tut=tut)

        def chunk_b(c_):
            tok0, d, want_out, z, ebt, dec, qst, kt, amt, v = (c_[k] for k in ("tok0", "d", "want_out", "z", "ebt", "dec", "qst", "kt", "amt", "v"))
            tut = c_["tut"]
            po = pO.next()
            S.op("pe", lambda en: en.matmul(po[:], lhsT=qst[:], rhs=Sbf[:], start=True, stop=False, skip_group_check=True),
                 reads=[qst, Sbf], writes=[po])
            for h in range(4):
                S.op("pe", lambda en: en.matmul(po[:, h * 64:(h + 1) * 64], lhsT=amt[:, h, :], rhs=v[:, h * 64:(h + 1) * 64],
                                               start=False, stop=True, skip_group_check=True), reads=[amt, v], writes=[po])
            S.op("dve", lambda en: en.scalar_tensor_tensor(out=Sblk[:], in0=Sblk[:], scalar=dec, in1=tut[:], op0=ALU.mult, op1=ALU.add),
                 reads=[Sblk, ebt, tut], writes=[Sblk])
            S.op("act", lambda en: en.copy(out=Sbf[:], in_=Sblk[:]), reads=[Sblk], writes=[Sbf])
            if not want_out:
                return
            if d == 0:
                o = of_.next()
                S.op("act", lambda en: en.copy(out=o[:], in_=po[:]), reads=[po], writes=[o])
                S.dma("sp", lambda en: en.dma_start(out=D["ofwd"][tok0:tok0 + 64, :], in_=o[:]), reads=[o])
                return
            o = c_["o_pre"]
            os_ = osum.next()
            S.op("dve", lambda en: en.tensor_tensor(out=os_[:], in0=po[:], in1=o[:], op=ALU.add), reads=[po, o], writes=[os_])
            q2 = sq.next()
            S.op("pool", lambda en: en.tensor_tensor(out=q2[:], in0=os_[:], in1=os_[:], op=ALU.mult), reads=[os_], writes=[q2])
            m = ms.next()
            S.op("dve", lambda en: en.tensor_reduce(out=m[:, 0:4], in_=q2[:].rearrange("p (h c) -> p h c", h=4), axis=AX.X, op=ALU.add),
                 reads=[q2], writes=[(m, 0)])
            S.op("dve", lambda en: en.tensor_scalar(out=m[:, 4:8], in0=m[:, 0:4], scalar1=1.0 / 64, scalar2=LN_EPS, op0=ALU.mult, op1=ALU.add),
                 reads=[(m, 0)], writes=[(m, 1)])
            S.op("act", lambda en: en.activation(out=m[:, 8:12], in_=m[:, 4:8], func=AF.Ln), reads=[(m, 1)], writes=[(m, 2)])
            S.op("act", lambda en: en.activation(out=m[:, 12:16], in_=m[:, 8:12], func=AF.Exp, scale=-0.5), reads=[(m, 2)], writes=[(m, 3)])
            S.op("dve", lambda en: en.tensor_tensor(out=os_[:].rearrange("p (h c) -> p h c", h=4), in0=os_[:].rearrange("p (h c) -> p h c", h=4),
                                                   in1=m[:, 12:16].unsqueeze(2).to_broadcast([64, 4, 64]), op=ALU.mult),
                 reads=[os_, (m, 3)], writes=[os_])
            S.op("pool", lambda en: en.tensor_tensor(out=os_[:], in0=os_[:], in1=ngb[0:64, :], op=ALU.mult), reads=[os_, ngb], writes=[os_])
            srt = sr.next()
            S.op("act", lambda en: en.activation(out=srt[:], in_=z[:, 512:768], func=AF.Exp, scale=-1.0), reads=[z], writes=[srt])
            S.op("pool", lambda en: en.tensor_scalar(out=srt[:], in0=srt[:], scalar1=1.0, scalar2=None, op0=ALU.add), reads=[srt], writes=[srt])
            S.op("dve", lambda en: en.reciprocal(out=srt[:], in_=srt[:]), reads=[srt], writes=[srt])
            S.op("pool", lambda en: en.tensor_tensor(out=srt[:], in0=srt[:], in1=z[:, 512:768], op=ALU.mult), reads=[srt, z], writes=[srt])
            y = yo.next()
            S.op("dve", lambda en: en.tensor_tensor(out=y[:], in0=os_[:], in1=srt[:], op=ALU.mult), reads=[os_, srt], writes=[y])
            S.dma("sp", lambda en: en.dma_start(out=D["ycat"][tok0:tok0 + 64, 256:512], in_=y[:]), reads=[y])

        for d in range(2):
            S.op("pool", lambda en: en.memset(Sblk[:], 0.0), writes=[Sblk])
            S.op("pool", lambda en: en.memset(Sbf[:], 0.0), writes=[Sbf])
            order = list(range(4)) if d == 0 else list(range(3, -1, -1))
            jobs = [(SEQ + 64 * i, False, 0, d, need_ctx) for i in order]
            order = list(range(lat_chunks)) if d == 0 else list(range(lat_chunks - 1, -1, -1))
            jobs += [(64 * i, True, i, d, True) for i in order]
            st_ = {}
            nj = len(jobs)
            PF = 4
            for i in range(nj + 3 + PF):
                if i < nj:
                    st_[i] = chunk_l(*jobs[i])
                if 0 <= i - PF < nj:
                    chunk_a1(st_[i - PF])
                if 0 <= i - PF - 1 < nj:
                    chunk_a2(st_[i - PF - 1])
                if 0 <= i - PF - 2 < nj:
                    chunk_a3(st_[i - PF - 2])
                if 0 <= i - PF - 3 < nj:
                    chunk_b(st_.pop(i - PF - 3))
            S.barrier()


def phase_peer_topk(S, D, l, tiles=range(NTT), xin="xs"):
    with S.phase():
        ident = D["ident_bf"]
        wq = S.tile([128, 8, 2048], BF16, "wq")
        S.dma("pool", lambda e: e.dma_start(out=wq[:], in_=D["peer_wq"][l].rearrange("(k p) n -> p k n", p=128)), writes=[wq])
        kT = S.tile([128, 16, 128], F32, "kT")
        S.dma("sp", lambda e: e.dma_start(out=kT[:], in_=D["peer_keysT"][l].rearrange("b d k -> d b k")), writes=[kT])
        iota = S.tile([128, 16, 16], F32, "iota")
        S.dma("sp", lambda e: e.dma_start(out=iota[:], in_=D["iota_kk"]), writes=[iota])
        scb = [bcast_load(S, D, l, j, 4096, 1024, "scb") for j in range(2)]
        shb = [bcast_load(S, D, l, j, 3072, 1024, "shb") for j in range(2)]
        xt = Rot(S, 2, [128, 1024], F32, name="xt")
        hf = Rot(S, 1, [128, 1024], F32, name="hf")
        hb = Rot(S, 2, [128, 1024], BF16, name="hb")
        hT = Rot(S, 2, [128, 8, 128], BF16, name="hT")
        qT = Rot(S, 2, [128, 16, 128], F32, name="qT")
        sc = Rot(S, 3, [128, 16, 128], F32, name="sc")
        wk16 = Rot(S, 1, [128, 16, 128], F32, name="wk16")
        wk8 = Rot(S, 1, [128, 8, 256], F32, name="wk8")
        t1 = Rot(S, 3, [128, 16, 16], F32, name="t1")
        ti = Rot(S, 3, [128, 16, 16], U32, name="ti")
        tif = Rot(S, 3, [128, 16, 16], F32, name="tif")
        cs = Rot(S, 2, [128, 8, 256], F32, name="cs")
        bs = Rot(S, 3, [128, 8, 16], F32, name="bs")
        bj = Rot(S, 3, [128, 8, 16], U32, name="bj")
        hi = Rot(S, 2, [128, 8, 16], U32, name="hi")
        lo = Rot(S, 2, [128, 8, 16], U32, name="lo")
        hif = Rot(S, 2, [128, 8, 16], F32, name="hif")
        lof = Rot(S, 2, [128, 8, 16], F32, name="lof")
        oh = Rot(S, 2, [128, 8, 16, 16], F32, name="oh")
        ee = Rot(S, 4, [128, 8, 16], F32, name="ee")
        ei = Rot(S, 2, [128, 128], I32, name="ei")
        gg = Rot(S, 2, [128, 8, 16], F32, name="gg")
        sm = Rot(S, 2, [128, 32], F32, name="sm")
        pT = Rot(S, 2, [128, 8, 128], BF16, psum=True, name="pT")
        pq = Rot(S, 3, [128, 4, 128], F32, psum=True, name="pq")
        def stage_a(tt):
            j = 0 if tt < 32 else 1
            r0 = tt * 128
            x = xt.next()
            S.dma("sp", lambda e: e.dma_start(out=x[:], in_=D[xin][r0:r0 + 128, :]), writes=[x])
            h1 = hf.next()
            S.op("pool", lambda e: e.tensor_tensor(out=h1[:], in0=x[:], in1=scb[j][:], op=ALU.mult), reads=[x, scb[j]], writes=[h1])
            h2 = hb.next()
            S.op("pool", lambda e: e.tensor_tensor(out=h2[:], in0=h1[:], in1=shb[j][:], op=ALU.add), reads=[h1, shb[j]], writes=[h2])
            p = pT.next()
            for k in range(8):
                S.op("pe", lambda e: e.transpose(out=p[:, k, :], in_=h2[:, k * 128:(k + 1) * 128], identity=ident[:]),
                     reads=[h2, ident], writes=[(p, k)])
            t = hT.next()
            S.op("act", lambda e: e.copy(out=t[:], in_=p[:]), reads=[p], writes=[t])
            q = qT.next()
            for g in range(4):
                pp = pq.next()
                for b in range(4):
                    blk = g * 4 + b
                    for k in range(8):
                        S.op("pe", lambda e: e.matmul(pp[:, b, :], lhsT=wq[:, k, blk * 128:(blk + 1) * 128], rhs=t[:, k, :],
                                                      start=(k == 0), stop=(k == 7)), reads=[wq, t], writes=[(pp, b)])
                S.op("act", lambda e: e.copy(out=q[:, g * 4:(g + 1) * 4, :], in_=pp[:]), reads=[pp], writes=[(q, g)])
            s = sc.next()
            for g in range(4):
                pp = pq.next()
                for b in range(4):
                    blk = g * 4 + b
                    S.op("pe", lambda e: e.matmul(pp[:, b, :], lhsT=q[:, blk, :], rhs=kT[:, blk, :], start=True, stop=True),
                         reads=[q, kT], writes=[(pp, b)])
                S.op("act", lambda e: e.copy(out=s[:, g * 4:(g + 1) * 4, :], in_=pp[:]), reads=[pp], writes=[(s, g)])
            return dict(r0=r0, s=s)

        def stage_b1(st_):
            r0, s = st_["r0"], st_["s"]
            tv = t1.next(); tix = ti.next()
            w16 = wk16.next()
            for blk in range(16):
                S.op("dve", lambda e: e.max(out=tv[:, blk, 0:8], in_=s[:, blk, :]), reads=[s], writes=[(tv, (blk, 0))])
            for blk in range(16):
                S.op("dve", lambda e: e.match_replace(out=w16[:, blk, :], in_to_replace=tv[:, blk, 0:8], in_values=s[:, blk, :], imm_value=-1e30),
                     reads=[s, (tv, (blk, 0))], writes=[(w16, blk)])
            for blk in range(16):
                S.op("dve", lambda e: e.max(out=tv[:, blk, 8:16], in_=w16[:, blk, :]), reads=[(w16, blk)], writes=[(tv, (blk, 1))])
            for blk in range(16):
                S.op("dve", lambda e: e.max_index(out=tix[:, blk, 0:8], in_max=tv[:, blk, 0:8], in_values=s[:, blk, :]),
                     reads=[s, (tv, (blk, 0))], writes=[(tix, (blk, 0))])
            for blk in range(16):
                S.op("dve", lambda e: e.max_index(out=tix[:, blk, 8:16], in_max=tv[:, blk, 8:16], in_values=w16[:, blk, :]),
                     reads=[(w16, blk), (tv, (blk, 1))], writes=[(tix, (blk, 1))])
            st_.update(tv=tv, tix=tix)

        def stage_b2(st_):
            tv, tix = st_["tv"], st_["tix"]
            tf = tif.next()
            S.op("pool", lambda e: e.tensor_copy(out=tf[:], in_=tix[:]), reads=[tix], writes=[tf])
            c = cs.next()
            tv4 = tv[:].rearrange("p (h s) k -> p h s k", s=2)
            S.op("dve", lambda e: e.tensor_tensor(out=c[:].rearrange("p h (i j) -> p h i j", i=16),
                                                  in0=tv4[:, :, 0, :].unsqueeze(3).to_broadcast([128, 8, 16, 16]),
                                                  in1=tv4[:, :, 1, :].unsqueeze(2).to_broadcast([128, 8, 16, 16]), op=ALU.add),
                 reads=[tv], writes=[c])
            b_ = bs.next(); bjx = bj.next()
            w8 = wk8.next()
            for h in range(8):
                S.op("dve", lambda e: e.max(out=b_[:, h, 0:8], in_=c[:, h, :]), reads=[c], writes=[(b_, (h, 0))])
            for h in range(8):
                S.op("dve", lambda e: e.match_replace(out=w8[:, h, :], in_to_replace=b_[:, h, 0:8], in_values=c[:, h, :], imm_value=-1e30),
                     reads=[c, (b_, (h, 0))], writes=[(w8, h)])
            for h in range(8):
                S.op("dve", lambda e: e.max(out=b_[:, h, 8:16], in_=w8[:, h, :]), reads=[(w8, h)], writes=[(b_, (h, 1))])
            for h in range(8):
                S.op("dve", lambda e: e.max_index(out=bjx[:, h, 0:8], in_max=b_[:, h, 0:8], in_values=c[:, h, :]),
                     reads=[c, (b_, (h, 0))], writes=[(bjx, (h, 0))])
            for h in range(8):
                S.op("dve", lambda e: e.max_index(out=bjx[:, h, 8:16], in_max=b_[:, h, 8:16], in_values=w8[:, h, :]),
                     reads=[(w8, h), (b_, (h, 1))], writes=[(bjx, (h, 1))])
            st_.update(tf=tf, b_=b_, bjx=bjx)

        def stage_b3(st_):
            r0, tf, b_, bjx = (st_[k] for k in ("r0", "tf", "b_", "bjx"))
            hx = hi.next(); lx = lo.next(); hfx = hif.next(); lfx = lof.next()
            S.op("dve", lambda e: e.tensor_single_scalar(out=hx[:], in_=bjx[:], scalar=4, op=ALU.logical_shift_right), reads=[bjx], writes=[hx])
            S.op("dve", lambda e: e.tensor_single_scalar(out=lx[:], in_=bjx[:], scalar=15, op=ALU.bitwise_and), reads=[bjx], writes=[lx])
            S.op("pool", lambda e: e.tensor_copy(out=hfx[:], in_=hx[:]), reads=[hx], writes=[hfx])
            S.op("pool", lambda e: e.tensor_copy(out=lfx[:], in_=lx[:]), reads=[lx], writes=[lfx])
            tf4 = tf[:].rearrange("p (h s) k -> p h s k", s=2)
            es = []
            for (sel, half) in ((hfx, 0), (lfx, 1)):
                o = oh.next()
                S.op("dve", lambda e: e.tensor_tensor(out=o[:], in0=sel[:].unsqueeze(3).to_broadcast([128, 8, 16, 16]),
                                                      in1=iota[:].unsqueeze(1).to_broadcast([128, 8, 16, 16]), op=ALU.is_equal),
                     reads=[sel, iota], writes=[o])
                S.op("pool", lambda e: e.tensor_tensor(out=o[:], in0=o[:], in1=tf4[:, :, half, :].unsqueeze(2).to_broadcast([128, 8, 16, 16]),
                                                       op=ALU.mult), reads=[o, tf], writes=[o])
                ex = ee.next()
                S.op("dve", lambda e: e.tensor_reduce(out=ex[:].rearrange("p h k -> p (h k)"), in_=o[:].rearrange("p h k i -> p (h k) i"),
                                                      axis=AX.X, op=ALU.add), reads=[o], writes=[ex])
                es.append(ex)
            ef = ee.next()
            S.op("dve", lambda e: e.scalar_tensor_tensor(out=ef[:], in0=es[0][:], scalar=128.0, in1=es[1][:], op0=ALU.mult, op1=ALU.add),
                 reads=[es[0], es[1]], writes=[ef])
            eix = ei.next()
            S.op("dve", lambda e: e.tensor_copy(out=eix[:], in_=ef[:].rearrange("p h k -> p (h k)")), reads=[ef], writes=[eix])
            identf = D["ident_f"]
            ptr = pq.next()
            S.op("pe", lambda e: e.transpose(out=ptr[:, 0, :], in_=ef[:].rearrange("p h k -> p (h k)"), identity=identf[:]),
                 reads=[ef, identf], writes=[(ptr, 0)])
            S.op("dve", lambda e: e.tensor_copy(out=eix[:], in_=ptr[:, 0, :]), reads=[(ptr, 0)], writes=[eix])
            S.dma("sp", lambda e: e.dma_start(out=D["pidx"][r0:r0 + 128, :], in_=eix[:]), reads=[eix])
            g_ = gg.next(); m = sm.next()
            S.op("dve", lambda e: e.tensor_tensor(out=g_[:], in0=b_[:], in1=b_[:, :, 0:1].to_broadcast([128, 8, 16]), op=ALU.subtract),
                 reads=[b_], writes=[g_])
            S.op("act", lambda e: e.activation(out=g_[:], in_=g_[:], func=AF.Exp), reads=[g_], writes=[g_])
            S.op("dve", lambda e: e.tensor_reduce(out=m[:, 0:8], in_=g_[:], axis=AX.X, op=ALU.add), reads=[g_], writes=[(m, 0)])
            S.op("dve", lambda e: e.reciprocal(out=m[:, 8:16], in_=m[:, 0:8]), reads=[(m, 0)], writes=[(m, 1)])
            S.op("dve", lambda e: e.tensor_tensor(out=g_[:], in0=g_[:], in1=m[:, 8:16].unsqueeze(2).to_broadcast([128, 8, 16]), op=ALU.mult),
                 reads=[g_, (m, 1)], writes=[g_])
            S.op("pe", lambda e: e.transpose(out=ptr[:, 1, :], in_=g_[:].rearrange("p h k -> p (h k)"), identity=identf[:]),
                 reads=[g_, identf], writes=[(ptr, 1)])
            gT_ = qT.next()
            S.op("act", lambda e: e.copy(out=gT_[:, 0, :], in_=ptr[:, 1, :]), reads=[(ptr, 1)], writes=[gT_])
            S.dma("sp", lambda e: e.dma_start(out=D["pgt"][r0:r0 + 128, :], in_=gT_[:, 0, :]), reads=[gT_])


        tiles = list(tiles)
        stt = {}
        nj = len(tiles)
        for i in range(nj + 3):
            if i < nj:
                stt[i] = stage_a(tiles[i])
            if 0 <= i - 1 < nj:
                stage_b1(stt[i - 1])
            if 0 <= i - 2 < nj:
                stage_b2(stt[i - 2])
            if 0 <= i - 3 < nj:
                stage_b3(stt.pop(i - 3))


TAB_CHUNK = 512


def start_table_convert(S, D, l):
    vals = []
    for ti, nm in enumerate(("peer_u", "peer_v")):
        sem = S.bg[2 * l + ti]
        n = 0
        for r0 in range(0, 16384, TAB_CHUNK):
            v = S.bg_dma("pool", lambda e: e.dma_start(out=D["peer_uvb%d" % l][r0:r0 + TAB_CHUNK, ti * 1024:(ti + 1) * 1024],
                                                       in_=D["%s%d" % (nm, l)][r0:r0 + TAB_CHUNK, :]), sem, n)
            n += 1
        vals.append(v)
    return vals


def phase_peer_ffn(S, D, l, tiles=range(NTT), xin="xs", xout="xs", conv_vals=None):
    tiles = list(tiles)
    if conv_vals is not None:
        for ti in range(2):
            S.wait_sem(("pool", "sp"), S.bg[2 * l + ti], conv_vals[ti])
    with S.phase():
        ident = D["ident_bf"]
        uvb = D["peer_uvb%d" % l]
        scb = [bcast_load(S, D, l, j, 4096, 1024, "scb") for j in range(2)]
        shb = [bcast_load(S, D, l, j, 3072, 1024, "shb") for j in range(2)]
        g2b = [bcast_load(S, D, l, j, 5120, 1024, "g2b") for j in range(2)]
        lng = vec_bcast(S, D["ln2_g"][l], 1024, "lng")
        lnb = vec_bcast(S, D["ln2_b"][l], 1024, "lnb")
        xt = Rot(S, 3, [128, 1024], F32, name="xt")
        hf = Rot(S, 2, [128, 1024], F32, name="hf")
        hb = Rot(S, 3, [128, 1024], BF16, name="hb")
        idx = Rot(S, 3, [128, 128], I32, name="idx")
        gt = Rot(S, 3, [128, 128], F32, name="gt")
        gb = Rot(S, 10, [128, 2048], BF16, name="gb")
        junk = Rot(S, 2, [128, 1024], BF16, name="junk")
        aT = Rot(S, 2, [128, 128], F32, name="aT")
        cf = Rot(S, 2, [128, 128], F32, name="cf")
        zb = Rot(S, 6, [128, 255], BF16, name="zb")
        for b in zb.bufs:
            S.op("pool", lambda e: e.memset(b[:], 0.0), writes=[b])
        tm = Rot(S, 2, [128, 1024], F32, name="tm")
        sm = Rot(S, 2, [128, 32], F32, name="sm")
        xo = Rot(S, 2, [128, 1024], F32, name="xo")
        pb = Rot(S, 3, [128, 1024], F32, psum=True, name="pb")
        po_r = Rot(S, 1, [128, 1024], F32, psum=True, name="po")
        def load_stage(tt):
            j = 0 if tt < 32 else 1
            r0 = tt * 128
            x = xt.next(); ix = idx.next(); g = gt.next()
            S.dma("sp", lambda e: e.dma_start(out=ix[:], in_=D["pidx"][r0:r0 + 128, :]), writes=[ix])
            S.dma("sp", lambda e: e.dma_start(out=x[:], in_=D[xin][r0:r0 + 128, :]), writes=[x])
            S.dma("sp", lambda e: e.dma_start(out=g[:], in_=D["pgt"][r0:r0 + 128, :]), writes=[g])
            h1 = hf.next()
            S.op("dve", lambda e: e.tensor_tensor(out=h1[:], in0=x[:], in1=scb[j][:], op=ALU.mult), reads=[x, scb[j]], writes=[h1])
            h = hb.next()
            S.op("dve", lambda e: e.tensor_tensor(out=h[:], in0=h1[:], in1=shb[j][:], op=ALU.add), reads=[h1, shb[j]], writes=[h])
            return (j, r0, x, ix, g, h)

        nxt = load_stage(tiles[0]) if tiles else None
        for ti_, tt in enumerate(tiles):
            j, r0, x, ix, g, h = nxt
            nxt = load_stage(tiles[ti_ + 1]) if ti_ + 1 < len(tiles) else None
            at = aT.next(); c = cf.next(); po = po_r.next()
            LAG = 3
            uvs = {}

            pbs = {}

            def stage_a0(t):
                uv = gb.next()
                uvs[t] = uv
                S.dma("pool", lambda e: e.indirect_dma_start(out=uv[:], out_offset=None, in_=uvb,
                                                             in_offset=bass.IndirectOffsetOnAxis(ap=ix[:, t:t + 1], axis=0)),
                      reads=[ix], writes=[uv])
                p = pb.next()
                pbs[t] = p
                for half in range(2):
                    S.op("pe", lambda e: e.matmul(p[:, half * 512:(half + 1) * 512], lhsT=ident[:, t:t + 1].to_broadcast([128, 128]),
                                                  rhs=h[:, half * 512:(half + 1) * 512], start=True, stop=True),
                         reads=[ident, h], writes=[(p, half)])

            def stage_a1(t):
                uv = uvs[t]
                p = pbs.pop(t)
                jk = junk.next()
                S.op("dve", lambda e: e.scalar_tensor_tensor(out=jk[:], in0=uv[:, 0:1024], scalar=1.0, in1=p[:], op0=ALU.mult, op1=ALU.mult,
                                                             accum_out=at[:, t:t + 1]), reads=[(uv, "u"), p], writes=[jk, (at, t)])
                S.op("act", lambda e: e.activation(out=c[:, t:t + 1], in_=at[:, t:t + 1], func=AF.Gelu_apprx_tanh),
                     reads=[(at, t)], writes=[(c, t)])

            def stage_b(t):
                uv = uvs.pop(t)
                z = zb.next()
                S.op("dve", lambda e: e.tensor_tensor(out=z[:, 127:128], in0=c[:, t:t + 1], in1=g[:, t:t + 1], op=ALU.mult),
                     reads=[(c, t), g], writes=[z])
                for half in range(2):
                    S.op("pe", lambda e: e.matmul(po[:, half * 512:(half + 1) * 512], lhsT=z[:, 127 - t:255 - t],
                                                  rhs=uv[:, 1024 + half * 512:1024 + (half + 1) * 512], start=(t == 0), stop=(t == 127)),
                         reads=[z, (uv, "v")], writes=[(po, half)])

            for s_i in range(128 + LAG + 1):
                if s_i < 128:
                    stage_a0(s_i)
                if 0 <= s_i - 1 < 128:
                    stage_a1(s_i - 1)
                if 0 <= s_i - 1 - LAG < 128:
                    stage_b(s_i - 1 - LAG)
            t_ = tm.next()
            S.op("dve", lambda e: e.tensor_tensor(out=t_[:], in0=po[:], in1=g2b[j][:], op=ALU.mult), reads=[po, g2b[j]], writes=[t_])
            S.op("dve", lambda e: e.scalar_tensor_tensor(out=t_[:], in0=x[:], scalar=ALPHA, in1=t_[:], op0=ALU.mult, op1=ALU.add),
                 reads=[x, t_], writes=[t_])
            t2 = tm.next(); s_ = sm.next(); o = xo.next()
            layer_norm(S, t_[:, :], [t_], 1024, lng, lnb, o, t2, s_)
            S.dma("sp", lambda e: e.dma_start(out=D[xout][r0:r0 + 128, :], in_=o[:]), reads=[o])


D_CONST = {}


def make_consts(S, D):
    mh = S.tile([128, 1], F32, "mhalf")
    S.op("pool", lambda e: e.memset(mh[:], -0.5), writes=[mh])
    D_CONST["mhalf"] = mh
    ib = S.tile([128, 128], BF16, "ident_bf")
    S.op("pool", lambda e: e.memset(ib[:], 0.0), writes=[ib])
    S.op("pool", lambda e: e.affine_select(out=ib[:], in_=ib[:], pattern=[[-1, 128]], compare_op=ALU.not_equal,
                                           fill=1.0, base=0, channel_multiplier=1), reads=[ib], writes=[ib])
    D["ident_bf"] = ib
    i32 = S.tile([128, 128], F32, "ident_f")
    S.op("pool", lambda e: e.memset(i32[:], 0.0), writes=[i32])
    S.op("pool", lambda e: e.affine_select(out=i32[:], in_=i32[:], pattern=[[-1, 128]], compare_op=ALU.not_equal,
                                           fill=1.0, base=0, channel_multiplier=1), reads=[i32], writes=[i32])
    D["ident_f"] = i32


INPUT_SPECS = {
    "xs": ([NTOK, 1024], F32),
    "cvecT": ([128, 8, 2], F32),
    "ada_w": ([2, 1024, 6144], F32),
    "ada_b": ([2, 6144], F32),
    "w_in": ([2, 1024, W_IN_COLS], F32),
    "w_branch": ([2, 1024, 1024], F32),
    "w_out": ([2, 1024, 1024], F32),
    "ln1_g": ([2, 1024], F32),
    "ln1_b": ([2, 1024], F32),
    "na_bias": ([2, 4, 8, 64, 512], F32),
    "ropeC": ([64, 64, 2, 8], F32),
    "ropeS": ([64, 64, 2, 8], F32),
    "gla_mt": ([64, 2, 64], F32),
    "gla_tri": ([64, 2, 64], F32),
    "gla_blk": ([128, 4, 64], F32),
    "gla_gw": ([2, 2, 33, 128], F32),
    "gla_norm_g": ([2, 256], F32),
    "peer_wq": ([2, 1024, 2048], F32),
    "peer_keysT": ([2, 16, 128, 128], F32),
    "peer_u0": ([16384, 1024], F32),
    "peer_u1": ([16384, 1024], F32),
    "peer_v0": ([16384, 1024], F32),
    "peer_v1": ([16384, 1024], F32),
    "ln2_g": ([2, 1024], F32),
    "ln2_b": ([2, 1024], F32),
    "iota_kk": ([128, 16, 16], F32),
    "conv_dwT": ([2, 256, 31], F32),
    "conv_bT": ([2, 128, 2], F32),
    "conv_ln_g": ([2, 256], F32),
    "conv_ln_b": ([2, 256], F32),
    "sgu_ln_g": ([2, 256], F32),
    "sgu_ln_b": ([2, 256], F32),
    "sgu_wsT": ([2, 4, 128, 128], F32),
    "sgu_bsT": ([2, 128, 4], F32),
}
SCRATCH_SPECS = {
    "modv": ([2, 2, 6144], F32),
    "z": ([NTOK, W_IN_COLS], F32),
    "ycat": ([NTOK, 1024], F32),
    "ofwd": ([NTOK, 256], F32),
    "pidx": ([NTOK, 128], I32),
    "pgt": ([NTOK, 128], F32),
    "xa": ([NTOK, 1024], F32),
    "peer_uvb0": ([16384, 2048], BF16),
    "peer_uvb1": ([16384, 2048], BF16),
    "out": ([SEQ, 1024], F32),
}


def build_program(plan, ext_in=(), ext_out=(), inputs=None):
    nc = bass.Bass("TRN2", target_bir_lowering=False)
    D = {}
    for name, (shape, dt) in INPUT_SPECS.items():
        if inputs is not None and name not in inputs:
            continue
        D[name] = nc.dram_tensor(name, shape, dt, kind="ExternalInput").ap()
    for name, (shape, dt) in SCRATCH_SPECS.items():
        kind = "ExternalInput" if name in ext_in else ("ExternalOutput" if name in ext_out else "Internal")
        D[name] = nc.dram_tensor(name, shape, dt, kind=kind).ap()
    with ExitStack() as st:
        S = Sync(nc, st)
        make_consts(S, D)
        plan(S, D)
        S.barrier()
    return nc


def na_bias_table(rpb):
    L = rpb.shape[0]
    W = 64
    col = np.arange(W)
    c0 = np.clip(col - 8, 0, W - 16)
    in_win = (col[None, :] >= c0[:, None]) & (col[None, :] < c0[:, None] + 16)
    dc = np.clip(col[None, :] - col[:, None], -15, 15) + 15
    out = np.empty((L, 4, 8, 64, 512), np.float32)
    reps = [0, 1, 2, 3, 30, 61, 62, 63]
    for ci, r in enumerate(reps):
        r0 = min(max(r - 4, 0), 56)
        for k in range(8):
            dr = r0 + k - r + 7
            b = rpb[:, :, dr][:, :, dc]
            out[:, :, ci, :, k * 64:(k + 1) * 64] = np.where(in_win[None, None], b, np.float32(-1e30))
    return out


def gla_consts():
    inv = (1.0 / (np.float32(100.0) ** (np.arange(8, dtype=np.float32) / np.float32(8)))).astype(np.float32)
    pos = np.arange(64, dtype=np.float32)
    ang = (pos[:, None] * inv[None, :]).astype(np.float32)
    C = np.cos(ang).astype(np.float32)
    Sn = np.sin(ang).astype(np.float32)
    ropeC = np.empty((64, 64, 2, 8), np.float32)
    ropeS = np.empty((64, 64, 2, 8), np.float32)
    ropeC[:, :, 0, :] = C[None, :, :]
    ropeS[:, :, 0, :] = Sn[None, :, :]
    ropeC[:, :, 1, :] = C[:, None, :]
    ropeS[:, :, 1, :] = Sn[:, None, :]
    s = np.arange(64)[:, None]
    c = np.arange(64)[None, :]
    tri = np.stack([(s <= c), (s >= c)], 1).astype(np.float32)
    mt = (tri * np.float32(-1.0 / 16)).astype(np.float32)
    blk = np.zeros((128, 4, 64), np.float32)
    for h in range(4):
        blk[h * 32:(h + 1) * 32, h, :] = 1
    return dict(ropeC=ropeC, ropeS=ropeS, gla_mt=mt, gla_tri=tri, gla_blk=blk)


def gla_gw_layout(gate_up, gate_b):
    L = gate_up.shape[0]
    out = np.zeros((L, 2, 33, 128), np.float32)
    for d in range(2):
        out[:, d, d * 16:(d + 1) * 16, :] = gate_up[:, d]
        out[:, d, 32, :] = gate_b[:, d]
    return out


def host_weights(inp):
    f = lambda a: np.ascontiguousarray(np.asarray(a, dtype=np.float32))
    W = {}
    for l in range(2):
        W["peer_u%d" % l] = f(np.asarray(inp["peer_u"])[l])
        W["peer_v%d" % l] = f(np.asarray(inp["peer_v"])[l])
    for k in ("ada_w", "ada_b", "w_in", "w_out", "ln1_g", "ln1_b", "peer_wq", "ln2_g", "ln2_b",
              "conv_ln_g", "conv_ln_b", "sgu_ln_g", "sgu_ln_b"):
        W[k] = f(inp[k])
    W["w_branch"] = f(np.asarray(inp["w_branch"]).reshape(2, 1024, 1024))
    W["na_bias"] = na_bias_table(f(inp["na_rpb"]))
    W.update(gla_consts())
    W["gla_gw"] = gla_gw_layout(f(inp["gla_gate_up"]), f(inp["gla_gate_b"]))
    W["gla_norm_g"] = f(np.asarray(inp["gla_norm_g"]).reshape(2, 256))
    W["conv_dwT"] = f(np.asarray(inp["conv_dw"]).transpose(0, 2, 1))
    W["conv_bT"] = f(np.asarray(inp["conv_b"]).reshape(2, 2, 128).transpose(0, 2, 1))
    W["sgu_wsT"] = f(np.asarray(inp["sgu_ws"]).transpose(0, 1, 3, 2))
    W["sgu_bsT"] = f(np.asarray(inp["sgu_bs"]).transpose(0, 2, 1))
    W["peer_keysT"] = f(np.asarray(inp["peer_keys"]).reshape(2, 16, 128, 128).transpose(0, 1, 3, 2))
    W["iota_kk"] = f(np.broadcast_to(np.arange(16, dtype=np.float32)[None, None, :], (128, 16, 16)))
    return W


def full_plan(S, D):
    cv = {}
    for l in range(DEPTH):
        need_ctx = l < DEPTH - 1
        tl = range(NTT) if need_ctx else range(32)
        xin = "xs" if l == 0 else "xa"
        phase_win(S, D, l, xin=xin, post_weights=(lambda l_=l: cv.__setitem__(l_, start_table_convert(S, D, l_))),
                  pre=(lambda l_=l: phase_mod(S, D, l_)))
        phase_na(S, D, l, need_ctx=need_ctx)
        phase_gla(S, D, l, need_ctx=need_ctx)
        phase_conv(S, D, l, do_ctx=need_ctx)
        phase_sgu(S, D, l, tiles=tl)
        phase_merge(S, D, l, tiles=tl, xin=xin, xout="xa")
        phase_peer_topk(S, D, l, tiles=tl, xin="xa")
        phase_peer_ffn(S, D, l, tiles=tl, xin="xa", xout=("xa" if need_ctx else "out"), conv_vals=cv[l])


_CACHE = {}


def kernel(**inputs):
    x = np.asarray(inputs["x"], dtype=np.float32)
    c = np.asarray(inputs["c"], dtype=np.float32)
    ctx = np.asarray(inputs["ctx"], dtype=np.float32)
    c_ctx = np.asarray(inputs["c_ctx"], dtype=np.float32)
    B = x.shape[0]
    W = host_weights(inputs)
    if "nc" not in _CACHE:
        _CACHE["nc"] = build_program(full_plan, ext_out=("out",))
    nc = _CACHE["nc"]
    in_maps = []
    for b in range(B):
        m = dict(W)
        m["xs"] = np.ascontiguousarray(np.concatenate([x[b], ctx[b]], 0))
        cvec = np.stack([c[b], c_ctx], 0)
        m["cvecT"] = np.ascontiguousarray(cvec.reshape(2, 8, 128).transpose(2, 1, 0))
        in_maps.append(m)
    res = run_bass_kernel_spmd(nc, in_maps, core_ids=list(range(B)))
    return np.stack([np.asarray(r["out"], dtype=np.float32) for r in res.results], 0)
```

```python
import numpy as np
from contextlib import ExitStack, contextmanager
import concourse.bass as bass
import concourse.mybir as mybir
from concourse.bass_utils import run_bass_kernel_spmd

F32 = mybir.dt.float32
BF16 = mybir.dt.bfloat16
I32 = mybir.dt.int32
U32 = mybir.dt.uint32
AF = mybir.ActivationFunctionType
ALU = mybir.AluOpType
AX = mybir.AxisListType

D_MODEL = 1024
SEQ = 4096
CTX = 256
NTOK = SEQ + CTX
NTT = NTOK // 128
DEPTH = 2
W_IN_COLS = 6688
ALPHA = (2 * DEPTH) ** 0.25
LN_EPS = 1e-6
C_QKV, C_GQ, C_GK, C_GV, C_GR, C_GLO, C_CONV, C_SGU, C_GATE = 0, 768, 896, 1024, 1280, 1536, 1568, 2080, 2592


class Buf:
    def __init__(self, h):
        self.h = h
        self.st = {}

    def __getitem__(self, idx):
        return self.h[idx]


class Sync:
    NDMA = 8

    def __init__(self, nc, stack):
        self.nc = nc
        self.stack = stack
        self.E = {}
        self.semobj = {}
        for name, eng in (("pe", nc.tensor), ("act", nc.scalar), ("dve", nc.vector),
                          ("pool", nc.gpsimd), ("sp", nc.sync)):
            sem = stack.enter_context(nc.semaphore("s_" + name))
            self.E[name] = dict(eng=eng, sem=sem, cnt=0, seen={}, dq=[], dn=0)
            self.semobj[id(sem)] = sem
        for q in ("sp", "pool", "act"):
            e = self.E[q]
            e["dq"] = [stack.enter_context(nc.semaphore("d_%s%d" % (q, i))) for i in range(self.NDMA)]
            for s in e["dq"]:
                self.semobj[id(s)] = s
        self.bg = [stack.enter_context(nc.semaphore("bg%d" % i)) for i in range(4)]
        for b in self.bg:
            self.semobj[id(b)] = b
        self.pending_dma = {}
        self.cur = stack
        self.uid = 0

    def tile(self, shape, dt, name=None):
        self.uid += 1
        return Buf(self.cur.enter_context(self.nc.sbuf_tensor("%s_%d" % (name or "t", self.uid), list(shape), dt)))

    def psum(self, shape, dt=F32, name=None):
        self.uid += 1
        return Buf(self.cur.enter_context(self.nc.psum_tensor("%s_%d" % (name or "p", self.uid), list(shape), dt)))

    @contextmanager
    def phase(self):
        prev = self.cur
        with ExitStack() as st:
            self.cur = st
            yield
            self.barrier()
        self.cur = prev

    @staticmethod
    def _merge(out, d):
        for k, v in d.items():
            if out.get(k, 0) < v:
                out[k] = v

    def _deps(self, reads, writes):
        out = {}
        for b, key in reads:
            keys = list(b.st.keys()) if key is None else [key, None]
            for k in keys:
                st = b.st.get(k)
                if st:
                    self._merge(out, st[0])
        for b, key in writes:
            keys = list(b.st.keys()) if key is None else [key, None]
            for k in keys:
                st = b.st.get(k)
                if st:
                    self._merge(out, st[0])
                    self._merge(out, st[1])
        return out

    def _wait(self, ename, deps):
        e = self.E[ename]
        own = id(e["sem"])
        for sid, val in deps.items():
            if ename == "pe" and sid == own:
                continue
            if e["seen"].get(sid, 0) >= val:
                continue
            e["eng"].wait_ge(self.semobj[sid], val)
            e["seen"][sid] = val

    def _mark(self, reads, writes, sid, val):
        for b, key in reads:
            st = b.st.setdefault(key, [{}, {}])
            if st[1].get(sid, 0) < val:
                st[1][sid] = val
        for b, key in writes:
            if key is None:
                b.st = {None: [{sid: val}, {}]}
            else:
                b.st[key] = [{sid: val}, {}]

    @staticmethod
    def _norm(lst):
        return [(x, None) if isinstance(x, Buf) else x for x in lst]

    def op(self, ename, fn, reads=(), writes=()):
        reads = self._norm(reads)
        writes = self._norm(writes)
        e = self.E[ename]
        self._wait(ename, self._deps(reads, writes))
        ins = fn(e["eng"])
        e["cnt"] += 1
        ins.then_inc(e["sem"], 1)
        self._mark(reads, writes, id(e["sem"]), e["cnt"])
        return ins

    def dma(self, qname, fn, reads=(), writes=()):
        reads = self._norm(reads)
        writes = self._norm(writes)
        e = self.E[qname]
        slot = e["dn"] % self.NDMA
        val = (e["dn"] // self.NDMA + 1) * 16
        sem = e["dq"][slot]
        deps = self._deps(reads, writes)
        if val > 16:
            self._merge(deps, {id(sem): val - 16})
        self._wait(qname, deps)
        ins = fn(e["eng"])
        ins.then_inc(sem, 16)
        e["dn"] += 1
        self._mark(reads, writes, id(sem), val)
        self._merge(self.pending_dma, {id(sem): val})
        return ins

    def bg_dma(self, qname, fn, sem, n_prev):
        e = self.E[qname]
        ins = fn(e["eng"])
        ins.then_inc(sem, 16)
        return (n_prev + 1) * 16

    def wait_sem(self, enames, sem, val):
        for n in enames:
            self._wait(n, {id(sem): val})

    def barrier(self):
        allv = dict(self.pending_dma)
        for n, e in self.E.items():
            if e["cnt"]:
                allv[id(e["sem"])] = e["cnt"]
        for n in self.E:
            self._wait(n, allv)

    def load(self, t, src, q="sp", key=None):
        return self.dma(q, lambda e: e.dma_start(out=t, in_=src), writes=[(self._b, key)] if False else [])


class Rot:
    def __init__(self, S, n, shape, dt, psum=False, name=None):
        self.bufs = [(S.psum(shape, dt, name) if psum else S.tile(shape, dt, name)) for _ in range(n)]
        self.i = 0

    def next(self):
        b = self.bufs[self.i % len(self.bufs)]
        self.i += 1
        return b


def phase_mod(S, D, l):
    with S.phase():
        cT = S.tile([128, 8, 2], F32, "cT")
        cs = S.tile([128, 8, 2], F32, "cs")
        ones = S.tile([1, 2], F32, "ones")
        ab = S.tile([1, 6144], F32, "ab")
        wa = Rot(S, 4, [128, 8, 512], F32, name="wa")
        pm = Rot(S, 4, [2, 512], F32, psum=True, name="pm")
        mr = Rot(S, 4, [2, 512], F32, name="mr")
        S.dma("sp", lambda e: e.dma_start(out=cT[:], in_=D["cvecT"]), writes=[cT])
        S.dma("sp", lambda e: e.dma_start(out=ab[:], in_=D["ada_b"][l:l + 1, :]), writes=[ab])
        S.op("dve", lambda e: e.memset(ones[:], 1.0), writes=[ones])
        S.op("act", lambda e: e.activation(out=cs[:], in_=cT[:], func=AF.Silu), reads=[cT], writes=[cs])
        aw = D["ada_w"][l].rearrange("(k p) n -> p k n", p=128)
        wl = {}

        def ld(n):
            w_ = wa.next()
            S.dma("sp", lambda e: e.dma_start(out=w_[:], in_=aw[:, :, n * 512:(n + 1) * 512]), writes=[w_])
            wl[n] = w_

        for n in range(3):
            ld(n)
        for n in range(12):
            if n + 3 < 12:
                ld(n + 3)
            w = wl.pop(n)
            p = pm.next()
            for k in range(8):
                S.op("pe", lambda e: e.matmul(p[:], lhsT=cs[:, k, :], rhs=w[:, k, :], start=(k == 0), stop=False),
                     reads=[cs, w], writes=[p])
            S.op("pe", lambda e: e.matmul(p[:], lhsT=ones[:], rhs=ab[:, n * 512:(n + 1) * 512], start=False, stop=True),
                 reads=[ones, ab], writes=[p])
            m = mr.next()
            plus1 = 1.0 if n in (2, 3, 8, 9) else 0.0
            S.op("dve", lambda e: e.tensor_scalar(out=m[:], in0=p[:], scalar1=plus1, scalar2=None, op0=ALU.add),
                 reads=[p], writes=[m])
            S.dma("sp", lambda e: e.dma_start(out=D["modv"][l, :, n * 512:(n + 1) * 512], in_=m[:]), reads=[m])


def bcast_load(S, D, l, j, c0, n, name):
    t = S.tile([128, n], F32, name)
    S.dma("sp", lambda e: e.dma_start(out=t[:], in_=D["modv"][l, j, c0:c0 + n].partition_broadcast(128)), writes=[t])
    return t


def vec_bcast(S, src, n, name):
    t = S.tile([128, n], F32, name)
    S.dma("sp", lambda e: e.dma_start(out=t[:], in_=src.partition_broadcast(128)), writes=[t])
    return t


def phase_win(S, D, l, tiles=range(NTT), xin="xs", post_weights=None):
    with S.phase():
        wb = S.tile([128, 8, W_IN_COLS], BF16, "wb")
        wv = D["w_in"][l].rearrange("(k p) n -> p k n", p=128)
        for k in range(8):
            S.dma("pool", lambda e: e.dma_start(out=wb[:, k, :], in_=wv[:, k, :]), writes=[(wb, k)])
        if post_weights is not None:
            post_weights()
        scb = [bcast_load(S, D, l, j, 1024, 1024, "scb") for j in range(2)]
        shb = [bcast_load(S, D, l, j, 0, 1024, "shb") for j in range(2)]
        xt = Rot(S, 3, [128, 1024], F32, name="xt")
        hf = Rot(S, 2, [128, 1024], F32, name="hf")
        hb = Rot(S, 2, [128, 1024], BF16, name="hb")
        hT = Rot(S, 3, [128, 8, 128], BF16, name="hT")
        pT = Rot(S, 2, [128, 8, 128], BF16, psum=True, name="pT")
        pz = Rot(S, 4, [128, 512], F32, psum=True, name="pz")
        zs = Rot(S, 3, [128, 2048], F32, name="zs")
        ident = D["ident_bf"]
        cnt = 0
        def st_l(tt):
            j = 0 if tt < 32 else 1
            x = xt.next()
            S.dma("sp", lambda e: e.dma_start(out=x[:], in_=D[xin][tt * 128:(tt + 1) * 128, :]), writes=[x])
            return dict(tt=tt, j=j, x=x)

        def st_a(c_):
            tt, j, x = c_["tt"], c_["j"], c_["x"]
            h1 = hf.next()
            S.op("dve", lambda e: e.tensor_tensor(out=h1[:], in0=x[:], in1=scb[j][:], op=ALU.mult), reads=[x, scb[j]], writes=[h1])
            h2 = hb.next()
            S.op("dve", lambda e: e.tensor_tensor(out=h2[:], in0=h1[:], in1=shb[j][:], op=ALU.add), reads=[h1, shb[j]], writes=[h2])
            p = pT.next()
            for k in range(8):
                S.op("pe", lambda e: e.transpose(out=p[:, k, :], in_=h2[:, k * 128:(k + 1) * 128], identity=ident[:]),
                     reads=[h2, ident], writes=[(p, k)])
            t = hT.next()
            S.op("act", lambda e: e.copy(out=t[:], in_=p[:]), reads=[p], writes=[t])
            c_.update(t=t)

        def st_b(c_):
            nonlocal cnt
            tt, t = c_["tt"], c_["t"]
            for g0 in range(0, W_IN_COLS, 2048):
                gw = min(2048, W_IN_COLS - g0)
                zt = zs.next()
                for c0 in range(g0, g0 + gw, 512):
                    cw = min(512, W_IN_COLS - c0)
                    pp = pz.next()
                    for k in range(8):
                        S.op("pe", lambda e: e.matmul(pp[:, :cw], lhsT=t[:, k, :], rhs=wb[:, k, c0:c0 + cw],
                                                      start=(k == 0), stop=(k == 7)), reads=[t, wb], writes=[pp])
                    eng = "act" if cnt % 2 == 0 else "dve"
                    cnt += 1
                    if eng == "act":
                        S.op("act", lambda e: e.copy(out=zt[:, c0 - g0:c0 - g0 + cw], in_=pp[:, :cw]), reads=[pp], writes=[(zt, c0)])
                    else:
                        S.op("dve", lambda e: e.tensor_copy(out=zt[:, c0 - g0:c0 - g0 + cw], in_=pp[:, :cw]), reads=[pp], writes=[(zt, c0)])
                S.dma("sp", lambda e: e.dma_start(out=D["z"][tt * 128:(tt + 1) * 128, g0:g0 + gw], in_=zt[:, :gw]), reads=[zt])

        tiles = list(tiles)
        st_ = {}
        nj = len(tiles)
        for i in range(nj + 2):
            if i < nj:
                st_[i] = st_l(tiles[i])
            if 0 <= i - 1 < nj:
                st_a(st_[i - 1])
            if 0 <= i - 2 < nj:
                st_b(st_.pop(i - 2))


def layer_norm(S, xin, xdeps, n, gb, bb, out, tmp, small):
    nch = (n + 511) // 512
    for i in range(nch):
        w = min(512, n - i * 512)
        S.op("dve", lambda e: e.bn_stats(out=small[:, 8 + 6 * i:8 + 6 * i + 6], in_=xin[:, i * 512:i * 512 + w]),
             reads=xdeps, writes=[(small, "st%d" % i)])
    S.op("dve", lambda e: e.bn_aggr(out=small[:, 0:2], in_=small[:, 8:8 + 6 * nch]), reads=[small], writes=[(small, "mv")])
    mh = D_CONST["mhalf"]
    S.op("pool", lambda e: e.tensor_scalar(out=small[:, 2:3], in0=small[:, 1:2], scalar1=LN_EPS, scalar2=None, op0=ALU.add),
         reads=[(small, "mv")], writes=[(small, "ve")])
    S.op("pool", lambda e: e.tensor_tensor(out=small[:, 4:5], in0=small[:, 2:3], in1=mh[:, 0:1], op=ALU.pow),
         reads=[(small, "ve"), mh], writes=[(small, "rs")])
    S.op("dve", lambda e: e.tensor_scalar(out=tmp[:, :n], in0=xin, scalar1=small[:, 0:1], scalar2=small[:, 4:5],
                                          op0=ALU.subtract, op1=ALU.mult), reads=list(xdeps) + [small], writes=[tmp])
    S.op("pool", lambda e: e.tensor_tensor(out=tmp[:, :n], in0=tmp[:, :n], in1=gb[:, :n], op=ALU.mult), reads=[tmp, gb], writes=[tmp])
    S.op("pool", lambda e: e.tensor_tensor(out=out[:, :n], in0=tmp[:, :n], in1=bb[:, :n], op=ALU.add), reads=[tmp, bb], writes=[out])


def phase_conv(S, D, l, do_ctx=True):
    PAD = 15
    with S.phase():
        dw = S.tile([128, 2, 31], F32, "dw")
        cb = S.tile([128, 2], F32, "cb")
        S.dma("sp", lambda e: e.dma_start(out=dw[:], in_=D["conv_dwT"][l].rearrange("(c p) j -> p c j", p=128)), writes=[dw])
        S.dma("sp", lambda e: e.dma_start(out=cb[:], in_=D["conv_bT"][l]), writes=[cb])
        lng = vec_bcast(S, D["conv_ln_g"][l], 256, "lng")
        lnb = vec_bcast(S, D["conv_ln_b"][l], 256, "lnb")
        identf = D["ident_f"]
        yT = S.tile([128, 2, PAD + SEQ + PAD], F32, "yT")
        cv = S.tile([128, 2, SEQ], F32, "cv")
        zt = Rot(S, 4, [128, 512], F32, name="zt")
        sg = Rot(S, 4, [128, 256], F32, name="sg")
        yy = Rot(S, 4, [128, 256], F32, name="yy")
        pcs = Rot(S, 4, [128, 256], F32, name="pcs")
        pt = Rot(S, 2, [128, 2, 128], F32, psum=True, name="pt")
        pb = Rot(S, 2, [128, 256], F32, psum=True, name="pb")
        tmp = Rot(S, 2, [128, 256], F32, name="tmp")
        sm = Rot(S, 2, [128, 32], F32, name="sm")
        ln = Rot(S, 2, [128, 256], F32, name="ln")
        yo = Rot(S, 2, [128, 256], F32, name="yo")
        for (base, T) in ((0, SEQ), (SEQ, CTX)) if do_ctx else ((0, SEQ),):
            S.op("pool", lambda e: e.memset(yT[:, :, 0:PAD], 0.0), writes=[(yT, "padl")])
            S.op("pool", lambda e: e.memset(yT[:, :, PAD + T:PAD + T + PAD], 0.0), writes=[(yT, "padr")])
            ys_ = {}

            def a_front(i):
                r0 = base + i * 128
                z = zt.next()
                S.dma("sp", lambda e: e.dma_start(out=z[:], in_=D["z"][r0:r0 + 128, C_CONV:C_CONV + 512]), writes=[z])
                s = sg.next()
                S.op("act", lambda e: e.activation(out=s[:], in_=z[:, 256:512], func=AF.Sigmoid), reads=[z], writes=[s])
                y = yy.next()
                S.op("dve", lambda e: e.tensor_tensor(out=y[:], in0=z[:, 0:256], in1=s[:], op=ALU.mult), reads=[z, s], writes=[y])
                ys_[i] = y

            NT_ = T // 128
            for i in range(min(2, NT_)):
                a_front(i)
            for i in range(NT_):
                if i + 2 < NT_:
                    a_front(i + 2)
                y = ys_.pop(i)
                p = pt.next()
                for c in range(2):
                    S.op("pe", lambda e: e.transpose(out=p[:, c, :], in_=y[:, c * 128:(c + 1) * 128], identity=identf[:]),
                         reads=[y, identf], writes=[(p, c)])
                S.op("act", lambda e: e.copy(out=yT[:, :, PAD + i * 128:PAD + (i + 1) * 128], in_=p[:]), reads=[p], writes=[(yT, i)])
            CH = 1024 if T >= 1024 else T
            for t0 in range(0, T, CH):
                for c in range(2):
                    key = ("cv", t0, c)
                    S.op("dve", lambda e: e.tensor_scalar(out=cv[:, c, t0:t0 + CH], in0=yT[:, c, t0:t0 + CH], scalar1=dw[:, c, 0:1],
                                                          scalar2=cb[:, c:c + 1], op0=ALU.mult, op1=ALU.add),
                         reads=[yT, dw, cb], writes=[(cv, key)])
                    for j in range(1, 31):
                        S.op("dve", lambda e: e.scalar_tensor_tensor(out=cv[:, c, t0:t0 + CH], in0=yT[:, c, t0 + j:t0 + j + CH],
                                                                     scalar=dw[:, c, j:j + 1], in1=cv[:, c, t0:t0 + CH],
                                                                     op0=ALU.mult, op1=ALU.add),
                             reads=[yT, dw, (cv, key)], writes=[(cv, key)])
            ps_ = {}

            def c_front(i):
                p_ = pb.next()
                for c in range(2):
                    S.op("pe", lambda e: e.transpose(out=p_[:, c * 128:(c + 1) * 128], in_=cv[:, c, i * 128:(i + 1) * 128], identity=identf[:]),
                         reads=[cv, identf], writes=[(p_, c)])
                pc_ = pcs.next()
                S.op("act", lambda e: e.copy(out=pc_[:], in_=p_[:]), reads=[p_], writes=[pc_])
                ps_[i] = pc_

            for i in range(min(2, NT_)):
                c_front(i)
            for i in range(NT_):
                if i + 2 < NT_:
                    c_front(i + 2)
                r0 = base + i * 128
                p = ps_.pop(i)
                t = tmp.next(); s = sm.next(); o = ln.next()
                layer_norm(S, p[:, :], [p], 256, lng, lnb, o, t, s)
                y = yo.next()
                S.op("act", lambda e: e.activation(out=y[:], in_=o[:], func=AF.Silu), reads=[o], writes=[y])
                S.dma("sp", lambda e: e.dma_start(out=D["ycat"][r0:r0 + 128, 512:768], in_=y[:]), reads=[y])
            S.barrier()


def phase_sgu(S, D, l, tiles=range(NTT)):
    with S.phase():
        ws = S.tile([128, 4, 128], F32, "ws")
        bs = S.tile([128, 4], F32, "bs")
        S.dma("sp", lambda e: e.dma_start(out=ws[:], in_=D["sgu_wsT"][l].rearrange("g q p -> q g p")), writes=[ws])
        S.dma("sp", lambda e: e.dma_start(out=bs[:], in_=D["sgu_bsT"][l]), writes=[bs])
        lng = vec_bcast(S, D["sgu_ln_g"][l], 256, "lng")
        lnb = vec_bcast(S, D["sgu_ln_b"][l], 256, "lnb")
        zt = Rot(S, 4, [128, 512], F32, name="zt")
        ge = Rot(S, 4, [128, 512], F32, name="ge")
        tmp = Rot(S, 2, [128, 256], F32, name="tmp")
        sm = Rot(S, 2, [128, 32], F32, name="sm")
        vn = Rot(S, 2, [128, 256], F32, name="vn")
        ps = Rot(S, 2, [128, 256], F32, psum=True, name="ps")
        sb = Rot(S, 2, [128, 256], F32, name="sb")
        yo = Rot(S, 2, [128, 256], F32, name="yo")
        def st_a(tt):
            r0 = tt * 128
            z = zt.next()
            S.dma("sp", lambda e: e.dma_start(out=z[:], in_=D["z"][r0:r0 + 128, C_SGU:C_SGU + 512]), writes=[z])
            g = ge.next()
            S.op("act", lambda e: e.activation(out=g[:], in_=z[:], func=AF.Gelu_apprx_tanh), reads=[z], writes=[g])
            return (r0, g)

        tiles = list(tiles)
        pend = {}
        for i in range(len(tiles) + 2):
            if i < len(tiles):
                pend[i] = st_a(tiles[i])
            if i - 2 < 0:
                continue
            r0, g = pend.pop(i - 2)
            t = tmp.next(); s = sm.next(); v = vn.next()
            layer_norm(S, g[:, 256:512], [g], 256, lng, lnb, v, t, s)
            p = ps.next()
            for gi in range(4):
                S.op("pe", lambda e: e.matmul(p[:, gi * 64:(gi + 1) * 64], lhsT=ws[:, gi, :], rhs=v[:, gi * 64:(gi + 1) * 64],
                                              start=True, stop=True), reads=[ws, v], writes=[(p, gi)])
            sbt = sb.next()
            S.op("dve", lambda e: e.tensor_tensor(out=sbt[:].rearrange("p (g c) -> p g c", g=4), in0=p[:].rearrange("p (g c) -> p g c", g=4),
                                                  in1=bs[:].unsqueeze(2).to_broadcast([128, 4, 64]), op=ALU.add),
                 reads=[p, bs], writes=[sbt])
            y = yo.next()
            S.op("dve", lambda e: e.tensor_tensor(out=y[:], in0=sbt[:], in1=g[:, 0:256], op=ALU.mult), reads=[sbt, g], writes=[y])
            S.dma("sp", lambda e: e.dma_start(out=D["ycat"][r0:r0 + 128, 768:1024], in_=y[:]), reads=[y])


def phase_merge(S, D, l, tiles=range(NTT), xin="xs", xout="xs"):
    with S.phase():
        wbr = S.tile([128, 8, 1024], BF16, "wbr")
        wo = S.tile([128, 8, 1024], BF16, "wo")
        S.dma("pool", lambda e: e.dma_start(out=wbr[:], in_=D["w_branch"][l].rearrange("(k p) n -> p k n", p=128)), writes=[wbr])
        S.dma("pool", lambda e: e.dma_start(out=wo[:], in_=D["w_out"][l].rearrange("(k p) n -> p k n", p=128)), writes=[wo])
        g1b = [bcast_load(S, D, l, j, 2048, 1024, "g1b") for j in range(2)]
        lng = vec_bcast(S, D["ln1_g"][l], 1024, "lng")
        lnb = vec_bcast(S, D["ln1_b"][l], 1024, "lnb")
        ident = D["ident_bf"]
        yt = Rot(S, 3, [128, 1024], F32, name="yt")
        yb = Rot(S, 2, [128, 1024], BF16, name="yb")
        yT = Rot(S, 2, [128, 8, 128], BF16, name="yT")
        gt = Rot(S, 3, [128, 4096], F32, name="gt")
        sg = Rot(S, 2, [128, 1024], F32, name="sg")
        tm = Rot(S, 2, [128, 1024], F32, name="tm")
        mg = Rot(S, 1, [128, 1024], F32, name="mg")
        mb = Rot(S, 3, [128, 1024], BF16, name="mb")
        mT = Rot(S, 2, [128, 8, 128], BF16, name="mT")
        xt = Rot(S, 4, [128, 1024], F32, name="xt")
        rr = Rot(S, 2, [128, 1024], F32, name="rr")
        sm = Rot(S, 2, [128, 32], F32, name="sm")
        xo = Rot(S, 2, [128, 1024], F32, name="xo")
        pT = Rot(S, 2, [128, 8, 128], BF16, psum=True, name="pT")
        pP = Rot(S, 2, [128, 1024], F32, psum=True, name="pP")
        pO = Rot(S, 1, [128, 1024], F32, psum=True, name="pO")
        def st_a(tt):
            j = 0 if tt < 32 else 1
            r0 = tt * 128
            y = yt.next()
            S.dma("sp", lambda e: e.dma_start(out=y[:], in_=D["ycat"][r0:r0 + 128, :]), writes=[y])
            g = gt.next()
            S.dma("sp", lambda e: e.dma_start(out=g[:], in_=D["z"][r0:r0 + 128, C_GATE:C_GATE + 4096]), writes=[g])
            x = xt.next()
            S.dma("sp", lambda e: e.dma_start(out=x[:], in_=D[xin][r0:r0 + 128, :]), writes=[x])
            return dict(j=j, r0=r0, y=y, g=g, x=x)

        def st_b(c_):
            j, r0, y, g, x = (c_[k] for k in ("j", "r0", "y", "g", "x"))
            b = yb.next()
            S.op("dve", lambda e: e.tensor_copy(out=b[:], in_=y[:]), reads=[y], writes=[b])
            p = pT.next()
            for k in range(8):
                S.op("pe", lambda e: e.transpose(out=p[:, k, :], in_=b[:, k * 128:(k + 1) * 128], identity=ident[:]),
                     reads=[b, ident], writes=[(p, k)])
            t = yT.next()
            S.op("act", lambda e: e.copy(out=t[:], in_=p[:]), reads=[p], writes=[t])
            m = mg.next()
            mbt = mb.next()
            for i in range(4):
                pp = pP.next()
                for half in range(2):
                    for kk in range(2):
                        S.op("pe", lambda e: e.matmul(pp[:, half * 512:(half + 1) * 512], lhsT=t[:, i * 2 + kk, :],
                                                      rhs=wbr[:, i * 2 + kk, half * 512:(half + 1) * 512], start=(kk == 0), stop=(kk == 1)),
                             reads=[t, wbr], writes=[(pp, half)])
                s = sg.next()
                S.op("act", lambda e: e.activation(out=s[:], in_=g[:, i * 1024:(i + 1) * 1024], func=AF.Sigmoid), reads=[g], writes=[s])
                if i == 0:
                    S.op("dve", lambda e: e.tensor_tensor(out=m[:], in0=pp[:], in1=s[:], op=ALU.mult), reads=[pp, s], writes=[m])
                else:
                    tmp = tm.next()
                    S.op("dve", lambda e: e.tensor_tensor(out=tmp[:], in0=pp[:], in1=s[:], op=ALU.mult), reads=[pp, s], writes=[tmp])
                    dst = mbt if i == 3 else m
                    S.op("pool", lambda e: e.tensor_tensor(out=dst[:], in0=m[:], in1=tmp[:], op=ALU.add), reads=[m, tmp], writes=[dst])
            c_.update(mbt=mbt)

        def st_c(c_):
            j, r0, x, mbt = (c_[k] for k in ("j", "r0", "x", "mbt"))
            p = pT.next()
            for k in range(8):
                S.op("pe", lambda e: e.transpose(out=p[:, k, :], in_=mbt[:, k * 128:(k + 1) * 128], identity=ident[:]),
                     reads=[mbt, ident], writes=[(p, k)])
            t2 = mT.next()
            S.op("act", lambda e: e.copy(out=t2[:], in_=p[:]), reads=[p], writes=[t2])
            po = pO.next()
            for half in range(2):
                for k in range(8):
                    S.op("pe", lambda e: e.matmul(po[:, half * 512:(half + 1) * 512], lhsT=t2[:, k, :],
                                                  rhs=wo[:, k, half * 512:(half + 1) * 512], start=(k == 0), stop=(k == 7)),
                         reads=[t2, wo], writes=[(po, half)])
            r = rr.next()
            S.op("dve", lambda e: e.tensor_tensor(out=r[:], in0=po[:], in1=g1b[j][:], op=ALU.mult), reads=[po, g1b[j]], writes=[r])
            S.op("dve", lambda e: e.scalar_tensor_tensor(out=r[:], in0=x[:], scalar=ALPHA, in1=r[:], op0=ALU.mult, op1=ALU.add),
                 reads=[x, r], writes=[r])
            tmp = tm.next(); s = sm.next(); o = xo.next()
            layer_norm(S, r[:, :], [r], 1024, lng, lnb, o, tmp, s)
            S.dma("sp", lambda e: e.dma_start(out=D[xout][r0:r0 + 128, :], in_=o[:]), reads=[o])

        tiles = list(tiles)
        st_ = {}
        nj = len(tiles)
        for i in range(nj + 2):
            if i < nj:
                st_[i] = st_a(tiles[i])
            if 0 <= i - 1 < nj:
                st_b(st_[i - 1])
            if 0 <= i - 2 < nj:
                st_c(st_.pop(i - 2))


def phase_na(S, D, l, need_ctx=True, rows=range(64), heads=range(4)):
    scale = 64 ** -0.5
    with S.phase():
        ident = D["ident_bf"]
        qkT = S.tile([128, 4, NTOK], BF16, "qkT")
        va = S.tile([128, 32, 256], BF16, "va")
        vb = S.tile([128, 31, 256], BF16, "vb")
        vc = S.tile([128, 2, 256], BF16, "vc")
        zv = D["z"]
        S.dma("pool", lambda e: e.dma_start(out=va[:], in_=zv[0:4096, 512:768].rearrange("(t p) c -> p t c", p=128)), writes=[va])
        S.dma("pool", lambda e: e.dma_start(out=vb[:], in_=zv[64:64 + 31 * 128, 512:768].rearrange("(t p) c -> p t c", p=128)), writes=[vb])
        S.dma("pool", lambda e: e.dma_start(out=vc[:], in_=zv[4096:4352, 512:768].rearrange("(t p) c -> p t c", p=128)), writes=[vc])
        with S.phase():
            qk = Rot(S, 3, [128, 512], BF16, name="qk")
            pq = Rot(S, 2, [128, 4, 128], BF16, psum=True, name="pq")
            for tt in range(NTT):
                t = qk.next()
                S.dma("pool", lambda e: e.dma_start(out=t[:], in_=zv[tt * 128:(tt + 1) * 128, 0:512]), writes=[t])
                p = pq.next()
                for c in range(4):
                    S.op("pe", lambda e: e.transpose(out=p[:, c, :], in_=t[:, c * 128:(c + 1) * 128], identity=ident[:]),
                         reads=[t, ident], writes=[(p, c)])
                S.op("act", lambda e: e.copy(out=qkT[:, :, tt * 128:(tt + 1) * 128], in_=p[:]), reads=[p], writes=[(qkT, tt)])
        bias = Rot(S, 4, [64, 8, 512], F32, name="bias")
        ps1 = Rot(S, 2, [128, 512], F32, psum=True, name="ps1")
        ps2 = Rot(S, 2, [128, 256], F32, psum=True, name="ps2")
        ppT = Rot(S, 2, [128, 6, 128], BF16, psum=True, name="ppT")
        po = Rot(S, 2, [128, 64], F32, psum=True, name="po")
        sc = Rot(S, 3, [128, 768], F32, name="sc")
        pe_ = Rot(S, 3, [128, 768], BF16, name="pexp")
        pT = Rot(S, 3, [128, 6, 128], BF16, name="pT")
        sm = Rot(S, 6, [128, 4], F32, name="sm")
        yo = Rot(S, 4, [128, 64], F32, name="yo")

        def att_s1(h, M, qcols, kwin, cls, vchunks, bt, out_ap):
            hp, pb = h // 2, (h % 2) * 64
            qTs = qkT[pb:pb + 64, hp, qcols[0]:qcols[1]]
            s = sc.next(); nk = 0
            if kwin is not None:
                p1 = ps1.next()
                S.op("pe", lambda e: e.matmul(p1[:M, :], lhsT=qTs, rhs=qkT[pb:pb + 64, 2 + hp, kwin:kwin + 512], start=True, stop=True),
                     reads=[qkT], writes=[p1])
                S.op("dve", lambda e: e.scalar_tensor_tensor(out=s[:M, 0:512], in0=p1[:M, :], scalar=scale, in1=bt[:M, cls, :],
                                                             op0=ALU.mult, op1=ALU.add), reads=[p1, bt], writes=[(s, "w")])
                nk = 512
            p2 = ps2.next()
            S.op("pe", lambda e: e.matmul(p2[:M, :], lhsT=qTs, rhs=qkT[pb:pb + 64, 2 + hp, 4096:4352], start=True, stop=True),
                 reads=[qkT], writes=[p2])
            S.op("act", lambda e: e.mul(out=s[:M, nk:nk + 256], in_=p2[:M, :], mul=scale), reads=[p2], writes=[(s, "c")])
            nk += 256
            m = sm.next()
            S.op("dve", lambda e: e.tensor_reduce(out=m[:M, 0:1], in_=s[:M, :nk], axis=AX.X, op=ALU.max, negate=True),
                 reads=[s], writes=[(m, 0)])
            return dict(h=h, M=M, vchunks=vchunks, out_ap=out_ap, s=s, m=m, nk=nk)

        def att_s2(c_):
            M, s, m, nk = c_["M"], c_["s"], c_["m"], c_["nk"]
            pe = pe_.next()
            S.op("act", lambda e: e.activation(out=pe[:M, :nk], in_=s[:M, :nk], func=AF.Exp, bias=m[:M, 0:1], scale=1.0,
                                               accum_out=m[:M, 1:2]), reads=[s, (m, 0)], writes=[pe, (m, 1)])
            nch = nk // 128
            pp = ppT.next()
            for kc in range(nch):
                S.op("pe", lambda e: e.transpose(out=pp[:, kc, :M], in_=pe[:M, kc * 128:(kc + 1) * 128], identity=ident[:M, :M]),
                     reads=[pe, ident], writes=[(pp, kc)])
            pt = pT.next()
            S.op("dve", lambda e: e.tensor_copy(out=pt[:, :nch, :M], in_=pp[:, :nch, :M]), reads=[pp], writes=[pt])
            c_.update(pt=pt, nch=nch)

        def att_s3(c_):
            h, M, vchunks, out_ap, m, pt, nch = (c_[k] for k in ("h", "M", "vchunks", "out_ap", "m", "pt", "nch"))
            o = po.next()
            for kc in range(nch):
                vbuf, vi = vchunks[kc]
                S.op("pe", lambda e: e.matmul(o[:M, :], lhsT=pt[:, kc, :M], rhs=vbuf[:, vi, h * 64:(h + 1) * 64],
                                              start=(kc == 0), stop=(kc == nch - 1)), reads=[pt, vbuf], writes=[o])
            S.op("dve", lambda e: e.reciprocal(out=m[:M, 2:3], in_=m[:M, 1:2]), reads=[(m, 1)], writes=[(m, 2)])
            y = yo.next()
            S.op("dve", lambda e: e.tensor_scalar(out=y[:M, :], in0=o[:M, :], scalar1=m[:M, 2:3], scalar2=None, op0=ALU.mult),
                 reads=[o, (m, 2)], writes=[y])
            S.dma("sp", lambda e: e.dma_start(out=out_ap, in_=y[:M, :]), reads=[y])

        jobs = []
        for h in heads:
            bt = bias.next()
            S.dma("sp", lambda e: e.dma_start(out=bt[:], in_=D["na_bias"][l, h].rearrange("c q k -> q c k")), writes=[bt])
            for r in rows:
                r0 = min(max(r - 4, 0), 56)
                cls = r if r < 4 else (4 if r <= 60 else r - 56)
                if r0 % 2 == 0:
                    vch = [(va, r0 // 2 + jj) for jj in range(4)]
                else:
                    vch = [(vb, (r0 - 1) // 2 + jj) for jj in range(4)]
                vch += [(vc, 0), (vc, 1)]
                jobs.append((h, 64, (r * 64, (r + 1) * 64), r0 * 64, cls, vch, bt,
                             D["ycat"][r * 64:(r + 1) * 64, h * 64:(h + 1) * 64]))
            if need_ctx:
                for ct in range(2):
                    jobs.append((h, 128, (4096 + ct * 128, 4096 + (ct + 1) * 128), None, None, [(vc, 0), (vc, 1)], None,
                                 D["ycat"][4096 + ct * 128:4096 + (ct + 1) * 128, h * 64:(h + 1) * 64]))
        st_ = {}
        nj = len(jobs)
        for i in range(nj + 2):
            if i < nj:
                st_[i] = att_s1(*jobs[i])
            if 0 <= i - 1 < nj:
                att_s2(st_[i - 1])
            if 0 <= i - 2 < nj:
                att_s3(st_.pop(i - 2))


def phase_gla(S, D, l, need_ctx=True, lat_chunks=64):
    qscale = 32 ** -0.5
    with S.phase():
        identf = D["ident_f"]
        identb = D["ident_bf"]
        ropeC = S.tile([64, 64, 2, 8], F32, "ropeC")
        ropeS = S.tile([64, 64, 2, 8], F32, "ropeS")
        mt = S.tile([64, 2, 64], F32, "mt")
        tri = S.tile([64, 2, 64], F32, "tri")
        blk = S.tile([128, 4, 64], F32, "blk")
        gw = S.tile([33, 2, 128], F32, "gw")
        ngb = vec_bcast(S, D["gla_norm_g"][l], 256, "ngb")
        for t, src in ((ropeC, D["ropeC"]), (ropeS, D["ropeS"]), (mt, D["gla_mt"]), (tri, D["gla_tri"]), (blk, D["gla_blk"]),
                       (gw, D["gla_gw"][l].rearrange("d k n -> k d n"))):
            S.dma("sp", lambda e: e.dma_start(out=t[:], in_=src), writes=[t])
        Sblk = S.tile([128, 256], F32, "Sblk")
        Sbf = S.tile([128, 256], BF16, "Sbf")
        zc_r = Rot(S, 10, [64, 800], F32, name="zc")
        qkr = Rot(S, 2, [64, 256], F32, name="qkr")
        rt = Rot(S, 4, [64, 8, 2, 8], F32, name="rt")
        loT = Rot(S, 3, [33, 64], F32, name="loT")
        for b in loT.bufs:
            S.op("pool", lambda e: e.memset(b[32:33, :], 1.0), writes=[(b, "one")])
        e1 = Rot(S, 2, [64, 128], F32, name="e1")
        sp = Rot(S, 2, [64, 128], F32, name="sp")
        eb = Rot(S, 5, [128, 64], F32, name="eb")
        enb = Rot(S, 3, [128, 64], F32, name="enb")
        qs = Rot(S, 3, [128, 64], BF16, name="qs")
        ks = Rot(S, 2, [128, 64], BF16, name="ks")
        ke = Rot(S, 2, [128, 64], BF16, name="ke")
        qb = Rot(S, 2, [128, 4, 64], BF16, name="qb")
        kend = Rot(S, 3, [64, 128], BF16, name="kend")
        am = Rot(S, 3, [64, 4, 64], BF16, name="am")
        vbf = Rot(S, 3, [64, 256], BF16, name="vbf")
        tu = Rot(S, 4, [128, 256], F32, name="tu")
        of_ = Rot(S, 10, [64, 256], F32, name="of")
        osum = Rot(S, 2, [64, 256], F32, name="osum")
        sq = Rot(S, 2, [64, 256], F32, name="sq")
        ms = Rot(S, 2, [64, 16], F32, name="ms")
        sr = Rot(S, 2, [64, 256], F32, name="sr")
        yo = Rot(S, 2, [64, 256], F32, name="yo")
        bankA = Rot(S, 3, [128, 512], F32, psum=True, name="bankA")
        pkT = Rot(S, 1, [64, 128], BF16, psum=True, name="pkT")
        pA = Rot(S, 1, [64, 256], F32, psum=True, name="pA")
        pO = Rot(S, 2, [64, 256], F32, psum=True, name="pO")
        pU = Rot(S, 1, [128, 256], F32, psum=True, name="pU")

        def chunk_l(tok0, latent, n, d, want_out):
            z = zc_r.next()
            S.dma("sp", lambda e: e.dma_start(out=z[:], in_=D["z"][tok0:tok0 + 64, C_GQ:C_GQ + 800]), writes=[z])
            o_pre = None
            if want_out and d == 1:
                o_pre = of_.next()
                S.dma("sp", lambda en: en.dma_start(out=o_pre[:], in_=D["ofwd"][tok0:tok0 + 64, :]), writes=[o_pre])
            return dict(tok0=tok0, latent=latent, n=n, d=d, want_out=want_out, z=z, o_pre=o_pre)

        def chunk_a1(c_):
            tok0, latent, n, d, want_out, z = (c_[k] for k in ("tok0", "latent", "n", "d", "want_out", "z"))
            if latent:
                q = qkr.next()
                zv = z[:, 0:256].rearrange("p (h a b f) -> p h a b f", h=8, a=2, b=2)
                qv = q[:].rearrange("p (h a b f) -> p h a b f", h=8, a=2, b=2)
                xa, xb_ = zv[:, :, :, 0, :], zv[:, :, :, 1, :]
                cosb = ropeC[:, n, :, :].unsqueeze(1).to_broadcast([64, 8, 2, 8])
                sinb = ropeS[:, n, :, :].unsqueeze(1).to_broadcast([64, 8, 2, 8])
                t1, t2, t3, t4 = rt.next(), rt.next(), rt.next(), rt.next()
                S.op("dve", lambda e: e.tensor_tensor(out=t1[:], in0=xa, in1=cosb, op=ALU.mult), reads=[z, ropeC], writes=[t1])
                S.op("pool", lambda e: e.tensor_tensor(out=t2[:], in0=xb_, in1=sinb, op=ALU.mult), reads=[z, ropeS], writes=[t2])
                S.op("pool", lambda e: e.tensor_tensor(out=t3[:], in0=xa, in1=sinb, op=ALU.mult), reads=[z, ropeS], writes=[t3])
                S.op("dve", lambda e: e.tensor_tensor(out=t4[:], in0=xb_, in1=cosb, op=ALU.mult), reads=[z, ropeC], writes=[t4])
                S.op("dve", lambda e: e.tensor_tensor(out=qv[:, :, :, 0, :], in0=t1[:], in1=t2[:], op=ALU.subtract), reads=[t1, t2], writes=[(q, "a")])
                S.op("pool", lambda e: e.tensor_tensor(out=qv[:, :, :, 1, :], in0=t3[:], in1=t4[:], op=ALU.add), reads=[t3, t4], writes=[(q, "b")])
                qsrc, qdep = q, q
            else:
                qsrc, qdep = z, z
            A = bankA.next()
            for c in range(2):
                S.op("pe", lambda e: e.transpose(out=A[:, c * 64:(c + 1) * 64], in_=qsrc[:, c * 128:(c + 1) * 128], identity=identf[:64, :64]),
                     reads=[qdep, identf], writes=[(A, "qk%d" % c)])
            S.op("pe", lambda e: e.transpose(out=A[0:32, 320:384], in_=z[:, 768:800], identity=identf[:64, :64]),
                 reads=[z, identf], writes=[(A, "lo")])
            lt = loT.next()
            S.op("act", lambda e: e.copy(out=lt[0:32, :], in_=A[0:32, 320:384]), reads=[(A, "lo")], writes=[(lt, "d")])
            c_.update(A=A, lt=lt)

        def chunk_a2(c_):
            d, A, lt = c_["d"], c_["A"], c_["lt"]
            S.op("pe", lambda e: e.matmul(A[0:64, 192:320], lhsT=lt[:, :], rhs=gw[:, d, :], start=True, stop=True),
                 reads=[lt, gw], writes=[(A, "g")])
            e = e1.next()
            S.op("act", lambda en: en.activation(out=e[:], in_=A[0:64, 192:320], func=AF.Exp, scale=-1.0), reads=[(A, "g")], writes=[e])
            s_ = sp.next()
            S.op("act", lambda en: en.activation(out=s_[:], in_=e[:], func=AF.Ln, bias=1.0, scale=1.0), reads=[e], writes=[s_])
            S.op("pe", lambda en: en.matmul(A[:, 128:192], lhsT=s_[:, :], rhs=mt[:, d, :], start=True, stop=True),
                 reads=[s_, mt], writes=[(A, "b")])
            ebt = eb.next(); enbt = enb.next()
            S.op("act", lambda en: en.activation(out=ebt[:], in_=A[:, 128:192], func=AF.Exp), reads=[(A, "b")], writes=[ebt])
            S.op("act", lambda en: en.activation(out=enbt[:], in_=A[:, 128:192], func=AF.Exp, scale=-1.0), reads=[(A, "b")], writes=[enbt])
            dec = ebt[:, 63:64] if d == 0 else ebt[:, 0:1]
            c_.update(ebt=ebt, enbt=enbt, dec=dec)

        def chunk_a3(c_):
            d, A, z, ebt, enbt, dec = (c_[k] for k in ("d", "A", "z", "ebt", "enbt", "dec"))
            qst = qs.next(); kst = ks.next(); ket = ke.next(); qbt = qb.next()
            S.op("dve", lambda en: en.scalar_tensor_tensor(out=qst[:], in0=A[:, 0:64], scalar=qscale, in1=ebt[:], op0=ALU.mult, op1=ALU.mult),
                 reads=[(A, "qk0"), ebt], writes=[qst])
            S.op("dve", lambda en: en.tensor_tensor(out=kst[:], in0=A[:, 64:128], in1=enbt[:], op=ALU.mult), reads=[(A, "qk1"), enbt], writes=[kst])
            S.op("dve", lambda en: en.scalar_tensor_tensor(out=ket[:], in0=A[:, 64:128], scalar=dec, in1=enbt[:], op0=ALU.mult, op1=ALU.mult),
                 reads=[(A, "qk1"), enbt, ebt], writes=[ket])
            S.op("pool", lambda en: en.tensor_tensor(out=qbt[:], in0=qst[:].unsqueeze(1).to_broadcast([128, 4, 64]), in1=blk[:], op=ALU.mult),
                 reads=[qst, blk], writes=[qbt])
            pk = pkT.next()
            S.op("pe", lambda en: en.transpose(out=pk[:], in_=ket[:], identity=identb[:]), reads=[ket, identb], writes=[pk])
            kt = kend.next()
            S.op("act", lambda en: en.copy(out=kt[:], in_=pk[:]), reads=[pk], writes=[kt])
            v = vbf.next()
            S.op("act", lambda en: en.copy(out=v[:], in_=z[:, 256:512]), reads=[z], writes=[v])
            pa = pA.next()
            S.op("pe", lambda en: en.matmul(pa[:], lhsT=kst[:], rhs=qbt[:].rearrange("p h c -> p (h c)"), start=True, stop=True),
                 reads=[kst, qbt], writes=[pa])
            amt = am.next()
            S.op("dve", lambda en: en.tensor_tensor(out=amt[:], in0=pa[:].rearrange("p (h c) -> p h c", h=4),
                                                   in1=tri[:, d, :].unsqueeze(1).to_broadcast([64, 4, 64]), op=ALU.mult),
                 reads=[pa, tri], writes=[amt])
            pu = pU.next()
            S.op("pe", lambda en: en.matmul(pu[:], lhsT=kt[:], rhs=v[:], start=True, stop=True), reads=[kt, v], writes=[pu])
            tut = tu.next()
            S.op("dve", lambda en: en.tensor_tensor(out=tut[:], in0=pu[:], in1=blk[:].rearrange("p h c -> p (h c)"), op=ALU.mult),
                 reads=[pu, blk], writes=[tut])
            c_.update(qst=qst, kt=kt, amt=amt, v=v, tut=tut)

        def chunk_b(c_):
            tok0, d, want_out, z, ebt, dec, qst, kt, amt, v = (c_[k] for k in ("tok0", "d", "want_out", "z", "ebt", "dec", "qst", "kt", "amt", "v"))
            tut = c_["tut"]
            po = pO.next()
            S.op("pe", lambda en: en.matmul(po[:], lhsT=qst[:], rhs=Sbf[:], start=True, stop=False, skip_group_check=True),
                 reads=[qst, Sbf], writes=[po])
            for h in range(4):
                S.op("pe", lambda en: en.matmul(po[:, h * 64:(h + 1) * 64], lhsT=amt[:, h, :], rhs=v[:, h * 64:(h + 1) * 64],
                                               start=False, stop=True, skip_group_check=True), reads=[amt, v], writes=[po])
            S.op("dve", lambda en: en.scalar_tensor_tensor(out=Sblk[:], in0=Sblk[:], scalar=dec, in1=tut[:], op0=ALU.mult, op1=ALU.add),
                 reads=[Sblk, ebt, tut], writes=[Sblk])
            S.op("act", lambda en: en.copy(out=Sbf[:], in_=Sblk[:]), reads=[Sblk], writes=[Sbf])
            if not want_out:
                return
            if d == 0:
                o = of_.next()
                S.op("act", lambda en: en.copy(out=o[:], in_=po[:]), reads=[po], writes=[o])
                S.dma("sp", lambda en: en.dma_start(out=D["ofwd"][tok0:tok0 + 64, :], in_=o[:]), reads=[o])
                return
            o = c_["o_pre"]
            os_ = osum.next()
            S.op("dve", lambda en: en.tensor_tensor(out=os_[:], in0=po[:], in1=o[:], op=ALU.add), reads=[po, o], writes=[os_])
            q2 = sq.next()
            S.op("pool", lambda en: en.tensor_tensor(out=q2[:], in0=os_[:], in1=os_[:], op=ALU.mult), reads=[os_], writes=[q2])
            m = ms.next()
            S.op("dve", lambda en: en.tensor_reduce(out=m[:, 0:4], in_=q2[:].rearrange("p (h c) -> p h c", h=4), axis=AX.X, op=ALU.add),
                 reads=[q2], writes=[(m, 0)])
            S.op("dve", lambda en: en.tensor_scalar(out=m[:, 4:8], in0=m[:, 0:4], scalar1=1.0 / 64, scalar2=LN_EPS, op0=ALU.mult, op1=ALU.add),
                 reads=[(m, 0)], writes=[(m, 1)])
            S.op("act", lambda en: en.activation(out=m[:, 8:12], in_=m[:, 4:8], func=AF.Ln), reads=[(m, 1)], writes=[(m, 2)])
            S.op("act", lambda en: en.activation(out=m[:, 12:16], in_=m[:, 8:12], func=AF.Exp, scale=-0.5), reads=[(m, 2)], writes=[(m, 3)])
            S.op("dve", lambda en: en.tensor_tensor(out=os_[:].rearrange("p (h c) -> p h c", h=4), in0=os_[:].rearrange("p (h c) -> p h c", h=4),
                                                   in1=m[:, 12:16].unsqueeze(2).to_broadcast([64, 4, 64]), op=ALU.mult),
                 reads=[os_, (m, 3)], writes=[os_])
            S.op("pool", lambda en: en.tensor_tensor(out=os_[:], in0=os_[:], in1=ngb[0:64, :], op=ALU.mult), reads=[os_, ngb], writes=[os_])
            srt = sr.next()
            S.op("act", lambda en: en.activation(out=srt[:], in_=z[:, 512:768], func=AF.Exp, scale=-1.0), reads=[z], writes=[srt])
            S.op("pool", lambda en: en.tensor_scalar(out=srt[:], in0=srt[:], scalar1=1.0, scalar2=None, op0=ALU.add), reads=[srt], writes=[srt])
            S.op("dve", lambda en: en.reciprocal(out=srt[:], in_=srt[:]), reads=[srt], writes=[srt])
            S.op("pool", lambda en: en.tensor_tensor(out=srt[:], in0=srt[:], in1=z[:, 512:768], op=ALU.mult), reads=[srt, z], writes=[srt])
            y = yo.next()
            S.op("dve", lambda en: en.tensor_tensor(out=y[:], in0=os_[:], in1=srt[:], op=ALU.mult), reads=[os_, srt], writes=[y])
            S.dma("sp", lambda en: en.dma_start(out=D["ycat"][tok0:tok0 + 64, 256:512], in_=y[:]), reads=[y])

        for d in range(2):
            S.op("pool", lambda en: en.memset(Sblk[:], 0.0), writes=[Sblk])
            S.op("pool", lambda en: en.memset(Sbf[:], 0.0), writes=[Sbf])
            order = list(range(4)) if d == 0 else list(range(3, -1, -1))
            jobs = [(SEQ + 64 * i, False, 0, d, need_ctx) for i in order]
            order = list(range(lat_chunks)) if d == 0 else list(range(lat_chunks - 1, -1, -1))
            jobs += [(64 * i, True, i, d, True) for i in order]
            st_ = {}
            nj = len(jobs)
            PF = 4
            for i in range(nj + 3 + PF):
                if i < nj:
                    st_[i] = chunk_l(*jobs[i])
                if 0 <= i - PF < nj:
                    chunk_a1(st_[i - PF])
                if 0 <= i - PF - 1 < nj:
                    chunk_a2(st_[i - PF - 1])
                if 0 <= i - PF - 2 < nj:
                    chunk_a3(st_[i - PF - 2])
                if 0 <= i - PF - 3 < nj:
                    chunk_b(st_.pop(i - PF - 3))
            S.barrier()


def phase_peer_topk(S, D, l, tiles=range(NTT), xin="xs"):
    with S.phase():
        ident = D["ident_bf"]
        wq = S.tile([128, 8, 2048], BF16, "wq")
        S.dma("pool", lambda e: e.dma_start(out=wq[:], in_=D["peer_wq"][l].rearrange("(k p) n -> p k n", p=128)), writes=[wq])
        kT = S.tile([128, 16, 128], F32, "kT")
        S.dma("sp", lambda e: e.dma_start(out=kT[:], in_=D["peer_keysT"][l].rearrange("b d k -> d b k")), writes=[kT])
        iota = S.tile([128, 16, 16], F32, "iota")
        S.dma("sp", lambda e: e.dma_start(out=iota[:], in_=D["iota_kk"]), writes=[iota])
        scb = [bcast_load(S, D, l, j, 4096, 1024, "scb") for j in range(2)]
        shb = [bcast_load(S, D, l, j, 3072, 1024, "shb") for j in range(2)]
        xt = Rot(S, 2, [128, 1024], F32, name="xt")
        hf = Rot(S, 1, [128, 1024], F32, name="hf")
        hb = Rot(S, 2, [128, 1024], BF16, name="hb")
        hT = Rot(S, 2, [128, 8, 128], BF16, name="hT")
        qT = Rot(S, 2, [128, 16, 128], F32, name="qT")
        sc = Rot(S, 3, [128, 16, 128], F32, name="sc")
        wk16 = Rot(S, 1, [128, 16, 128], F32, name="wk16")
        wk8 = Rot(S, 1, [128, 8, 256], F32, name="wk8")
        t1 = Rot(S, 3, [128, 16, 16], F32, name="t1")
        ti = Rot(S, 3, [128, 16, 16], U32, name="ti")
        tif = Rot(S, 3, [128, 16, 16], F32, name="tif")
        cs = Rot(S, 2, [128, 8, 256], F32, name="cs")
        bs = Rot(S, 3, [128, 8, 16], F32, name="bs")
        bj = Rot(S, 3, [128, 8, 16], U32, name="bj")
        hi = Rot(S, 2, [128, 8, 16], U32, name="hi")
        lo = Rot(S, 2, [128, 8, 16], U32, name="lo")
        hif = Rot(S, 2, [128, 8, 16], F32, name="hif")
        lof = Rot(S, 2, [128, 8, 16], F32, name="lof")
        oh = Rot(S, 2, [128, 8, 16, 16], F32, name="oh")
        ee = Rot(S, 4, [128, 8, 16], F32, name="ee")
        ei = Rot(S, 2, [128, 128], I32, name="ei")
        gg = Rot(S, 2, [128, 8, 16], F32, name="gg")
        sm = Rot(S, 2, [128, 32], F32, name="sm")
        pT = Rot(S, 2, [128, 8, 128], BF16, psum=True, name="pT")
        pq = Rot(S, 3, [128, 4, 128], F32, psum=True, name="pq")
        def stage_a(tt):
            j = 0 if tt < 32 else 1
            r0 = tt * 128
            x = xt.next()
            S.dma("sp", lambda e: e.dma_start(out=x[:], in_=D[xin][r0:r0 + 128, :]), writes=[x])
            h1 = hf.next()
            S.op("pool", lambda e: e.tensor_tensor(out=h1[:], in0=x[:], in1=scb[j][:], op=ALU.mult), reads=[x, scb[j]], writes=[h1])
            h2 = hb.next()
            S.op("pool", lambda e: e.tensor_tensor(out=h2[:], in0=h1[:], in1=shb[j][:], op=ALU.add), reads=[h1, shb[j]], writes=[h2])
            p = pT.next()
            for k in range(8):
                S.op("pe", lambda e: e.transpose(out=p[:, k, :], in_=h2[:, k * 128:(k + 1) * 128], identity=ident[:]),
                     reads=[h2, ident], writes=[(p, k)])
            t = hT.next()
            S.op("act", lambda e: e.copy(out=t[:], in_=p[:]), reads=[p], writes=[t])
            q = qT.next()
            for g in range(4):
                pp = pq.next()
                for b in range(4):
                    blk = g * 4 + b
                    for k in range(8):
                        S.op("pe", lambda e: e.matmul(pp[:, b, :], lhsT=wq[:, k, blk * 128:(blk + 1) * 128], rhs=t[:, k, :],
                                                      start=(k == 0), stop=(k == 7)), reads=[wq, t], writes=[(pp, b)])
                S.op("act", lambda e: e.copy(out=q[:, g * 4:(g + 1) * 4, :], in_=pp[:]), reads=[pp], writes=[(q, g)])
            s = sc.next()
            for g in range(4):
                pp = pq.next()
                for b in range(4):
                    blk = g * 4 + b
                    S.op("pe", lambda e: e.matmul(pp[:, b, :], lhsT=q[:, blk, :], rhs=kT[:, blk, :], start=True, stop=True),
                         reads=[q, kT], writes=[(pp, b)])
                S.op("act", lambda e: e.copy(out=s[:, g * 4:(g + 1) * 4, :], in_=pp[:]), reads=[pp], writes=[(s, g)])
            return dict(r0=r0, s=s)

        def stage_b1(st_):
            r0, s = st_["r0"], st_["s"]
            tv = t1.next(); tix = ti.next()
            w16 = wk16.next()
            for blk in range(16):
                S.op("dve", lambda e: e.max(out=tv[:, blk, 0:8], in_=s[:, blk, :]), reads=[s], writes=[(tv, (blk, 0))])
            for blk in range(16):
                S.op("dve", lambda e: e.match_replace(out=w16[:, blk, :], in_to_replace=tv[:, blk, 0:8], in_values=s[:, blk, :], imm_value=-1e30),
                     reads=[s, (tv, (blk, 0))], writes=[(w16, blk)])
            for blk in range(16):
                S.op("dve", lambda e: e.max(out=tv[:, blk, 8:16], in_=w16[:, blk, :]), reads=[(w16, blk)], writes=[(tv, (blk, 1))])
            for blk in range(16):
                S.op("dve", lambda e: e.max_index(out=tix[:, blk, 0:8], in_max=tv[:, blk, 0:8], in_values=s[:, blk, :]),
                     reads=[s, (tv, (blk, 0))], writes=[(tix, (blk, 0))])
            for blk in range(16):
                S.op("dve", lambda e: e.max_index(out=tix[:, blk, 8:16], in_max=tv[:, blk, 8:16], in_values=w16[:, blk, :]),
                     reads=[(w16, blk), (tv, (blk, 1))], writes=[(tix, (blk, 1))])
            st_.update(tv=tv, tix=tix)

        def stage_b2(st_):
            tv, tix = st_["tv"], st_["tix"]
            tf = tif.next()
            S.op("pool", lambda e: e.tensor_copy(out=tf[:], in_=tix[:]), reads=[tix], writes=[tf])
            c = cs.next()
            tv4 = tv[:].rearrange("p (h s) k -> p h s k", s=2)
            S.op("dve", lambda e: e.tensor_tensor(out=c[:].rearrange("p h (i j) -> p h i j", i=16),
                                                  in0=tv4[:, :, 0, :].unsqueeze(3).to_broadcast([128, 8, 16, 16]),
                                                  in1=tv4[:, :, 1, :].unsqueeze(2).to_broadcast([128, 8, 16, 16]), op=ALU.add),
                 reads=[tv], writes=[c])
            b_ = bs.next(); bjx = bj.next()
            w8 = wk8.next()
            for h in range(8):
                S.op("dve", lambda e: e.max(out=b_[:, h, 0:8], in_=c[:, h, :]), reads=[c], writes=[(b_, (h, 0))])
            for h in range(8):
                S.op("dve", lambda e: e.match_replace(out=w8[:, h, :], in_to_replace=b_[:, h, 0:8], in_values=c[:, h, :], imm_value=-1e30),
                     reads=[c, (b_, (h, 0))], writes=[(w8, h)])
            for h in range(8):
                S.op("dve", lambda e: e.max(out=b_[:, h, 8:16], in_=w8[:, h, :]), reads=[(w8, h)], writes=[(b_, (h, 1))])
            for h in range(8):
                S.op("dve", lambda e: e.max_index(out=bjx[:, h, 0:8], in_max=b_[:, h, 0:8], in_values=c[:, h, :]),
                     reads=[c, (b_, (h, 0))], writes=[(bjx, (h, 0))])
            for h in range(8):
                S.op("dve", lambda e: e.max_index(out=bjx[:, h, 8:16], in_max=b_[:, h, 8:16], in_values=w8[:, h, :]),
                     reads=[(w8, h), (b_, (h, 1))], writes=[(bjx, (h, 1))])
            st_.update(tf=tf, b_=b_, bjx=bjx)

        def stage_b3(st_):
            r0, tf, b_, bjx = (st_[k] for k in ("r0", "tf", "b_", "bjx"))
            hx = hi.next(); lx = lo.next(); hfx = hif.next(); lfx = lof.next()
            S.op("dve", lambda e: e.tensor_single_scalar(out=hx[:], in_=bjx[:], scalar=4, op=ALU.logical_shift_right), reads=[bjx], writes=[hx])
            S.op("dve", lambda e: e.tensor_single_scalar(out=lx[:], in_=bjx[:], scalar=15, op=ALU.bitwise_and), reads=[bjx], writes=[lx])
            S.op("pool", lambda e: e.tensor_copy(out=hfx[:], in_=hx[:]), reads=[hx], writes=[hfx])
            S.op("pool", lambda e: e.tensor_copy(out=lfx[:], in_=lx[:]), reads=[lx], writes=[lfx])
            tf4 = tf[:].rearrange("p (h s) k -> p h s k", s=2)
            es = []
            for (sel, half) in ((hfx, 0), (lfx, 1)):
                o = oh.next()
                S.op("dve", lambda e: e.tensor_tensor(out=o[:], in0=sel[:].unsqueeze(3).to_broadcast([128, 8, 16, 16]),
                                                      in1=iota[:].unsqueeze(1).to_broadcast([128, 8, 16, 16]), op=ALU.is_equal),
                     reads=[sel, iota], writes=[o])
                S.op("pool", lambda e: e.tensor_tensor(out=o[:], in0=o[:], in1=tf4[:, :, half, :].unsqueeze(2).to_broadcast([128, 8, 16, 16]),
                                                       op=ALU.mult), reads=[o, tf], writes=[o])
                ex = ee.next()
                S.op("dve", lambda e: e.tensor_reduce(out=ex[:].rearrange("p h k -> p (h k)"), in_=o[:].rearrange("p h k i -> p (h k) i"),
                                                      axis=AX.X, op=ALU.add), reads=[o], writes=[ex])
                es.append(ex)
            ef = ee.next()
            S.op("dve", lambda e: e.scalar_tensor_tensor(out=ef[:], in0=es[0][:], scalar=128.0, in1=es[1][:], op0=ALU.mult, op1=ALU.add),
                 reads=[es[0], es[1]], writes=[ef])
            eix = ei.next()
            S.op("dve", lambda e: e.tensor_copy(out=eix[:], in_=ef[:].rearrange("p h k -> p (h k)")), reads=[ef], writes=[eix])
            identf = D["ident_f"]
            ptr = pq.next()
            S.op("pe", lambda e: e.transpose(out=ptr[:, 0, :], in_=ef[:].rearrange("p h k -> p (h k)"), identity=identf[:]),
                 reads=[ef, identf], writes=[(ptr, 0)])
            S.op("dve", lambda e: e.tensor_copy(out=eix[:], in_=ptr[:, 0, :]), reads=[(ptr, 0)], writes=[eix])
            S.dma("sp", lambda e: e.dma_start(out=D["pidx"][r0:r0 + 128, :], in_=eix[:]), reads=[eix])
            g_ = gg.next(); m = sm.next()
            S.op("dve", lambda e: e.tensor_tensor(out=g_[:], in0=b_[:], in1=b_[:, :, 0:1].to_broadcast([128, 8, 16]), op=ALU.subtract),
                 reads=[b_], writes=[g_])
            S.op("act", lambda e: e.activation(out=g_[:], in_=g_[:], func=AF.Exp), reads=[g_], writes=[g_])
            S.op("dve", lambda e: e.tensor_reduce(out=m[:, 0:8], in_=g_[:], axis=AX.X, op=ALU.add), reads=[g_], writes=[(m, 0)])
            S.op("dve", lambda e: e.reciprocal(out=m[:, 8:16], in_=m[:, 0:8]), reads=[(m, 0)], writes=[(m, 1)])
            S.op("dve", lambda e: e.tensor_tensor(out=g_[:], in0=g_[:], in1=m[:, 8:16].unsqueeze(2).to_broadcast([128, 8, 16]), op=ALU.mult),
                 reads=[g_, (m, 1)], writes=[g_])
            S.op("pe", lambda e: e.transpose(out=ptr[:, 1, :], in_=g_[:].rearrange("p h k -> p (h k)"), identity=identf[:]),
                 reads=[g_, identf], writes=[(ptr, 1)])
            gT_ = qT.next()
            S.op("act", lambda e: e.copy(out=gT_[:, 0, :], in_=ptr[:, 1, :]), reads=[(ptr, 1)], writes=[gT_])
            S.dma("sp", lambda e: e.dma_start(out=D["pgt"][r0:r0 + 128, :], in_=gT_[:, 0, :]), reads=[gT_])


        tiles = list(tiles)
        stt = {}
        nj = len(tiles)
        for i in range(nj + 3):
            if i < nj:
                stt[i] = stage_a(tiles[i])
            if 0 <= i - 1 < nj:
                stage_b1(stt[i - 1])
            if 0 <= i - 2 < nj:
                stage_b2(stt[i - 2])
            if 0 <= i - 3 < nj:
                stage_b3(stt.pop(i - 3))


TAB_CHUNK = 512


def start_table_convert(S, D, l):
    vals = []
    for ti, nm in enumerate(("peer_u", "peer_v")):
        sem = S.bg[2 * l + ti]
        n = 0
        for r0 in range(0, 16384, TAB_CHUNK):
            v = S.bg_dma("pool", lambda e: e.dma_start(out=D["peer_uvb%d" % l][r0:r0 + TAB_CHUNK, ti * 1024:(ti + 1) * 1024],
                                                       in_=D["%s%d" % (nm, l)][r0:r0 + TAB_CHUNK, :]), sem, n)
            n += 1
        vals.append(v)
    return vals


def phase_peer_ffn(S, D, l, tiles=range(NTT), xin="xs", xout="xs", conv_vals=None):
    tiles = list(tiles)
    if conv_vals is not None:
        for ti in range(2):
            S.wait_sem(("pool", "sp"), S.bg[2 * l + ti], conv_vals[ti])
    with S.phase():
        ident = D["ident_bf"]
        uvb = D["peer_uvb%d" % l]
        scb = [bcast_load(S, D, l, j, 4096, 1024, "scb") for j in range(2)]
        shb = [bcast_load(S, D, l, j, 3072, 1024, "shb") for j in range(2)]
        g2b = [bcast_load(S, D, l, j, 5120, 1024, "g2b") for j in range(2)]
        lng = vec_bcast(S, D["ln2_g"][l], 1024, "lng")
        lnb = vec_bcast(S, D["ln2_b"][l], 1024, "lnb")
        xt = Rot(S, 3, [128, 1024], F32, name="xt")
        hf = Rot(S, 2, [128, 1024], F32, name="hf")
        hb = Rot(S, 3, [128, 1024], BF16, name="hb")
        idx = Rot(S, 3, [128, 128], I32, name="idx")
        gt = Rot(S, 3, [128, 128], F32, name="gt")
        gb = Rot(S, 10, [128, 2048], BF16, name="gb")
        junk = Rot(S, 2, [128, 1024], BF16, name="junk")
        aT = Rot(S, 2, [128, 128], F32, name="aT")
        cf = Rot(S, 2, [128, 128], F32, name="cf")
        zb = Rot(S, 6, [128, 255], BF16, name="zb")
        for b in zb.bufs:
            S.op("pool", lambda e: e.memset(b[:], 0.0), writes=[b])
        tm = Rot(S, 2, [128, 1024], F32, name="tm")
        sm = Rot(S, 2, [128, 32], F32, name="sm")
        xo = Rot(S, 2, [128, 1024], F32, name="xo")
        pb = Rot(S, 3, [128, 1024], F32, psum=True, name="pb")
        po_r = Rot(S, 1, [128, 1024], F32, psum=True, name="po")
        def load_stage(tt):
            j = 0 if tt < 32 else 1
            r0 = tt * 128
            x = xt.next(); ix = idx.next(); g = gt.next()
            S.dma("sp", lambda e: e.dma_start(out=ix[:], in_=D["pidx"][r0:r0 + 128, :]), writes=[ix])
            S.dma("sp", lambda e: e.dma_start(out=x[:], in_=D[xin][r0:r0 + 128, :]), writes=[x])
            S.dma("sp", lambda e: e.dma_start(out=g[:], in_=D["pgt"][r0:r0 + 128, :]), writes=[g])
            h1 = hf.next()
            S.op("dve", lambda e: e.tensor_tensor(out=h1[:], in0=x[:], in1=scb[j][:], op=ALU.mult), reads=[x, scb[j]], writes=[h1])
            h = hb.next()
            S.op("dve", lambda e: e.tensor_tensor(out=h[:], in0=h1[:], in1=shb[j][:], op=ALU.add), reads=[h1, shb[j]], writes=[h])
            return (j, r0, x, ix, g, h)

        nxt = load_stage(tiles[0]) if tiles else None
        for ti_, tt in enumerate(tiles):
            j, r0, x, ix, g, h = nxt
            nxt = load_stage(tiles[ti_ + 1]) if ti_ + 1 < len(tiles) else None
            at = aT.next(); c = cf.next(); po = po_r.next()
            LAG = 3
            uvs = {}

            pbs = {}

            def stage_a0(t):
                uv = gb.next()
                uvs[t] = uv
                S.dma("pool", lambda e: e.indirect_dma_start(out=uv[:], out_offset=None, in_=uvb,
                                                             in_offset=bass.IndirectOffsetOnAxis(ap=ix[:, t:t + 1], axis=0)),
                      reads=[ix], writes=[uv])
                p = pb.next()
                pbs[t] = p
                for half in range(2):
                    S.op("pe", lambda e: e.matmul(p[:, half * 512:(half + 1) * 512], lhsT=ident[:, t:t + 1].to_broadcast([128, 128]),
                                                  rhs=h[:, half * 512:(half + 1) * 512], start=True, stop=True),
                         reads=[ident, h], writes=[(p, half)])

            def stage_a1(t):
                uv = uvs[t]
                p = pbs.pop(t)
                jk = junk.next()
                S.op("dve", lambda e: e.scalar_tensor_tensor(out=jk[:], in0=uv[:, 0:1024], scalar=1.0, in1=p[:], op0=ALU.mult, op1=ALU.mult,
                                                             accum_out=at[:, t:t + 1]), reads=[(uv, "u"), p], writes=[jk, (at, t)])
                S.op("act", lambda e: e.activation(out=c[:, t:t + 1], in_=at[:, t:t + 1], func=AF.Gelu_apprx_tanh),
                     reads=[(at, t)], writes=[(c, t)])

            def stage_b(t):
                uv = uvs.pop(t)
                z = zb.next()
                S.op("dve", lambda e: e.tensor_tensor(out=z[:, 127:128], in0=c[:, t:t + 1], in1=g[:, t:t + 1], op=ALU.mult),
                     reads=[(c, t), g], writes=[z])
                for half in range(2):
                    S.op("pe", lambda e: e.matmul(po[:, half * 512:(half + 1) * 512], lhsT=z[:, 127 - t:255 - t],
                                                  rhs=uv[:, 1024 + half * 512:1024 + (half + 1) * 512], start=(t == 0), stop=(t == 127)),
                         reads=[z, (uv, "v")], writes=[(po, half)])

            for s_i in range(128 + LAG + 1):
                if s_i < 128:
                    stage_a0(s_i)
                if 0 <= s_i - 1 < 128:
                    stage_a1(s_i - 1)
                if 0 <= s_i - 1 - LAG < 128:
                    stage_b(s_i - 1 - LAG)
            t_ = tm.next()
            S.op("dve", lambda e: e.tensor_tensor(out=t_[:], in0=po[:], in1=g2b[j][:], op=ALU.mult), reads=[po, g2b[j]], writes=[t_])
            S.op("dve", lambda e: e.scalar_tensor_tensor(out=t_[:], in0=x[:], scalar=ALPHA, in1=t_[:], op0=ALU.mult, op1=ALU.add),
                 reads=[x, t_], writes=[t_])
            t2 = tm.next(); s_ = sm.next(); o = xo.next()
            layer_norm(S, t_[:, :], [t_], 1024, lng, lnb, o, t2, s_)
            S.dma("sp", lambda e: e.dma_start(out=D[xout][r0:r0 + 128, :], in_=o[:]), reads=[o])


D_CONST = {}


def make_consts(S, D):
    mh = S.tile([128, 1], F32, "mhalf")
    S.op("pool", lambda e: e.memset(mh[:], -0.5), writes=[mh])
    D_CONST["mhalf"] = mh
    ib = S.tile([128, 128], BF16, "ident_bf")
    S.op("pool", lambda e: e.memset(ib[:], 0.0), writes=[ib])
    S.op("pool", lambda e: e.affine_select(out=ib[:], in_=ib[:], pattern=[[-1, 128]], compare_op=ALU.not_equal,
                                           fill=1.0, base=0, channel_multiplier=1), reads=[ib], writes=[ib])
    D["ident_bf"] = ib
    i32 = S.tile([128, 128], F32, "ident_f")
    S.op("pool", lambda e: e.memset(i32[:], 0.0), writes=[i32])
    S.op("pool", lambda e: e.affine_select(out=i32[:], in_=i32[:], pattern=[[-1, 128]], compare_op=ALU.not_equal,
                                           fill=1.0, base=0, channel_multiplier=1), reads=[i32], writes=[i32])
    D["ident_f"] = i32


INPUT_SPECS = {
    "xs": ([NTOK, 1024], F32),
    "cvecT": ([128, 8, 2], F32),
    "ada_w": ([2, 1024, 6144], F32),
    "ada_b": ([2, 6144], F32),
    "w_in": ([2, 1024, W_IN_COLS], F32),
    "w_branch": ([2, 1024, 1024], F32),
    "w_out": ([2, 1024, 1024], F32),
    "ln1_g": ([2, 1024], F32),
    "ln1_b": ([2, 1024], F32),
    "na_bias": ([2, 4, 8, 64, 512], F32),
    "ropeC": ([64, 64, 2, 8], F32),
    "ropeS": ([64, 64, 2, 8], F32),
    "gla_mt": ([64, 2, 64], F32),
    "gla_tri": ([64, 2, 64], F32),
    "gla_blk": ([128, 4, 64], F32),
    "gla_gw": ([2, 2, 33, 128], F32),
    "gla_norm_g": ([2, 256], F32),
    "peer_wq": ([2, 1024, 2048], F32),
    "peer_keysT": ([2, 16, 128, 128], F32),
    "peer_u0": ([16384, 1024], F32),
    "peer_u1": ([16384, 1024], F32),
    "peer_v0": ([16384, 1024], F32),
    "peer_v1": ([16384, 1024], F32),
    "ln2_g": ([2, 1024], F32),
    "ln2_b": ([2, 1024], F32),
    "iota_kk": ([128, 16, 16], F32),
    "conv_dwT": ([2, 256, 31], F32),
    "conv_bT": ([2, 128, 2], F32),
    "conv_ln_g": ([2, 256], F32),
    "conv_ln_b": ([2, 256], F32),
    "sgu_ln_g": ([2, 256], F32),
    "sgu_ln_b": ([2, 256], F32),
    "sgu_wsT": ([2, 4, 128, 128], F32),
    "sgu_bsT": ([2, 128, 4], F32),
}
SCRATCH_SPECS = {
    "modv": ([2, 2, 6144], F32),
    "z": ([NTOK, W_IN_COLS], F32),
    "ycat": ([NTOK, 1024], F32),
    "ofwd": ([NTOK, 256], F32),
    "pidx": ([NTOK, 128], I32),
    "pgt": ([NTOK, 128], F32),
    "xa": ([NTOK, 1024], F32),
    "peer_uvb0": ([16384, 2048], BF16),
    "peer_uvb1": ([16384, 2048], BF16),
    "out": ([SEQ, 1024], F32),
}


def build_program(plan, ext_in=(), ext_out=(), inputs=None):
    nc = bass.Bass("TRN2", target_bir_lowering=False)
    D = {}
    for name, (shape, dt) in INPUT_SPECS.items():
        if inputs is not None and name not in inputs:
            continue
        D[name] = nc.dram_tensor(name, shape, dt, kind="ExternalInput").ap()
    for name, (shape, dt) in SCRATCH_SPECS.items():
        kind = "ExternalInput" if name in ext_in else ("ExternalOutput" if name in ext_out else "Internal")
        D[name] = nc.dram_tensor(name, shape, dt, kind=kind).ap()
    with ExitStack() as st:
        S = Sync(nc, st)
        make_consts(S, D)
        plan(S, D)
        S.barrier()
    return nc


def na_bias_table(rpb):
    L = rpb.shape[0]
    W = 64
    col = np.arange(W)
    c0 = np.clip(col - 8, 0, W - 16)
    in_win = (col[None, :] >= c0[:, None]) & (col[None, :] < c0[:, None] + 16)
    dc = np.clip(col[None, :] - col[:, None], -15, 15) + 15
    out = np.empty((L, 4, 8, 64, 512), np.float32)
    reps = [0, 1, 2, 3, 30, 61, 62, 63]
    for ci, r in enumerate(reps):
        r0 = min(max(r - 4, 0), 56)
        for k in range(8):
            dr = r0 + k - r + 7
            b = rpb[:, :, dr][:, :, dc]
            out[:, :, ci, :, k * 64:(k + 1) * 64] = np.where(in_win[None, None], b, np.float32(-1e30))
    return out


def gla_consts():
    inv = (1.0 / (np.float32(100.0) ** (np.arange(8, dtype=np.float32) / np.float32(8)))).astype(np.float32)
    pos = np.arange(64, dtype=np.float32)
    ang = (pos[:, None] * inv[None, :]).astype(np.float32)
    C = np.cos(ang).astype(np.float32)
    Sn = np.sin(ang).astype(np.float32)
    ropeC = np.empty((64, 64, 2, 8), np.float32)
    ropeS = np.empty((64, 64, 2, 8), np.float32)
    ropeC[:, :, 0, :] = C[None, :, :]
    ropeS[:, :, 0, :] = Sn[None, :, :]
    ropeC[:, :, 1, :] = C[:, None, :]
    ropeS[:, :, 1, :] = Sn[:, None, :]
    s = np.arange(64)[:, None]
    c = np.arange(64)[None, :]
    tri = np.stack([(s <= c), (s >= c)], 1).astype(np.float32)
    mt = (tri * np.float32(-1.0 / 16)).astype(np.float32)
    blk = np.zeros((128, 4, 64), np.float32)
    for h in range(4):
        blk[h * 32:(h + 1) * 32, h, :] = 1
    return dict(ropeC=ropeC, ropeS=ropeS, gla_mt=mt, gla_tri=tri, gla_blk=blk)


def gla_gw_layout(gate_up, gate_b):
    L = gate_up.shape[0]
    out = np.zeros((L, 2, 33, 128), np.float32)
    for d in range(2):
        out[:, d, d * 16:(d + 1) * 16, :] = gate_up[:, d]
        out[:, d, 32, :] = gate_b[:, d]
    return out


def host_weights(inp):
    f = lambda a: np.ascontiguousarray(np.asarray(a, dtype=np.float32))
    W = {}
    for l in range(2):
        W["peer_u%d" % l] = f(np.asarray(inp["peer_u"])[l])
        W["peer_v%d" % l] = f(np.asarray(inp["peer_v"])[l])
    for k in ("ada_w", "ada_b", "w_in", "w_out", "ln1_g", "ln1_b", "peer_wq", "ln2_g", "ln2_b",
              "conv_ln_g", "conv_ln_b", "sgu_ln_g", "sgu_ln_b"):
        W[k] = f(inp[k])
    W["w_branch"] = f(np.asarray(inp["w_branch"]).reshape(2, 1024, 1024))
    W["na_bias"] = na_bias_table(f(inp["na_rpb"]))
    W.update(gla_consts())
    W["gla_gw"] = gla_gw_layout(f(inp["gla_gate_up"]), f(inp["gla_gate_b"]))
    W["gla_norm_g"] = f(np.asarray(inp["gla_norm_g"]).reshape(2, 256))
    W["conv_dwT"] = f(np.asarray(inp["conv_dw"]).transpose(0, 2, 1))
    W["conv_bT"] = f(np.asarray(inp["conv_b"]).reshape(2, 2, 128).transpose(0, 2, 1))
    W["sgu_wsT"] = f(np.asarray(inp["sgu_ws"]).transpose(0, 1, 3, 2))
    W["sgu_bsT"] = f(np.asarray(inp["sgu_bs"]).transpose(0, 2, 1))
    W["peer_keysT"] = f(np.asarray(inp["peer_keys"]).reshape(2, 16, 128, 128).transpose(0, 1, 3, 2))
    W["iota_kk"] = f(np.broadcast_to(np.arange(16, dtype=np.float32)[None, None, :], (128, 16, 16)))
    return W


def full_plan(S, D):
    cv = {}
    for l in range(DEPTH):
        need_ctx = l < DEPTH - 1
        tl = range(NTT) if need_ctx else range(32)
        xin = "xs" if l == 0 else "xa"
        phase_mod(S, D, l)
        phase_win(S, D, l, xin=xin, post_weights=(lambda l_=l: cv.__setitem__(l_, start_table_convert(S, D, l_))))
        phase_na(S, D, l, need_ctx=need_ctx)
        phase_gla(S, D, l, need_ctx=need_ctx)
        phase_conv(S, D, l, do_ctx=need_ctx)
        phase_sgu(S, D, l, tiles=tl)
        phase_merge(S, D, l, tiles=tl, xin=xin, xout="xa")
        phase_peer_topk(S, D, l, tiles=tl, xin="xa")
        phase_peer_ffn(S, D, l, tiles=tl, xin="xa", xout=("xa" if need_ctx else "out"), conv_vals=cv[l])


_CACHE = {}


def kernel(**inputs):
    x = np.asarray(inputs["x"], dtype=np.float32)
    c = np.asarray(inputs["c"], dtype=np.float32)
    ctx = np.asarray(inputs["ctx"], dtype=np.float32)
    c_ctx = np.asarray(inputs["c_ctx"], dtype=np.float32)
    B = x.shape[0]
    W = host_weights(inputs)
    if "nc" not in _CACHE:
        _CACHE["nc"] = build_program(full_plan, ext_out=("out",))
    nc = _CACHE["nc"]
    in_maps = []
    for b in range(B):
        m = dict(W)
        m["xs"] = np.ascontiguousarray(np.concatenate([x[b], ctx[b]], 0))
        cvec = np.stack([c[b], c_ctx], 0)
        m["cvecT"] = np.ascontiguousarray(cvec.reshape(2, 8, 128).transpose(2, 1, 0))
        in_maps.append(m)
    res = run_bass_kernel_spmd(nc, in_maps, core_ids=list(range(B)))
    return np.stack([np.asarray(r["out"], dtype=np.float32) for r in res.results], 0)
```
